# Optimizing a Trainium2 kernel written in Bass

```python
import numpy as np
import jax
import jax.numpy as jnp
from jax import lax

D_MODEL = 2048
BATCH = 2
SEQ = 16384
DEPTH = 1

PLE_DIM = 256
HEAD_DIM = 64
N_HEADS = D_MODEL // 128
N_KV_HEADS = max(1, N_HEADS // 8)
GQA_GROUP = N_HEADS // N_KV_HEADS
WINDOW = 128
M_HEADS = 4
M_DV = D_MODEL // (2 * M_HEADS)
M_DQK = M_DV // 2
M_CHUNK = 64
GATE_SOFTCAP = 15.0
N_EXPERTS = 64
TOP_K = 8
D_EXPERT = D_MODEL // 4
D_SHARED = D_MODEL // 4
ROUTED_SCALE = 2.5
MOE_BLOCK = 256
LN_EPS = 1e-5
RMS_EPS = 1e-6
DEEPNORM_ALPHA = (2.0 * DEPTH) ** 0.25
DEEPNORM_BETA = (8.0 * DEPTH) ** -0.25

ATTN_Q_W = N_HEADS * HEAD_DIM
ATTN_KV_W = N_KV_HEADS * HEAD_DIM
M_QK_W = M_HEADS * M_DQK
M_V_W = M_HEADS * M_DV
IN_SIZES = (ATTN_Q_W, ATTN_KV_W, ATTN_KV_W, M_QK_W, M_QK_W, M_V_W, M_V_W, M_HEADS, M_HEADS, D_MODEL, D_MODEL)
IN_COLS = sum(IN_SIZES)

kernel_name = 'hybrid_swa_mlstm_moe_deepnorm'


def layer_norm(x, g, b):
    xf = x.astype(jnp.float32)
    mu = jnp.mean(xf, axis=-1, keepdims=True)
    var = jnp.mean(jnp.square(xf - mu), axis=-1, keepdims=True)
    y = (xf - mu) * lax.rsqrt(var + LN_EPS) * g.astype(jnp.float32) + b.astype(jnp.float32)
    return y.astype(x.dtype)


def soft_cap(z):
    return GATE_SOFTCAP * jnp.tanh(z / GATE_SOFTCAP)


def alibi_slopes():
    return jnp.exp2(-8.0 / N_HEADS * jnp.arange(1, N_HEADS + 1, dtype=jnp.float32))


def swa_sink_attention(q, k, v, sinks):
    B, S = q.shape[0], q.shape[1]
    nb = S // WINDOW
    qb = q.reshape(B, nb, WINDOW, N_KV_HEADS, GQA_GROUP, HEAD_DIM)
    kb = k.reshape(B, nb, WINDOW, N_KV_HEADS, HEAD_DIM)
    vb = v.reshape(B, nb, WINDOW, N_KV_HEADS, HEAD_DIM)
    pad = ((0, 0), (1, 0), (0, 0), (0, 0), (0, 0))
    kk = jnp.concatenate([jnp.pad(kb[:, :-1], pad), kb], axis=2)
    vv = jnp.concatenate([jnp.pad(vb[:, :-1], pad), vb], axis=2)
    s = jnp.einsum('bnqhgd,bnkhd->bnhgqk', qb, kk).astype(jnp.float32) * (HEAD_DIM ** -0.5)
    qi = jnp.arange(WINDOW)[:, None]
    kj = jnp.arange(2 * WINDOW)[None, :]
    dist = qi - kj + WINDOW
    in_band = (dist >= 0) & (dist < WINDOW)
    key_pos = jnp.arange(nb)[:, None] * WINDOW - WINDOW + jnp.arange(2 * WINDOW)[None, :]
    mask = in_band[None] & (key_pos >= 0)[:, None, :]
    slopes = alibi_slopes().reshape(N_KV_HEADS, GQA_GROUP)
    bias = -slopes[:, :, None, None] * dist.astype(jnp.float32)
    s = jnp.where(mask[None, :, None, None], s + bias, -jnp.inf)
    sink = jnp.broadcast_to(sinks.astype(jnp.float32).reshape(N_KV_HEADS, GQA_GROUP, 1, 1), s.shape[:-1] + (1,))
    probs = jax.nn.softmax(jnp.concatenate([s, sink], axis=-1), axis=-1)[..., :-1]
    o = jnp.einsum('bnhgqk,bnkhd->bnqhgd', probs.astype(v.dtype), vv)
    return o.reshape(B, S, N_HEADS * HEAD_DIM)


def _to_chunks(a, nc):
    B = a.shape[0]
    a = a.reshape((B, nc, M_CHUNK) + a.shape[2:])
    perm = (1, 0, 3, 2) + tuple(range(4, a.ndim))
    return a.transpose(perm)


def mlstm_chunkwise(q, k, v, i_pre, f_pre, norm_g):
    B, S = q.shape[0], q.shape[1]
    nc = S // M_CHUNK
    f32 = jnp.float32
    qc = _to_chunks(q.astype(f32), nc)
    kc = _to_chunks(k.astype(f32) * (M_DQK ** -0.5), nc)
    vc = _to_chunks(v.astype(f32), nc)
    igc = _to_chunks(soft_cap(i_pre.astype(f32)), nc)
    lfc = _to_chunks(jax.nn.log_sigmoid(soft_cap(f_pre.astype(f32))), nc)
    causal = jnp.tril(jnp.ones((M_CHUNK, M_CHUNK), dtype=bool))

    def step(carry, inp):
        C, n, m = carry
        qx, kx, vx, ig, lf = inp
        b = jnp.cumsum(lf, axis=-1)
        inter = b + m[..., None]
        dmat = jnp.where(causal, b[..., :, None] - b[..., None, :] + ig[..., None, :], -jnp.inf)
        mt = jnp.maximum(inter, jnp.max(dmat, axis=-1))
        w = jnp.exp(dmat - mt[..., None])
        sqk = jnp.einsum('bhtd,bhsd->bhts', qx, kx) * w
        si = jnp.exp(inter - mt)
        num = si[..., None] * jnp.einsum('bhtd,bhdv->bhtv', qx, C) + jnp.einsum('bhts,bhsv->bhtv', sqk, vx)
        nq = si * jnp.einsum('bhtd,bhd->bht', qx, n) + jnp.sum(sqk, axis=-1)
        h = num / jnp.maximum(jnp.abs(nq), jnp.exp(-mt))[..., None]
        bl = b[..., -1]
        g = bl[..., None] - b + ig
        m_new = jnp.maximum(bl + m, jnp.max(g, axis=-1))
        decay = jnp.exp(bl + m - m_new)
        wg = jnp.exp(g - m_new[..., None])
        C_new = decay[..., None, None] * C + jnp.einsum('bhs,bhsd,bhsv->bhdv', wg, kx, vx)
        n_new = decay[..., None] * n + jnp.einsum('bhs,bhsd->bhd', wg, kx)
        return (C_new, n_new, m_new), h

    init = (jnp.zeros((B, M_HEADS, M_DQK, M_DV), f32), jnp.zeros((B, M_HEADS, M_DQK), f32), jnp.zeros((B, M_HEADS), f32))
    _, hs = lax.scan(step, init, (qc, kc, vc, igc, lfc))
    h = hs.transpose(1, 0, 3, 2, 4).reshape(B, S, M_HEADS, M_DV)
    h = h * lax.rsqrt(jnp.mean(jnp.square(h), axis=-1, keepdims=True) + RMS_EPS) * norm_g.astype(f32)
    return h.astype(q.dtype)


def moe_ffn(xf, w_router, b_router, w_eg, w_eu, w_ed, w_sg, w_su, w_sd):
    T, D = xf.shape
    scores = jax.nn.sigmoid((xf @ w_router).astype(jnp.float32))
    _, idx = lax.top_k(scores + b_router.astype(jnp.float32), TOP_K)
    gw = jnp.take_along_axis(scores, idx, axis=-1)
    gw = gw / jnp.sum(gw, axis=-1, keepdims=True) * ROUTED_SCALE
    e_flat = idx.reshape(-1)
    w_flat = gw.reshape(-1)
    tok_flat = jnp.arange(T * TOP_K, dtype=jnp.int32) // TOP_K
    order = jnp.argsort(e_flat)
    e_sorted = e_flat[order]
    counts = jnp.bincount(e_flat, length=N_EXPERTS)
    starts = jnp.cumsum(counts) - counts
    padded = (counts + MOE_BLOCK - 1) // MOE_BLOCK * MOE_BLOCK
    ends = jnp.cumsum(padded)
    pstarts = ends - padded
    dest = pstarts[e_sorted] + (jnp.arange(T * TOP_K) - starts[e_sorted])
    n_blocks = -(-(T * TOP_K) // MOE_BLOCK) + N_EXPERTS
    n_rows = n_blocks * MOE_BLOCK
    row_tok = jnp.full((n_rows,), T, dtype=jnp.int32).at[dest].set(tok_flat[order])
    row_w = jnp.zeros((n_rows,), jnp.float32).at[dest].set(w_flat[order])
    block_e = jnp.minimum(jnp.searchsorted(ends, jnp.arange(n_blocks) * MOE_BLOCK, side='right'), N_EXPERTS - 1)
    x_pad = jnp.concatenate([xf, jnp.zeros((1, D), xf.dtype)], axis=0)

    def body(acc, blk):
        toks, wts, e = blk
        xb = x_pad[toks]
        hb = jax.nn.silu(xb @ w_eg[e]) * (xb @ w_eu[e])
        yb = (hb @ w_ed[e]).astype(jnp.float32) * wts[:, None]
        return acc.at[toks].add(yb), None

    acc0 = jnp.zeros((T + 1, D), jnp.float32)
    acc, _ = lax.scan(body, acc0, (row_tok.reshape(n_blocks, MOE_BLOCK), row_w.reshape(n_blocks, MOE_BLOCK), block_e))
    shared = (jax.nn.silu(xf @ w_sg) * (xf @ w_su)) @ w_sd
    return (acc[:T] + shared.astype(jnp.float32)).astype(xf.dtype)


def hybrid_layer(x, p_i, w_in, sinks, b_i, b_f, norm_g, w_ba, w_bm, w_out, ln_mix_g, ln_mix_b,
                 w_router, b_router, w_eg, w_eu, w_ed, w_sg, w_su, w_sd, ln_ffn_g, ln_ffn_b,
                 w_pp, w_pg, ln_ple_g, ln_ple_b):
    B, S, D = x.shape
    proj = x @ w_in
    splits = np.cumsum(IN_SIZES)[:-1].tolist()
    aq, ak, av, mq, mk, mv, mo, mi, mf, ga, gb = jnp.split(proj, splits, axis=-1)
    attn = swa_sink_attention(aq.reshape(B, S, N_HEADS, HEAD_DIM), ak.reshape(B, S, N_KV_HEADS, HEAD_DIM),
                              av.reshape(B, S, N_KV_HEADS, HEAD_DIM), sinks)
    mh = mlstm_chunkwise(mq.reshape(B, S, M_HEADS, M_DQK), mk.reshape(B, S, M_HEADS, M_DQK),
                         mv.reshape(B, S, M_HEADS, M_DV), mi + b_i, mf + b_f, norm_g)
    mh = (mh * jax.nn.sigmoid(mo).reshape(B, S, M_HEADS, M_DV)).reshape(B, S, M_V_W)
    merged = jax.nn.sigmoid(ga) * (attn @ w_ba) + jax.nn.sigmoid(gb) * (mh @ w_bm)
    x = layer_norm(DEEPNORM_ALPHA * x + merged @ w_out, ln_mix_g, ln_mix_b)
    ffn = moe_ffn(x.reshape(B * S, D), w_router, b_router, w_eg, w_eu, w_ed, w_sg, w_su, w_sd).reshape(B, S, D)
    x = layer_norm(DEEPNORM_ALPHA * x + ffn, ln_ffn_g, ln_ffn_b)
    ple = (p_i @ w_pp) * jax.nn.sigmoid(x @ w_pg)
    x = layer_norm(DEEPNORM_ALPHA * x + ple, ln_ple_g, ln_ple_b)
    return x


def setup_inputs(seed: int = 0) -> dict:
    key = jax.random.key(seed)
    ks = jax.random.split(key, 32)
    f32 = jnp.float32

    def nrm(k, shape, scale):
        return jax.random.normal(k, shape, f32) * scale

    L, D, E = DEPTH, D_MODEL, N_EXPERTS
    return {
        'x': nrm(ks[0], (BATCH, SEQ, D), 1.0),
        'p': nrm(ks[1], (DEPTH, BATCH, SEQ, PLE_DIM), 1.0),
        'ln_in_g': 1.0 + nrm(ks[2], (D,), 0.02),
        'ln_in_b': nrm(ks[3], (D,), 0.02),
        'w_in': nrm(ks[4], (L, D, IN_COLS), D ** -0.5),
        'attn_sinks': nrm(ks[5], (L, N_HEADS), 1.0),
        'mlstm_b_i': nrm(ks[6], (L, M_HEADS), 0.1),
        'mlstm_b_f': 3.0 + nrm(ks[7], (L, M_HEADS), 0.5),
        'mlstm_norm_g': 1.0 + nrm(ks[8], (L, M_HEADS, M_DV), 0.02),
        'w_branch_attn': nrm(ks[9], (L, ATTN_Q_W, D), ATTN_Q_W ** -0.5),
        'w_branch_mlstm': nrm(ks[10], (L, M_V_W, D), M_V_W ** -0.5),
        'w_out': nrm(ks[11], (L, D, D), D ** -0.5 * DEEPNORM_BETA),
        'ln_mix_g': 1.0 + nrm(ks[12], (L, D), 0.02),
        'ln_mix_b': nrm(ks[13], (L, D), 0.02),
        'w_router': nrm(ks[14], (L, D, E), D ** -0.5),
        'b_router': nrm(ks[15], (L, E), 0.01),
        'w_exp_gate': nrm(ks[16], (L, E, D, D_EXPERT), D ** -0.5),
        'w_exp_up': nrm(ks[17], (L, E, D, D_EXPERT), D ** -0.5),
        'w_exp_down': nrm(ks[18], (L, E, D_EXPERT, D), D_EXPERT ** -0.5 * DEEPNORM_BETA),
        'w_sh_gate': nrm(ks[19], (L, D, D_SHARED), D ** -0.5),
        'w_sh_up': nrm(ks[20], (L, D, D_SHARED), D ** -0.5),
        'w_sh_down': nrm(ks[21], (L, D_SHARED, D), D_SHARED ** -0.5 * DEEPNORM_BETA),
        'ln_ffn_g': 1.0 + nrm(ks[22], (L, D), 0.02),
        'ln_ffn_b': nrm(ks[23], (L, D), 0.02),
        'w_ple_proj': nrm(ks[24], (L, PLE_DIM, D), PLE_DIM ** -0.5 * DEEPNORM_BETA),
        'w_ple_gate': nrm(ks[25], (L, D, D), D ** -0.5),
        'ln_ple_g': 1.0 + nrm(ks[26], (L, D), 0.02),
        'ln_ple_b': nrm(ks[27], (L, D), 0.02),
    }


def reference(x, p, ln_in_g, ln_in_b, w_in, attn_sinks, mlstm_b_i, mlstm_b_f, mlstm_norm_g,
              w_branch_attn, w_branch_mlstm, w_out, ln_mix_g, ln_mix_b, w_router, b_router,
              w_exp_gate, w_exp_up, w_exp_down, w_sh_gate, w_sh_up, w_sh_down, ln_ffn_g, ln_ffn_b,
              w_ple_proj, w_ple_gate, ln_ple_g, ln_ple_b):
    h = layer_norm(x, ln_in_g, ln_in_b)
    for i in range(DEPTH):
        h = hybrid_layer(h, p[i], w_in[i], attn_sinks[i], mlstm_b_i[i], mlstm_b_f[i], mlstm_norm_g[i],
                         w_branch_attn[i], w_branch_mlstm[i], w_out[i], ln_mix_g[i], ln_mix_b[i],
                         w_router[i], b_router[i], w_exp_gate[i], w_exp_up[i], w_exp_down[i],
                         w_sh_gate[i], w_sh_up[i], w_sh_down[i], ln_ffn_g[i], ln_ffn_b[i],
                         w_ple_proj[i], w_ple_gate[i], ln_ple_g[i], ln_ple_b[i])
    return h
```

```python
import numpy as np
from contextlib import ExitStack
import concourse.bass as bass
import concourse.mybir as mybir
from concourse.bass_utils import run_bass_kernel_spmd

F32 = mybir.dt.float32
BF16 = mybir.dt.bfloat16
AF = mybir.ActivationFunctionType
ALU = mybir.AluOpType
AX = mybir.AxisListType

D = 2048
NCH = 16
NE = 64
ALPHA = 2.0 ** 0.25
IALPHA = 1.0 / ALPHA
EPS_A = 1e-5 / (ALPHA * ALPHA)
C_AQ, C_AK, C_AV, C_MQ, C_MK, C_MV, C_MO, C_MI, C_GA, C_GB = 0, 1024, 1152, 1280, 1792, 2304, 3328, 4352, 4360, 6408
NEGBIG = -30000.0


class Slot:
    __slots__ = ("name", "w", "r", "dsem", "dcnt")

    def __init__(self, name=""):
        self.name = name; self.w = None; self.r = {}; self.dsem = None; self.dcnt = 0


class Eng:
    def __init__(self, obj, sem):
        self.obj = obj; self.sem = sem; self.n = 0; self.waited = {}; self.own = {id(sem)}


class Sched:
    def __init__(self, nc, sems):
        self.nc = nc
        self.E = {"pe": Eng(nc.tensor, sems["pe"]), "dve": Eng(nc.vector, sems["dve"]),
                  "act": Eng(nc.scalar, sems["act"]), "pool": Eng(nc.gpsimd, sems["pool"]),
                  "sp": Eng(nc.sync, sems["sp"])}
        self.nops = 0; self.nwaits = 0

    def new_epoch(self, sems):
        for k, e in self.E.items():
            e.sem = sems[k]; e.n = 0; e.own.add(id(e.sem))

    def _wait(self, e, deps):
        best = {}
        for (sem, val, raw) in deps:
            if id(sem) in e.own and not raw:
                continue
            k = id(sem)
            if e.waited.get(k, 0) >= val:
                continue
            if k not in best or best[k][1] < val:
                best[k] = (sem, val)
        for k, (sem, val) in best.items():
            e.obj.wait_ge(sem, val); e.waited[k] = val; self.nwaits += 1

    def _deps(self, reads, writes):
        deps = []
        for s in reads:
            if s.w is not None:
                deps.append((s.w[0], s.w[1], True))
        for s in writes:
            if s.w is not None:
                deps.append((s.w[0], s.w[1], False))
            deps.extend((a, b, False) for (a, b) in s.r.values())
        return deps

    def op(self, eng, fn, reads=(), writes=(), inc=True):
        e = self.E[eng]
        self._wait(e, self._deps(reads, writes))
        inst = fn(e.obj)
        ev = (e.sem, e.n + 1)
        if inc:
            inst.then_inc(e.sem, 1); e.n += 1
        for s in reads:
            s.r[id(e.sem)] = ev
        for s in writes:
            s.w = ev; s.r = {}
        self.nops += 1
        return ev

    def dma(self, eng, out, in_, reads=(), writes=(), dslot=None):
        e = self.E[eng]
        waw = {(id(w.w[0]), w.w[1]) for w in writes if w.w is not None and w.w[0] is dslot.dsem}
        self._wait(e, [d for d in self._deps(reads, writes) if (id(d[0]), d[1]) not in waw])
        dslot.dcnt += 1
        e.obj.dma_start(out=out, in_=in_).then_inc(dslot.dsem, 16)
        ev = (dslot.dsem, 16 * dslot.dcnt)
        for s in reads:
            s.r[id(dslot.dsem)] = ev
        for s in writes:
            s.w = ev; s.r = {}
        return ev

    def wait_all(self, eng, slots):
        e = self.E[eng]
        deps = []
        for s in slots:
            if s.w is not None:
                deps.append((s.w[0], s.w[1], True))
            deps.extend((a, b, True) for (a, b) in s.r.values())
        self._wait(e, deps)

    @staticmethod
    def alias(olds, news):
        m = {}
        for s in olds:
            evs = list(s.r.values()) + ([s.w] if s.w is not None else [])
            for (sem, val) in evs:
                k = id(sem)
                if k not in m or m[k][1] < val:
                    m[k] = (sem, val)
        for s in news:
            s.w = None; s.r = dict(m)


def build(TOK, NPRE, SPARSE=True):
    NT = TOK // 128
    NBLK = TOK // 512
    NTT = NPRE + NT
    nc = bass.Bass("TRN2", target_bir_lowering=False)

    def din(name, shape, dt=F32):
        return nc.dram_tensor(name, list(shape), dt, kind="ExternalInput").ap()

    xin = din("xin", [NTT * 128, D])
    pin = din("pin", [TOK, 256])
    vmk = din("vmk", [128, NPRE + 1])
    cst = din("cst", [128, 4, 128])
    abias_d = din("abias", [128, 2, 16, 128])
    lng = din("lng", [8, D])
    w_in = din("w_in", [D, 8456])
    sinks = din("sinks", [1, 16])
    bif_d = din("bif", [1, 8])
    ng_d = din("ng", [1, 1024])
    w_ba = din("w_ba", [1024, D])
    w_bm = din("w_bm", [1024, D])
    w_out = din("w_out", [D, D])
    w_rt = din("w_rt", [D, NE])
    b_rt = din("b_rt", [1, NE])
    w_eg = din("w_eg", [NE + 1, D, 512])
    w_eu = din("w_eu", [NE + 1, D, 512])
    w_ed = din("w_ed", [NE + 1, 512, D])
    w_pp = din("w_pp", [256, D])
    w_pg = din("w_pg", [D, D])
    out = nc.dram_tensor("out", [TOK, D], F32, kind="ExternalOutput").ap()
    flag_o = nc.dram_tensor("flag", [128, 1], F32, kind="ExternalOutput").ap()
    import os as _os
    PRECAST = SPARSE and bool(_os.environ.get('KDBG_PC'))
    if PRECAST:
        wbg = nc.dram_tensor("wbg", [NE + 1, D, 512], BF16, kind="Internal").ap()
        wbu = nc.dram_tensor("wbu", [NE + 1, D, 512], BF16, kind="Internal").ap()
        wbd = nc.dram_tensor("wbd", [NE + 1, 512, D], BF16, kind="Internal").ap()

    with ExitStack() as es:
        def sb(name, shape, dt):
            return es.enter_context(nc.sbuf_tensor(name, list(shape), dt))

        def sem(name):
            return es.enter_context(nc.semaphore(name))

        nsem = [0]

        def engsems():
            nsem[0] += 1
            return {k: sem("e%d_%s" % (nsem[0], k)) for k in ["pe", "dve", "act", "pool", "sp"]}

        def dslot(name):
            s = Slot(name); s.dsem = sem("d_" + name); return s

        S = Sched(nc, engsems())

        WSN = 54816
        R = sb("R", [128, 4, D], F32)
        xT = sb("xT", [128, NCH, 512], BF16)
        WS = sb("WS", [128, WSN], BF16)
        cf = sb("cf", [128, 4, 128], F32)
        idb = sb("idb", [128, 128], BF16)
        onesb = sb("onesb", [128, 128], BF16)
        abias = sb("abias_s", [128, 2, 16, 128], F32)
        esink = sb("esink", [128, 16], F32)
        bif = sb("bifs", [128, 8], F32)
        ngB = sb("ngB", [128, 1024], F32)
        brB = sb("brB", [128, NE], F32)
        vm = sb("vm", [128, NPRE + 1], F32)
        kvb = sb("kvb", [128, 1], F32)
        wrt = sb("wrt", [128, NCH, NE], BF16)
        C32 = sb("C32", [128, 4, 256], F32)
        n32 = sb("n32", [128, 4], F32)
        Cb = sb("Cb", [128, 4, 258], BF16)
        gw = sb("gw", [128, 4, NE + 1], F32)
        sm = sb("sm", [128, 160], F32)
        st6 = sb("st6", [128, 4, 6], F32)
        junk = sb("junk", [128, 256], BF16)
        stri = sb("stri", [128, 128], BF16)
        flagmax = sb("flagmax", [128, 1], F32)
        gB = sb("gB", [128, D], F32)
        bB = sb("bB", [128, D], F32)
        idf = cf[:, 0, :]; tri = cf[:, 1, :]; onesf = cf[:, 2, :]; iotaf = cf[:, 3, :]

        P = [es.enter_context(nc.psum_tensor("ps%d" % i, [128, 512], F32)) for i in range(8)]
        PS = [Slot("ps%d" % i) for i in range(8)]

        s_R = [Slot("R%d" % i) for i in range(4)]
        s_xT = Slot("xT")
        s_c = dslot("c"); s_ab = dslot("ab"); s_es = dslot("es"); s_bif = dslot("bif"); s_ng = dslot("ng")
        s_br = dslot("br"); s_vm = dslot("vm"); s_wrt = dslot("wrt"); s_gB = dslot("gB"); s_bB = dslot("bB")
        s_idb = Slot("idb"); s_C = Slot("C"); s_Cb = Slot("Cb"); s_gw = Slot("gw"); s_sm = Slot("sm")
        s_st6 = Slot("st"); s_junk = Slot("junk"); s_kvb = Slot("kvb")
        s_xl = [dslot("xl%d" % i) for i in range(4)]

        def ws(off, n, dt=BF16):
            if dt == F32:
                return WS[:, off:off + 2 * n].bitcast(F32)
            return WS[:, off:off + n]

        block = es.enter_context(nc.Block())

        @block.sync
        def _(sync):
            dve = lambda fn, r=(), w=(): S.op("dve", fn, r, w)
            act = lambda fn, r=(), w=(): S.op("act", fn, r, w)
            pool = lambda fn, r=(), w=(): S.op("pool", fn, r, w)
            pe = lambda fn, r=(), w=(), inc=True: S.op("pe", fn, r, w, inc)

            S.dma("sp", cf[:], cst, writes=[s_c], dslot=s_c)
            S.dma("sp", abias[:], abias_d, writes=[s_ab], dslot=s_ab)
            S.dma("sp", esink[:], sinks.partition_broadcast(128).rearrange("p a b -> p (a b)"), writes=[s_es], dslot=s_es)
            S.dma("sp", bif[:], bif_d.partition_broadcast(128).rearrange("p a b -> p (a b)"), writes=[s_bif], dslot=s_bif)
            S.dma("sp", ngB[:], ng_d.partition_broadcast(128).rearrange("p a b -> p (a b)"), writes=[s_ng], dslot=s_ng)
            S.dma("sp", brB[:], b_rt.partition_broadcast(128).rearrange("p a b -> p (a b)"), writes=[s_br], dslot=s_br)
            S.dma("sp", vm[:], vmk, writes=[s_vm], dslot=s_vm)
            S.dma("pool", wrt[:], w_rt.rearrange("(c p) n -> p c n", p=128), writes=[s_wrt], dslot=s_wrt)
            dve(lambda v: v.tensor_copy(out=idb[:], in_=idf), [s_c], [s_idb])
            dve(lambda v: v.tensor_copy(out=onesb[:], in_=onesf), [s_c], [s_idb])
            act(lambda a: a.activation(out=esink[:], in_=esink[:], func=AF.Exp), [s_es], [s_es])
            dve(lambda v: v.tensor_scalar(out=kvb[:], in0=vm[:, NPRE:NPRE + 1], scalar1=-1.0, scalar2=-NEGBIG,
                                          op0=ALU.add, op1=ALU.mult), [s_vm], [s_kvb])
            dve(lambda v: v.memset(C32[:], 0.0), [], [s_C])
            dve(lambda v: v.memset(n32[:], 0.0), [], [s_C])
            dve(lambda v: v.memset(Cb[:], 0.0), [], [s_Cb])
            dve(lambda v: v.memset(gw[:], IALPHA), [], [s_gw])
            s_flag = dslot("flag")
            dve(lambda v: v.memset(flagmax[:], 0.0), [], [s_flag])
            dve(lambda v: v.tensor_tensor(out=stri[:], in0=tri, in1=idf, op=ALU.subtract), [s_c], [s_idb])
            s_pc = [dslot("pc%d" % i) for i in range(9)]
            if PRECAST:
                hist = []
                for e in range(NE + 1):
                    for (dst_, src_) in ((wbg, w_eg), (wbu, w_eu), (wbd, w_ed)):
                        if len(hist) >= 6:
                            psem, pval = hist[-6]
                            nc.gpsimd.wait_ge(psem, pval)
                        ev = S.dma("pool", dst_[e], src_[e], writes=[s_pc[e // 8]], dslot=s_pc[e // 8])
                        hist.append(ev)

            def load_ln(idx):
                S.dma("sp", gB[:], lng[idx].partition_broadcast(128), writes=[s_gB], dslot=s_gB)
                S.dma("sp", bB[:], lng[idx + 1].partition_broadcast(128), writes=[s_bB], dslot=s_bB)

            def layer_norm(t, eps, xb, s_xb, use_pool=False, sc0=0, s_st=None):
                s_q = s_sm if s_st is None else s_st
                rt = R[:, t, :]
                c0, c1, c2, c3 = sc0, sc0 + 1, sc0 + 2, sc0 + 3
                for i in range(4):
                    dve(lambda v: v.bn_stats(out=st6[:, i, :], in_=rt[:, i * 512:(i + 1) * 512]), [s_R[t]], [s_st6])
                dve(lambda v: v.bn_aggr(out=sm[:, c0:c0 + 2], in_=st6[:].rearrange("p a b -> p (a b)")), [s_st6], [s_q])
                act(lambda a: a.activation(out=sm[:, c2:c2 + 1], in_=sm[:, c1:c1 + 1], func=AF.Sqrt, bias=float(eps), scale=1.0), [s_q], [s_q])
                dve(lambda v: v.reciprocal(out=sm[:, c2:c2 + 1], in_=sm[:, c2:c2 + 1]), [s_q], [s_q])
                if use_pool:
                    dve(lambda v: v.tensor_scalar(out=sm[:, c3:c3 + 1], in0=sm[:, c0:c0 + 1], scalar1=sm[:, c2:c2 + 1], scalar2=-1.0,
                                                  op0=ALU.mult, op1=ALU.mult), [s_q], [s_q])
                    act(lambda a: a.activation(out=rt, in_=rt, func=AF.Identity, bias=sm[:, c3:c3 + 1], scale=sm[:, c2:c2 + 1]),
                        [s_R[t], s_q], [s_R[t]])
                    pool(lambda g: g.tensor_tensor(out=rt, in0=rt, in1=gB[:], op=ALU.mult), [s_R[t], s_gB], [s_R[t]])
                    pool(lambda g: g.tensor_tensor(out=xb, in0=rt, in1=bB[:], op=ALU.add), [s_R[t], s_bB], [s_xb])
                    return
                dve(lambda v: v.scalar_tensor_tensor(out=rt, in0=rt, scalar=sm[:, c0:c0 + 1], in1=gB[:], op0=ALU.subtract, op1=ALU.mult),
                    [s_R[t], s_q, s_gB], [s_R[t]])
                dve(lambda v: v.scalar_tensor_tensor(out=rt, in0=rt, scalar=sm[:, c2:c2 + 1], in1=bB[:], op0=ALU.mult, op1=ALU.add),
                    [s_R[t], s_q, s_bB], [s_R[t]])
                act(lambda a: a.copy(out=xb, in_=rt), [s_R[t]], [s_xb])

            def transpose_to(xb, s_xb, dst_fn, s_dst, nchunks=NCH, pb=0):
                pbv = P[pb][:].bitcast(BF16)
                for g0 in range(0, nchunks, 8):
                    n = min(8, nchunks - g0)
                    for j in range(n):
                        c = g0 + j
                        pe(lambda t: t.transpose(out=pbv[:, j * 128:(j + 1) * 128], in_=xb[:, c * 128:(c + 1) * 128], identity=idb[:]),
                           [s_xb, s_idb], [PS[pb]], inc=(j == n - 1))
                    act(lambda a: a.copy(out=dst_fn(g0, n), in_=pbv[:, 0:n * 128].rearrange("p (c k) -> p c k", k=128)),
                        [PS[pb]], [s_dst])

            ring = {"slots": [], "aps": [], "i": 0}
            RING_E = 4096

            def wload(src, shape_str, q="pool", xr=()):
                i = ring["i"] % len(ring["slots"]); ring["i"] += 1
                sl = ring["slots"][i]
                n = 1
                for d in src.shape[1:]:
                    n *= d
                dst = ring["aps"][i][0:src.shape[0], 0:n]
                if len(src.shape) == 3:
                    dst = dst.rearrange("p (a b) -> p a b", b=src.shape[2])
                S.dma(q, dst, src, reads=list(xr), writes=[sl], dslot=sl)
                return dst, sl

            def wcols(wd, c0, n):
                return wd.rearrange("(c p) n -> p c n", p=128)[:, :, c0:c0 + n]

            def mlstm_gates(gate_ps, s_gate_ps, pg, vcol, co=0, s_gs=None):
                sq = s_sm if s_gs is None else s_gs
                k = lambda a, b: sm[:, co + a:co + b]
                if gate_ps is not None:
                    dve(lambda v: v.tensor_tensor(out=k(8, 16), in0=gate_ps, in1=bif[:], op=ALU.add), [s_gate_ps, s_bif], [sq])
                act(lambda a: a.activation(out=k(8, 16), in_=k(8, 16), func=AF.Tanh, scale=1.0 / 15.0), [sq], [sq])
                act(lambda a: a.activation(out=k(16, 20), in_=k(12, 16), func=AF.Exp, scale=-15.0), [sq], [sq])
                act(lambda a: a.activation(out=k(16, 20), in_=k(16, 20), func=AF.Ln, bias=1.0, scale=1.0), [sq], [sq])
                pe(lambda t: t.matmul(P[pg][:, 0:4], lhsT=tri, rhs=k(16, 20), start=True, stop=True), [s_c, sq], [PS[pg]], inc=False)
                pe(lambda t: t.matmul(P[pg][:, 4:8], lhsT=onesf, rhs=k(16, 20), start=True, stop=True), [s_c, sq], [PS[pg]])
                dve(lambda v: v.tensor_copy(out=k(20, 24), in_=P[pg][:, 0:4]), [PS[pg]], [sq])
                dve(lambda v: v.scalar_tensor_tensor(out=k(24, 28), in0=k(8, 12), scalar=15.0, in1=k(20, 24),
                                                     op0=ALU.mult, op1=ALU.add), [sq], [sq])
                dve(lambda v: v.tensor_tensor(out=k(28, 32), in0=k(24, 28), in1=P[pg][:, 4:8], op=ALU.subtract), [sq, PS[pg]], [sq])
                act(lambda a: a.activation(out=k(28, 32), in_=k(28, 32), func=AF.Exp), [sq], [sq])
                if vcol is not None:
                    dve(lambda v: v.tensor_scalar(out=k(28, 32), in0=k(28, 32), scalar1=vm[:, vcol:vcol + 1], scalar2=None,
                                                  op0=ALU.mult), [sq, s_vm], [sq])
                act(lambda a: a.activation(out=k(32, 36), in_=P[pg][:, 4:8], func=AF.Exp, scale=-1.0), [PS[pg]], [sq])

            def mlstm_update(ktok_ps, s_ktok, kw, s_kw, vaug, s_vaug, pu0, pu1, pn):
                for h in range(4):
                    act(lambda a: a.activation(out=kw[:, h, :], in_=ktok_ps[:, h, :], func=AF.Copy, scale=sm[:, 28 + h:29 + h]),
                        [s_ktok, s_sm], [s_kw])
                for h in range(4):
                    pb = pu0 if h < 2 else pu1
                    pe(lambda t: t.matmul(P[pb][:, (h % 2) * 256:(h % 2 + 1) * 256], lhsT=kw[:, h, :], rhs=vaug[:, h, 0:256],
                                          start=True, stop=True), [s_kw, s_vaug], [PS[pb]], inc=(h % 2 == 1))
                for h in range(4):
                    pe(lambda t: t.matmul(P[pn][:, 16 + h:17 + h], lhsT=kw[:, h, :], rhs=vaug[:, h, 256:257], start=True, stop=True),
                       [s_kw, s_vaug], [PS[pn]], inc=(h == 3))
                for h in range(4):
                    pb = pu0 if h < 2 else pu1
                    dve(lambda v: v.scalar_tensor_tensor(out=C32[:, h, :], in0=C32[:, h, :], scalar=sm[:, 32 + h:33 + h],
                                                         in1=P[pb][:, (h % 2) * 256:(h % 2 + 1) * 256], op0=ALU.mult, op1=ALU.add),
                        [s_C, s_sm, PS[pb]], [s_C])
                dve(lambda v: v.tensor_tensor(out=n32[:], in0=n32[:], in1=sm[:, 32:36], op=ALU.mult), [s_C, s_sm], [s_C])
                dve(lambda v: v.tensor_tensor(out=n32[:], in0=n32[:], in1=P[pn][:, 16:20], op=ALU.add), [s_C, PS[pn]], [s_C])
                act(lambda a: a.copy(out=Cb[:, :, 0:256], in_=C32[:]), [s_C], [s_Cb])
                act(lambda a: a.copy(out=Cb[:, :, 256:257], in_=n32[:].rearrange("p (h o) -> p h o", o=1)), [s_C], [s_Cb])

            PW = 512 + 1024 + 8 + 128 + 128
            o = 0
            wpre = ws(o, NCH * PW).rearrange("p (c n) -> p c n", n=PW); o += NCH * PW
            p_xb = []; p_hT = []; p_va = []; p_kt = []
            for b in range(2):
                p_xb.append(ws(o, D)); o += D
                p_hT.append(ws(o, NCH * 128).rearrange("p (c k) -> p c k", k=128)); o += NCH * 128
                p_va.append(ws(o, 4 * 258).rearrange("p (h k) -> p h k", k=258)); o += 4 * 258
                p_kt.append(ws(o, 512).rearrange("p (h k) -> p h k", k=128)); o += 512
            p_kw = ws(o, 512).rearrange("p (h k) -> p h k", k=128); o += 512
            HALO = WSN - 256
            assert o <= HALO
            kT_halo = ws(HALO, 128); V_halo = ws(HALO + 128, 128)
            s_wpre = dslot("wpre"); s_pkw = Slot(); s_halo = Slot()
            s_pxb = [Slot(), Slot()]; s_phT = [Slot(), Slot()]; s_pva = [Slot(), Slot()]; s_pkt = [Slot(), Slot()]
            s_g = [Slot(), Slot()]
            GCO = [0, 52]

            def pA(pt):
                b = pt % 2; t = pt % 4
                S.dma("sp", R[:, t, :], xin[pt * 128:(pt + 1) * 128, :], writes=[s_R[t]], dslot=s_xl[t])
                layer_norm(t, 1e-5, p_xb[b], s_pxb[b], use_pool=True, sc0=104 + 4 * b, s_st=s_g[b])

            def pT(pt):
                b = pt % 2
                transpose_to(p_xb[b], s_pxb[b], lambda c0, n: p_hT[b][:, c0:c0 + n, :], s_phT[b], pb=0)

            def pB(pt):
                b = pt % 2; co = GCO[b]
                for (pb, po, n) in [(1, 0, 512), (2, 512, 512), (3, 1024, 512)]:
                    for c in range(NCH):
                        pe(lambda tt: tt.matmul(P[pb][:, 0:n], lhsT=p_hT[b][:, c, :], rhs=wpre[:, c, po:po + n], start=(c == 0), stop=(c == NCH - 1)),
                           [s_phT[b], s_wpre], [PS[pb]], inc=(c == NCH - 1))
                for c in range(NCH):
                    pe(lambda tt: tt.matmul(P[6][:, 0:8], lhsT=p_hT[b][:, c, :], rhs=wpre[:, c, 1536:1544], start=(c == 0), stop=(c == NCH - 1)),
                       [s_phT[b], s_wpre], [PS[6]], inc=(c == NCH - 1))
                act(lambda a: a.activation(out=p_kt[b][:].rearrange("p h k -> p (h k)"), in_=P[1][:], func=AF.Copy, scale=128.0 ** -0.5), [PS[1]], [s_pkt[b]])
                act(lambda a: a.copy(out=p_va[b][:, 0:2, 0:256], in_=P[2][:].rearrange("p (h k) -> p h k", k=256)), [PS[2]], [s_pva[b]])
                act(lambda a: a.copy(out=p_va[b][:, 2:4, 0:256], in_=P[3][:].rearrange("p (h k) -> p h k", k=256)), [PS[3]], [s_pva[b]])
                dve(lambda v: v.tensor_tensor(out=sm[:, co + 8:co + 16], in0=P[6][:, 0:8], in1=bif[:], op=ALU.add), [PS[6], s_bif], [s_g[b]])
                if pt == NPRE - 1:
                    for c in range(NCH):
                        pe(lambda tt: tt.matmul(P[1][:, 0:128], lhsT=wpre[:, c, 1544:1672], rhs=p_hT[b][:, c, :], start=(c == 0), stop=(c == NCH - 1)),
                           [s_phT[b], s_wpre], [PS[1]], inc=(c == NCH - 1))
                    for c in range(NCH):
                        pe(lambda tt: tt.matmul(P[2][:, 0:128], lhsT=p_hT[b][:, c, :], rhs=wpre[:, c, 1672:1800], start=(c == 0), stop=(c == NCH - 1)),
                           [s_phT[b], s_wpre], [PS[2]], inc=(c == NCH - 1))
                    act(lambda a: a.copy(out=kT_halo, in_=P[1][:, 0:128]), [PS[1]], [s_halo])
                    act(lambda a: a.copy(out=V_halo, in_=P[2][:, 0:128]), [PS[2]], [s_halo])

            def pC1(pt):
                b = pt % 2
                mlstm_gates(None, None, 7, pt, co=GCO[b], s_gs=s_g[b])

            def pC2(pt):
                b = pt % 2; co = GCO[b]
                for h in range(4):
                    act(lambda a: a.activation(out=p_kw[:, h, :], in_=p_kt[b][:, h, :], func=AF.Copy, scale=sm[:, co + 28 + h:co + 29 + h]),
                        [s_pkt[b], s_g[b]], [s_pkw])
                for h in range(4):
                    pb = 4 if h < 2 else 5
                    pe(lambda tt: tt.matmul(P[pb][:, (h % 2) * 256:(h % 2 + 1) * 256], lhsT=p_kw[:, h, :], rhs=p_va[b][:, h, 0:256], start=True, stop=True),
                       [s_pkw, s_pva[b]], [PS[pb]], inc=(h % 2 == 1))
                for h in range(4):
                    pe(lambda tt: tt.matmul(P[7][:, 16 + h:17 + h], lhsT=p_kw[:, h, :], rhs=p_va[b][:, h, 256:257], start=True, stop=True),
                       [s_pkw, s_pva[b]], [PS[7]], inc=(h == 3))
                for h in range(4):
                    pb = 4 if h < 2 else 5
                    dve(lambda v: v.scalar_tensor_tensor(out=C32[:, h, :], in0=C32[:, h, :], scalar=sm[:, co + 32 + h:co + 33 + h],
                                                         in1=P[pb][:, (h % 2) * 256:(h % 2 + 1) * 256], op0=ALU.mult, op1=ALU.add),
                        [s_C, s_g[b], PS[pb]], [s_C])
                dve(lambda v: v.tensor_tensor(out=n32[:], in0=n32[:], in1=sm[:, co + 32:co + 36], op=ALU.mult), [s_C, s_g[b]], [s_C])
                dve(lambda v: v.tensor_tensor(out=n32[:], in0=n32[:], in1=P[7][:, 16:20], op=ALU.add), [s_C, PS[7]], [s_C])

            if NPRE > 0:
                segs = [(C_MK, 512), (C_MV, 1024), (C_MI, 8), (C_AK, 128), (C_AV, 128)]
                po = 0
                for (c0, n) in segs:
                    S.dma("pool", wpre[:, :, po:po + n], wcols(w_in, c0, n), writes=[s_wpre], dslot=s_wpre)
                    po += n
                for b in range(2):
                    dve(lambda v: v.memset(p_va[b][:], 1.0), [], [s_pva[b]])
                load_ln(0)
                pA(0); pT(0)
                for pt in range(NPRE):
                    if pt + 1 < NPRE:
                        pA(pt + 1)
                    pB(pt)
                    if pt >= 1:
                        pC2(pt - 1)
                    if pt + 1 < NPRE:
                        pT(pt + 1)
                    pC1(pt)
                pC2(NPRE - 1)
                act(lambda a: a.copy(out=Cb[:, :, 0:256], in_=C32[:]), [s_C], [s_Cb])
                act(lambda a: a.copy(out=Cb[:, :, 256:257], in_=n32[:].rearrange("p (h o) -> p h o", o=1)), [s_C], [s_Cb])
            if NPRE == 0:
                dve(lambda v: v.memset(kT_halo, 0.0), [], [s_halo])
                dve(lambda v: v.memset(V_halo, 0.0), [], [s_halo])

            SB = 512
            RING_E = 4096
            NRM, NRE = 5, 12
            Sched.alias([s_sm] + s_g, [s_sm])
            ring_sl = [dslot("rg%d" % i) for i in range(NRE)]
            s_pf = dslot("pf")
            rot = {"i": 0}

            def nbank(banks=(1, 2, 6, 7)):
                rot["i"] += 1
                return banks[rot["i"] % len(banks)]

            def ring_setup(nslots):
                ring["slots"] = ring_sl[:nslots]
                ring["aps"] = [ws(i * RING_E, RING_E) for i in range(nslots)]
                ring["i"] = 0

            w3 = w_in.rearrange("(c p) n -> p c n", p=128)
            wba4 = w_ba.rearrange("(g j d) n -> g d j n", g=2, d=64)
            wbm3 = w_bm.rearrange("(c p) n -> p c n", p=128)
            moe_slots = None
            for blk in range(NBLK):
                S.new_epoch(engsems())
                o = NRM * RING_E

                def take(n, dt=BF16):
                    nonlocal o
                    v = ws(o, n, dt); o += n * (2 if dt == F32 else 1); return v
                o_mg = o
                m_qT = take(8 * SB).rearrange("p (j k) -> p j k", k=SB)
                m_mqT = take(4 * SB).rearrange("p (h k) -> p h k", k=SB)
                m_mkT = take(4 * SB).rearrange("p (h k) -> p h k", k=SB)
                m_mgT = ws(o_mg, 16 * SB).rearrange("p (c k) -> p c k", k=SB)
                m_kT = take(5 * 128).rearrange("p (t k) -> p t k", k=128)
                m_V = take(5 * 128).rearrange("p (t k) -> p t k", k=128)
                m_va = take(4 * 4 * 258).rearrange("p (t h k) -> p t h k", h=4, k=258)
                m_gs = take(4 * 1024).rearrange("p (t k) -> p t k", k=1024)
                m_attnT = take(8 * SB).rearrange("p (h k) -> p h k", k=SB)
                m_mhT = take(8 * SB).rearrange("p (c k) -> p c k", k=SB)
                m_xb = take(D)
                m_sc = take(512, F32)
                m_PT = take(2 * 512).rearrange("p (t k) -> p t k", k=512)
                m_dn = take(512, F32)
                m_eb = take(512).rearrange("p (h k) -> p h k", k=128)
                m_Sq = take(512).rearrange("p (h k) -> p h k", k=128)
                m_qd = take(512).rearrange("p (h k) -> p h k", k=128)
                m_kw = take(512).rearrange("p (h k) -> p h k", k=128)
                m_mh = take(1024)
                m_Dt = m_dn.rearrange("p (h k) -> p h k", k=128)
                m_rE = m_sc.rearrange("p (h k) -> p h k", k=128)
                m_t1 = m_sc
                m_t2 = m_PT.rearrange("p t k -> p (t k)").bitcast(F32)
                assert o <= HALO, o
                names = "xb V va gs qT kT mqT mkT attnT mhT mgT sc PT eb Sq qd kw mh dn".split()
                sl = {n: Slot(n) for n in names}
                sl["Dt"] = sl["dn"]; sl["rE"] = sl["sc"]; sl["t1"] = sl["sc"]; sl["t2"] = sl["PT"]
                ring_setup(NRM)
                mix_slots = [v for k, v in sl.items() if k != "mgT"] + ring["slots"]
                if blk == 0:
                    Sched.alias([s_wpre, s_pkw] + s_pxb + s_phT + s_pva + s_pkt, mix_slots)
                else:
                    Sched.alias(moe_slots, mix_slots)
                dve(lambda v: v.memset(m_va[:], 1.0), [], [sl["va"]])
                tiles = [0, 1, 2, 3]
                gt0 = NPRE + blk * 4
                load_ln(0)
                for t in tiles:
                    S.dma("sp", R[:, t, :], xin[(gt0 + t) * 128:(gt0 + t + 1) * 128, :], writes=[s_R[t]], dslot=s_xl[t])
                for t in tiles:
                    layer_norm(t, 1e-5, m_xb, sl["xb"])
                    transpose_to(m_xb, sl["xb"], lambda c0, n: xT[:, c0:c0 + n, t * 128:(t + 1) * 128], s_xT, pb=0)
                act(lambda a: a.copy(out=m_kT[:, 0, :], in_=kT_halo), [s_halo], [sl["kT"]])
                act(lambda a: a.copy(out=m_V[:, 0, :], in_=V_halo), [s_halo], [sl["V"]])

                def fmm(wt2, wsl, pb):
                    for c in range(NCH):
                        pe(lambda tt: tt.matmul(P[pb][:], lhsT=wt2[:, c, :], rhs=xT[:, c, :], start=(c == 0), stop=(c == NCH - 1)),
                           [wsl, s_xT], [PS[pb]], inc=(c == NCH - 1))
                for jp in range(4):
                    i = ring["i"] % NRM; ring["i"] += 1
                    wsl = ring["slots"][i]
                    wt = ring["aps"][i].rearrange("p (m c g r) -> p m c g r", m=2, g=2, r=64)
                    for mm in range(2):
                        j = jp * 2 + mm
                        src = w3[:, :, j * 64:j * 64 + 1024].rearrange("p c (g r) -> p c g r", r=512)
                        for gg in range(2):
                            S.dma("pool", wt[:, mm, :, gg, :], src[:, :, gg, 0:64], writes=[wsl], dslot=wsl)
                    wt2 = ring["aps"][i].rearrange("p (m c k) -> p m c k", m=2, k=128)
                    for mm in range(2):
                        j = jp * 2 + mm
                        pb = nbank()
                        fmm(wt2[:, mm], wsl, pb)
                        act(lambda a: a.copy(out=m_qT[:, j, :], in_=P[pb][:]), [PS[pb]], [sl["qT"]])
                wt, wsl = wload(wcols(w_in, C_AK, 128), "")
                pb = nbank(); fmm(wt, wsl, pb)
                act(lambda a: a.copy(out=m_kT[:, 1:5, :], in_=P[pb][:].rearrange("p (t k) -> p t k", k=128)), [PS[pb]], [sl["kT"]])
                for hp in range(2):
                    wt, wsl = wload(wcols(w_in, C_MQ + hp * 256, 256), "")
                    for mm in range(2):
                        h = hp * 2 + mm
                        pb = nbank(); fmm(wt[:, :, mm * 128:(mm + 1) * 128], wsl, pb)
                        act(lambda a: a.copy(out=m_mqT[:, h, :], in_=P[pb][:]), [PS[pb]], [sl["mqT"]])
                for hp in range(2):
                    wt, wsl = wload(wcols(w_in, C_MK + hp * 256, 256), "")
                    for mm in range(2):
                        h = hp * 2 + mm
                        pb = nbank(); fmm(wt[:, :, mm * 128:(mm + 1) * 128], wsl, pb)
                        act(lambda a: a.activation(out=m_mkT[:, h, :], in_=P[pb][:], func=AF.Copy, scale=128.0 ** -0.5), [PS[pb]], [sl["mkT"]])
                gate_sb = sm[:, 112:144].rearrange("p (t k) -> p t k", k=8)

                def tproj(c0, n, evac):
                    wt, wsl = wload(wcols(w_in, c0, n), "")
                    for t in tiles:
                        pb = nbank()
                        for c in range(NCH):
                            pe(lambda tt: tt.matmul(P[pb][:, 0:n], lhsT=xT[:, c, t * 128:(t + 1) * 128], rhs=wt[:, c, :], start=(c == 0), stop=(c == NCH - 1)),
                               [wsl, s_xT], [PS[pb]], inc=(c == NCH - 1))
                        evac(t, pb)
                tproj(C_AV, 128, lambda t, pb: act(lambda a: a.copy(out=m_V[:, 1 + t, :], in_=P[pb][:, 0:128]), [PS[pb]], [sl["V"]]))
                for h in range(4):
                    tproj(C_MV + h * 256, 256, lambda t, pb: act(lambda a: a.copy(out=m_va[:, t, h, 0:256], in_=P[pb][:, 0:256]), [PS[pb]], [sl["va"]]))
                for q in range(4):
                    def ev_mo(t, pb):
                        act(lambda a: a.activation(out=m_gs[:, t, q * 256:(q + 1) * 256], in_=P[pb][:, 0:256], func=AF.Sigmoid), [PS[pb]], [sl["gs"]])
                        dve(lambda v: v.tensor_tensor(out=m_gs[:, t, q * 256:(q + 1) * 256], in0=m_gs[:, t, q * 256:(q + 1) * 256],
                                                      in1=ngB[:, q * 256:(q + 1) * 256], op=ALU.mult), [sl["gs"], s_ng], [sl["gs"]])
                    tproj(C_MO + q * 256, 256, ev_mo)
                tproj(C_MI, 8, lambda t, pb: dve(lambda v: v.tensor_copy(out=gate_sb[:, t, :], in_=P[pb][:, 0:8]), [PS[pb]], [s_sm]))

                for t in tiles:
                    qc = slice(t * 128, (t + 1) * 128)
                    first_tile = (blk == 0 and t == 0)
                    for g in range(2):
                        pr = slice(g * 64, (g + 1) * 64)
                        for half in range(2):
                            hh = g * 8 + half * 4
                            for kt in range(2):
                                pb = nbank((1, 2))
                                pe(lambda tt: tt.matmul(P[pb][:], lhsT=m_kT[pr, t + kt, :], rhs=m_qT[pr, half * 4:half * 4 + 4, qc],
                                                        start=True, stop=True), [sl["kT"], sl["qT"]], [PS[pb]])
                                dve(lambda v: v.scalar_tensor_tensor(out=m_sc, in0=P[pb][:], scalar=0.125,
                                                                     in1=abias[:, kt, hh:hh + 4, :].rearrange("p h k -> p (h k)"),
                                                                     op0=ALU.mult, op1=ALU.add), [PS[pb], s_ab], [sl["sc"]])
                                if kt == 0 and first_tile:
                                    act(lambda a: a.activation(out=m_PT[:, kt, :], in_=m_sc, func=AF.Exp, bias=kvb[:, 0:1], scale=1.0),
                                        [sl["sc"], s_kvb], [sl["PT"]])
                                else:
                                    act(lambda a: a.activation(out=m_PT[:, kt, :], in_=m_sc, func=AF.Exp), [sl["sc"]], [sl["PT"]])
                            for kt in range(2):
                                pe(lambda tt: tt.matmul(P[4][:], lhsT=m_V[:, t + kt, :], rhs=m_PT[:, kt, :], start=(kt == 0), stop=(kt == 1)),
                                   [sl["V"], sl["PT"]], [PS[4]], inc=(kt == 1))
                            for kt in range(2):
                                pe(lambda tt: tt.matmul(P[5][:], lhsT=onesb[:], rhs=m_PT[:, kt, :], start=(kt == 0), stop=(kt == 1)),
                                   [s_idb, sl["PT"]], [PS[5]], inc=(kt == 1))
                            for hq in range(4):
                                dve(lambda v: v.tensor_scalar(out=m_dn[pr, hq * 128:(hq + 1) * 128], in0=P[5][pr, hq * 128:(hq + 1) * 128],
                                                              scalar1=esink[pr, hh + hq:hh + hq + 1], scalar2=None, op0=ALU.add),
                                    [PS[5], s_es], [sl["dn"]])
                            dve(lambda v: v.reciprocal(out=m_dn[pr, :], in_=m_dn[pr, :]), [sl["dn"]], [sl["dn"]])
                            dve(lambda v: v.tensor_tensor(out=m_attnT[pr, half * 4:half * 4 + 4, qc], in0=P[4][pr, :].rearrange("p (h k) -> p h k", k=128),
                                                          in1=m_dn[pr, :].rearrange("p (h k) -> p h k", k=128), op=ALU.mult),
                                [PS[4], sl["dn"]], [sl["attnT"]])

                for t in tiles:
                    qc = slice(t * 128, (t + 1) * 128)
                    mlstm_gates(gate_sb[:, t, :], s_sm, 7, None)
                    for h in range(4):
                        dve(lambda v: v.tensor_scalar(out=m_rE[:, h, :], in0=idf, scalar1=sm[:, 20 + h:21 + h], scalar2=None, op0=ALU.mult),
                            [s_c, s_sm], [sl["rE"]])
                    pe(lambda tt: tt.matmul(P[3][:], lhsT=onesf, rhs=m_rE[:].rearrange("p h k -> p (h k)"), start=True, stop=True),
                       [s_c, sl["rE"]], [PS[3]])
                    act(lambda a: a.activation(out=m_eb[:].rearrange("p h k -> p (h k)"), in_=P[3][:], func=AF.Exp, scale=-1.0), [PS[3]], [sl["eb"]])
                    for h in range(4):
                        act(lambda a: a.activation(out=m_Dt[:, h, :], in_=P[3][:, h * 128:(h + 1) * 128], func=AF.Exp,
                                                   bias=sm[:, 24 + h:25 + h], scale=-1.0), [PS[3], s_sm], [sl["Dt"]])
                    for h in range(4):
                        dve(lambda v: v.tensor_tensor(out=m_Dt[:, h, :], in0=m_Dt[:, h, :], in1=tri, op=ALU.mult), [sl["Dt"], s_c], [sl["Dt"]])
                    for h in range(4):
                        pe(lambda tt: tt.matmul(P[1][:, h * 128:(h + 1) * 128], lhsT=m_mkT[:, h, qc], rhs=m_mqT[:, h, qc], start=True, stop=True),
                           [sl["mkT"], sl["mqT"]], [PS[1]], inc=(h == 3))
                    dve(lambda v: v.tensor_tensor(out=m_Sq[:].rearrange("p h k -> p (h k)"), in0=P[1][:],
                                                  in1=m_Dt[:].rearrange("p h k -> p (h k)"), op=ALU.mult), [PS[1], sl["Dt"]], [sl["Sq"]])
                    dve(lambda v: v.tensor_tensor(out=m_qd[:], in0=m_mqT[:, :, qc], in1=m_eb[:], op=ALU.mult), [sl["mqT"], sl["eb"]], [sl["qd"]])
                    for h in range(4):
                        pb = 4 if h < 2 else 5
                        osl = P[pb][:, (h % 2) * 256:(h % 2 + 1) * 256]
                        pe(lambda tt: tt.matmul(osl, lhsT=m_Sq[:, h, :], rhs=m_va[:, t, h, 0:256], start=True, stop=False),
                           [sl["Sq"], sl["va"]], [PS[pb]], inc=False)
                        pe(lambda tt: tt.matmul(osl, lhsT=m_qd[:, h, :], rhs=Cb[:, h, 0:256], start=False, stop=True),
                           [sl["qd"], s_Cb], [PS[pb]], inc=(h % 2 == 1))
                    for h in range(4):
                        pe(lambda tt: tt.matmul(P[7][:, 32 + h:33 + h], lhsT=m_Sq[:, h, :], rhs=m_va[:, t, h, 256:257], start=True, stop=False),
                           [sl["Sq"], sl["va"]], [PS[7]], inc=False)
                        pe(lambda tt: tt.matmul(P[7][:, 32 + h:33 + h], lhsT=m_qd[:, h, :], rhs=Cb[:, h, 256:257], start=False, stop=True),
                           [sl["qd"], s_Cb], [PS[7]], inc=(h == 3))
                    act(lambda a: a.activation(out=sm[:, 36:40], in_=P[7][:, 32:36], func=AF.Abs), [PS[7]], [s_sm])
                    dve(lambda v: v.tensor_scalar(out=sm[:, 36:40], in0=sm[:, 36:40], scalar1=1.0, scalar2=None, op0=ALU.max), [s_sm], [s_sm])
                    dve(lambda v: v.reciprocal(out=sm[:, 36:40], in_=sm[:, 36:40]), [s_sm], [s_sm])
                    for h in range(4):
                        pb = 4 if h < 2 else 5
                        act(lambda a: a.activation(out=junk[:], in_=P[pb][:, (h % 2) * 256:(h % 2 + 1) * 256], func=AF.Square,
                                                   accum_out=sm[:, 40 + h:41 + h]), [PS[pb]], [s_junk, s_sm])
                    dve(lambda v: v.tensor_tensor(out=sm[:, 48:52], in0=sm[:, 40:44], in1=sm[:, 36:40], op=ALU.mult), [s_sm], [s_sm])
                    dve(lambda v: v.tensor_tensor(out=sm[:, 48:52], in0=sm[:, 48:52], in1=sm[:, 36:40], op=ALU.mult), [s_sm], [s_sm])
                    act(lambda a: a.activation(out=sm[:, 48:52], in_=sm[:, 48:52], func=AF.Sqrt, bias=1e-6, scale=1.0 / 256.0), [s_sm], [s_sm])
                    dve(lambda v: v.reciprocal(out=sm[:, 48:52], in_=sm[:, 48:52]), [s_sm], [s_sm])
                    dve(lambda v: v.tensor_tensor(out=sm[:, 44:48], in0=sm[:, 48:52], in1=sm[:, 36:40], op=ALU.mult), [s_sm], [s_sm])
                    for h in range(4):
                        pb = 4 if h < 2 else 5
                        dve(lambda v: v.scalar_tensor_tensor(out=m_mh[:, h * 256:(h + 1) * 256], in0=P[pb][:, (h % 2) * 256:(h % 2 + 1) * 256],
                                                             scalar=sm[:, 44 + h:45 + h], in1=m_gs[:, t, h * 256:(h + 1) * 256],
                                                             op0=ALU.mult, op1=ALU.mult), [PS[pb], s_sm, sl["gs"]], [sl["mh"]])
                    transpose_to(m_mh, sl["mh"], lambda c0, n: m_mhT[:, c0:c0 + n, qc], sl["mhT"], nchunks=8, pb=0)
                    pbv = P[0][:].bitcast(BF16)
                    for h in range(4):
                        pe(lambda tt: tt.transpose(out=pbv[:, h * 128:(h + 1) * 128], in_=m_mkT[:, h, qc], identity=idb[:]),
                           [sl["mkT"], s_idb], [PS[0]], inc=(h == 3))
                    mlstm_update(pbv[:, 0:512].rearrange("p (h k) -> p h k", k=128), PS[0], m_kw, sl["kw"], m_va[:, t], sl["va"], 4, 5, 7)
                act(lambda a: a.copy(out=kT_halo, in_=m_kT[:, 4, :]), [sl["kT"]], [s_halo])
                act(lambda a: a.copy(out=V_halo, in_=m_V[:, 4, :]), [sl["V"]], [s_halo])

                Sched.alias([sl["qT"], sl["mqT"], sl["mkT"]], [sl["mgT"]])
                for mp in range(8):
                    wga, s1 = wload(wcols(w_in, C_GA + mp * 256, 256), "")
                    wgb, s2 = wload(wcols(w_in, C_GB + mp * 256, 256), "")
                    i = ring["i"] % NRM; ring["i"] += 1
                    s3 = ring["slots"][i]
                    wa = ring["aps"][i][:, 0:2048].rearrange("p (j n) -> p j n", n=256)
                    for gg in range(2):
                        S.dma("pool", wa[gg * 64:(gg + 1) * 64], wba4[gg][:, :, mp * 256:(mp + 1) * 256], writes=[s3], dslot=s3)
                    wb, s4 = wload(wbm3[:, :, mp * 256:(mp + 1) * 256], "")
                    for mm in range(2):
                        m = mp * 2 + mm
                        ms = slice(mm * 128, (mm + 1) * 128)
                        b1, b2, b3, b4 = (1, 2, 3, 6) if m % 2 == 0 else (4, 5, 7, 0)
                        for c in range(NCH):
                            pe(lambda tt: tt.matmul(P[b1][:], lhsT=wga[:, c, ms], rhs=xT[:, c, :], start=(c == 0), stop=(c == NCH - 1)),
                               [s1, s_xT], [PS[b1]], inc=(c == NCH - 1))
                        for c in range(NCH):
                            pe(lambda tt: tt.matmul(P[b2][:], lhsT=wgb[:, c, ms], rhs=xT[:, c, :], start=(c == 0), stop=(c == NCH - 1)),
                               [s2, s_xT], [PS[b2]], inc=(c == NCH - 1))
                        for c in range(8):
                            pe(lambda tt: tt.matmul(P[b3][:], lhsT=wa[:, c, ms], rhs=m_attnT[:, c, :], start=(c == 0), stop=(c == 7)),
                               [s3, sl["attnT"]], [PS[b3]], inc=(c == 7))
                        for c in range(8):
                            pe(lambda tt: tt.matmul(P[b4][:], lhsT=wb[:, c, ms], rhs=m_mhT[:, c, :], start=(c == 0), stop=(c == 7)),
                               [s4, sl["mhT"]], [PS[b4]], inc=(c == 7))
                        act(lambda a: a.activation(out=m_t1, in_=P[b1][:], func=AF.Sigmoid), [PS[b1]], [sl["t1"]])
                        act(lambda a: a.activation(out=m_t2, in_=P[b2][:], func=AF.Sigmoid), [PS[b2]], [sl["t2"]])
                        dve(lambda v: v.tensor_tensor(out=m_t1, in0=m_t1, in1=P[b3][:], op=ALU.mult), [sl["t1"], PS[b3]], [sl["t1"]])
                        dve(lambda v: v.tensor_tensor(out=m_t2, in0=m_t2, in1=P[b4][:], op=ALU.mult), [sl["t2"], PS[b4]], [sl["t2"]])
                        dve(lambda v: v.tensor_tensor(out=m_mgT[:, m, :], in0=m_t1, in1=m_t2, op=ALU.add), [sl["t1"], sl["t2"]], [sl["mgT"]])
                for nb in range(8):
                    wt, wsl = wload(wcols(w_out, nb * 256, 256), "")
                    for t in tiles:
                        pb = nbank()
                        for c in range(NCH):
                            pe(lambda tt: tt.matmul(P[pb][:, 0:256], lhsT=m_mgT[:, c, t * 128:(t + 1) * 128], rhs=wt[:, c, :], start=(c == 0), stop=(c == NCH - 1)),
                               [wsl, sl["mgT"]], [PS[pb]], inc=(c == NCH - 1))
                        dve(lambda v: v.scalar_tensor_tensor(out=R[:, t, nb * 256:(nb + 1) * 256], in0=P[pb][:, 0:256], scalar=IALPHA,
                                                             in1=R[:, t, nb * 256:(nb + 1) * 256], op0=ALU.mult, op1=ALU.add),
                            [PS[pb], s_R[t]], [s_R[t]])
                load_ln(2)
                if SPARSE:
                    X1tok = ws(38176, 4 * D).rearrange("p (t k) -> p t k", k=D)
                    s_x1t = Slot("x1t")
                    Sched.alias([sl["attnT"], sl["mhT"]], [s_x1t])
                for t in tiles:
                    if SPARSE:
                        layer_norm(t, EPS_A, X1tok[:, t, :], s_x1t)
                        transpose_to(X1tok[:, t, :], s_x1t, lambda c0, n: xT[:, c0:c0 + n, t * 128:(t + 1) * 128], s_xT, pb=0)
                    else:
                        layer_norm(t, EPS_A, m_xb, sl["xb"])
                        transpose_to(m_xb, sl["xb"], lambda c0, n: xT[:, c0:c0 + n, t * 128:(t + 1) * 128], s_xT, pb=0)

                if SPARSE:
                    o = 46368
                    x_mskf = take(256, F32).rearrange("p (t k) -> p t k", k=64)
                    x_rank = take(256, F32).rearrange("p (t k) -> p t k", k=64)
                    x_mskb = take(256).rearrange("p (t k) -> p t k", k=64)
                    s_msk = Slot("msk")
                    Sched.alias([sl["xb"]], [s_msk])
                for t in range(4):
                    pb = nbank()
                    for c in range(NCH):
                        pe(lambda tt: tt.matmul(P[pb][:, 0:NE], lhsT=xT[:, c, t * 128:(t + 1) * 128], rhs=wrt[:, c, :], start=(c == 0), stop=(c == NCH - 1)),
                           [s_xT, s_wrt], [PS[pb]], inc=(c == NCH - 1))
                    sc_ = m_sc[:, 0:64]; sel_ = m_sc[:, 64:128]; top_ = m_sc[:, 128:136]; msk_ = m_sc[:, 192:256]
                    act(lambda a: a.activation(out=sc_, in_=P[pb][:, 0:NE], func=AF.Sigmoid), [PS[pb]], [sl["sc"]])
                    dve(lambda v: v.tensor_tensor(out=sel_, in0=sc_, in1=brB[:], op=ALU.add), [sl["sc"], s_br], [sl["sc"]])
                    dve(lambda v: v.max(out=top_, in_=sel_), [sl["sc"]], [sl["sc"]])
                    dve(lambda v: v.tensor_scalar(out=msk_, in0=sel_, scalar1=top_[:, 7:8], scalar2=None, op0=ALU.is_ge), [sl["sc"]], [sl["sc"]])
                    if SPARSE:
                        dve(lambda v: v.tensor_copy(out=x_mskf[:, t, :], in_=msk_), [sl["sc"]], [s_msk])
                        dve(lambda v: v.tensor_copy(out=x_mskb[:, t, :], in_=msk_), [sl["sc"]], [s_msk])
                    dve(lambda v: v.tensor_tensor(out=msk_, in0=msk_, in1=sc_, op=ALU.mult), [sl["sc"]], [sl["sc"]])
                    dve(lambda v: v.reduce_sum(out=sm[:, 150:151], in_=msk_, axis=AX.X), [sl["sc"]], [s_sm])
                    dve(lambda v: v.reciprocal(out=sm[:, 150:151], in_=sm[:, 150:151]), [s_sm], [s_sm])
                    dve(lambda v: v.tensor_scalar(out=gw[:, t, 0:NE], in0=msk_, scalar1=sm[:, 150:151], scalar2=2.5 * IALPHA,
                                                  op0=ALU.mult, op1=ALU.mult), [sl["sc"], s_sm], [s_gw])
                if SPARSE:
                    for t in range(4):
                        pb = nbank()
                        seq = [(stri[:], x_mskb[:, t, :])] + [(onesb[:], x_mskb[:, tp, :]) for tp in range(t)]
                        for i, (lt, rh) in enumerate(seq):
                            pe(lambda tt: tt.matmul(P[pb][:, 0:NE], lhsT=lt, rhs=rh, start=(i == 0), stop=(i == len(seq) - 1)),
                               [s_idb, s_msk], [PS[pb]], inc=(i == len(seq) - 1))
                        dve(lambda v: v.tensor_copy(out=x_rank[:, t, :], in_=P[pb][:, 0:NE]), [PS[pb]], [s_msk])
                    pb = nbank()
                    for t in range(4):
                        pe(lambda tt: tt.matmul(P[pb][:, 0:NE], lhsT=onesb[:], rhs=x_mskb[:, t, :], start=(t == 0), stop=(t == 3)),
                           [s_idb, s_msk], [PS[pb]], inc=(t == 3))
                    dve(lambda v: v.tensor_reduce(out=sm[:, 152:153], in_=P[pb][:, 0:NE], axis=AX.X, op=ALU.max), [PS[pb]], [s_sm])
                    dve(lambda v: v.tensor_tensor(out=flagmax[:], in0=flagmax[:], in1=sm[:, 152:153], op=ALU.max), [s_sm, s_flag], [s_flag])

                wq = "sp" if (PRECAST and not _os.environ.get("KDBG_POOLQ")) else "pool"

                def eload(e):
                    if PRECAST:
                        wg3 = wbg[e].rearrange("(c p) n -> p c n", p=128)
                        wu3 = wbu[e].rearrange("(c p) n -> p c n", p=128)
                        wd3 = wbd[e].rearrange("(c p) n -> p c n", p=128)
                        xr = [s_pc[e // 8]]
                    else:
                        wg3 = w_eg[e].rearrange("(c p) n -> p c n", p=128)
                        wu3 = w_eu[e].rearrange("(c p) n -> p c n", p=128)
                        wd3 = w_ed[e].rearrange("(c p) n -> p c n", p=128)
                        xr = []
                    wgs = [wload(wg3[:, :, hp * 256:(hp + 1) * 256], "", q=wq, xr=xr) for hp in range(2)]
                    wus = [wload(wu3[:, :, hp * 256:(hp + 1) * 256], "", q=wq, xr=xr) for hp in range(2)]
                    wds = [wload(wd3[:, hp * 2:hp * 2 + 2, :], "", q=wq, xr=xr) for hp in range(2)]
                    return wgs, wus, wds

                def dense_expert(e, e_h1, e_sg, s_h1, s_sg):
                    wgs, wus, wds = eload(e)
                    for m in range(4):
                        pg_, pu_ = (0, 1) if m % 2 == 0 else (2, 3)
                        wg_, sg_w = wgs[m // 2]; wu_, su_w = wus[m // 2]
                        ms = slice((m % 2) * 128, (m % 2 + 1) * 128)
                        for c in range(NCH):
                            pe(lambda tt: tt.matmul(P[pg_][:], lhsT=wg_[:, c, ms], rhs=xT[:, c, :], start=(c == 0), stop=(c == NCH - 1)),
                               [sg_w, s_xT], [PS[pg_]], inc=(c == NCH - 1))
                        for c in range(NCH):
                            pe(lambda tt: tt.matmul(P[pu_][:], lhsT=wu_[:, c, ms], rhs=xT[:, c, :], start=(c == 0), stop=(c == NCH - 1)),
                               [su_w, s_xT], [PS[pu_]], inc=(c == NCH - 1))
                        act(lambda a: a.activation(out=e_sg, in_=P[pg_][:], func=AF.Silu), [PS[pg_]], [s_sg])
                        dve(lambda v: v.tensor_tensor(out=e_h1[:, m, :], in0=e_sg, in1=P[pu_][:], op=ALU.mult), [s_sg, PS[pu_]], [s_h1])
                    for t in range(4):
                        for nb in range(4):
                            for m in range(4):
                                wd_, sd_w = wds[m // 2]
                                pe(lambda tt: tt.matmul(P[4 + nb][:], lhsT=e_h1[:, m, t * 128:(t + 1) * 128], rhs=wd_[:, m % 2, nb * 512:(nb + 1) * 512],
                                                        start=(m == 0), stop=(m == 3)), [s_h1, sd_w], [PS[4 + nb]], inc=(m == 3))
                            dve(lambda v: v.scalar_tensor_tensor(out=R[:, t, nb * 512:(nb + 1) * 512], in0=P[4 + nb][:], scalar=gw[:, t, e:e + 1],
                                                                 in1=R[:, t, nb * 512:(nb + 1) * 512], op0=ALU.mult, op1=ALU.add),
                                [PS[4 + nb], s_gw, s_R[t]], [s_R[t]])

                if not SPARSE:
                    o = NRE * RING_E
                    e_h1 = take(4 * 512).rearrange("p (m k) -> p m k", k=512)
                    e_sg = take(512, F32)
                    assert o <= HALO
                    s_h1 = Slot("h1"); s_sg = Slot("sg")
                    ring_setup(NRE)
                    moe_slots = [s_h1, s_sg] + ring["slots"]
                    Sched.alias(list(sl.values()) + ring_sl[:NRM], moe_slots)
                    for e in range(NE + 1):
                        dense_expert(e, e_h1, e_sg, s_h1, s_sg)
                    ple_olds = [s_h1, s_sg]
                    PLE_O = NRE * RING_E
                else:
                    NRS = 8
                    ring_setup(NRS)
                    o = NRS * RING_E
                    x_Sel = [take(512).rearrange("p (t k) -> p t k", k=128) for _ in range(2)]
                    x_SelT = take(512).rearrange("p (t k) -> p t k", k=128)
                    x_h1 = take(512); x_h1T = take(512).rearrange("p (m k) -> p m k", k=128)
                    x_yb = take(2048)
                    assert o <= 38176, o
                    o = 49440
                    oB = o
                    x_XeT = [take(2048).rearrange("p (c k) -> p c k", k=128) for _ in range(2)]
                    x_sg = take(512, F32)
                    assert o <= HALO, o
                    e_h1 = ws(oB, 2048).rearrange("p (m k) -> p m k", k=512)
                    e_sg = ws(oB + 2048, 512, F32)
                    s_h1 = Slot("h1"); s_sg = Slot("sg")
                    s_Sel = [Slot(), Slot()]; s_SelT = Slot(); s_xh1 = Slot(); s_xh1T = Slot(); s_yb = Slot()
                    s_XeT = [Slot(), Slot()]; s_xsg = Slot()
                    first = [s_h1, s_sg] + s_Sel + [s_SelT, s_xh1, s_xh1T, s_yb] + ring["slots"]
                    Sched.alias([v for k, v in sl.items() if k not in ("attnT", "mhT", "xb")] + ring_sl[:NRM], first)
                    dense_expert(NE, e_h1, e_sg, s_h1, s_sg)
                    Sched.alias([s_h1, s_sg], s_XeT + [s_xsg])
                    moe_slots = first + s_XeT + [s_xsg, s_x1t, s_msk]

                    def xSel(e):
                        b = e % 2
                        for t in range(4):
                            dve(lambda v: v.tensor_scalar(out=x_Sel[b][:, t, :], in0=iotaf, scalar1=x_rank[:, t, e:e + 1], scalar2=x_mskf[:, t, e:e + 1],
                                                          op0=ALU.is_equal, op1=ALU.mult), [s_c, s_msk], [s_Sel[b]])

                    def xGather(e):
                        b = e % 2
                        for g4 in range(4):
                            pb = g4 % 2
                            for cc in range(4):
                                c = g4 * 4 + cc
                                for t in range(4):
                                    pe(lambda tt: tt.matmul(P[pb][:, cc * 128:(cc + 1) * 128], lhsT=X1tok[:, t, c * 128:(c + 1) * 128], rhs=x_Sel[b][:, t, :],
                                                            start=(t == 0), stop=(t == 3)), [s_x1t, s_Sel[b]], [PS[pb]], inc=(cc == 3 and t == 3))
                            act(lambda a: a.copy(out=x_XeT[b][:, g4 * 4:(g4 + 1) * 4, :], in_=P[pb][:].rearrange("p (c k) -> p c k", k=128)),
                                [PS[pb]], [s_XeT[b]])

                    def xFFN(e, wgs, wus):
                        b = e % 2
                        for (bank, ws_) in ((2, wgs), (3, wus)):
                            for hp in range(2):
                                w_, wsl_ = ws_[hp]
                                for c in range(NCH):
                                    pe(lambda tt: tt.matmul(P[bank][:, hp * 256:(hp + 1) * 256], lhsT=x_XeT[b][:, c, :], rhs=w_[:, c, :],
                                                            start=(c == 0), stop=(c == NCH - 1)), [s_XeT[b], wsl_], [PS[bank]], inc=(c == NCH - 1))
                        act(lambda a: a.activation(out=x_sg, in_=P[2][:], func=AF.Silu), [PS[2]], [s_xsg])
                        dve(lambda v: v.tensor_tensor(out=x_h1, in0=x_sg, in1=P[3][:], op=ALU.mult), [s_xsg, PS[3]], [s_xh1])

                    def xT1(e):
                        transpose_to(x_h1, s_xh1, lambda c0, n: x_h1T[:, c0:c0 + n, :], s_xh1T, nchunks=4, pb=4)

                    def xY(e, wds):
                        for nb in range(4):
                            pb = 5 + nb % 2
                            for m in range(4):
                                wd_, sd_w = wds[m // 2]
                                pe(lambda tt: tt.matmul(P[pb][:], lhsT=x_h1T[:, m, :], rhs=wd_[:, m % 2, nb * 512:(nb + 1) * 512], start=(m == 0), stop=(m == 3)),
                                   [s_xh1T, sd_w], [PS[pb]], inc=(m == 3))
                            act(lambda a: a.copy(out=x_yb[:, nb * 512:(nb + 1) * 512], in_=P[pb][:]), [PS[pb]], [s_yb])

                    def xT2(e):
                        b = e % 2
                        transpose_to(x_Sel[b][:].rearrange("p t k -> p (t k)"), s_Sel[b], lambda c0, n: x_SelT[:, c0:c0 + n, :], s_SelT, nchunks=4, pb=4)

                    def xScatter(e):
                        i = 0
                        for t in range(4):
                            for nb in range(4):
                                pb = (7, 5, 6, 4)[i % 4]; i += 1
                                pe(lambda tt: tt.matmul(P[pb][:], lhsT=x_SelT[:, t, :], rhs=x_yb[:, nb * 512:(nb + 1) * 512], start=True, stop=True),
                                   [s_SelT, s_yb], [PS[pb]])
                                dve(lambda v: v.scalar_tensor_tensor(out=R[:, t, nb * 512:(nb + 1) * 512], in0=P[pb][:], scalar=gw[:, t, e:e + 1],
                                                                     in1=R[:, t, nb * 512:(nb + 1) * 512], op0=ALU.mult, op1=ALU.add),
                                    [PS[pb], s_gw, s_R[t]], [s_R[t]])

                    NEX = NE
                    xSel(0); xGather(0)
                    wcur = eload(0)
                    for e in range(NEX):
                        if e + 1 < NE:
                            xSel(e + 1)
                        xFFN(e, wcur[0], wcur[1])
                        wds = wcur[2]
                        if e + 1 < NE:
                            xGather(e + 1)
                        xT1(e)
                        xY(e, wds)
                        if e + 1 < NE:
                            wcur = eload(e + 1)
                        xT2(e)
                        xScatter(e)
                    ple_olds = [s_Sel[0], s_Sel[1], s_SelT, s_xh1, s_xh1T, s_yb]
                    PLE_O = NRS * RING_E
                o = PLE_O
                p_xb2 = take(D); p_pb = take(256); p_pT = take(2 * 512).rearrange("p (c k) -> p c k", k=512)
                p_pf = take(256, F32); p_sg = take(256, F32)
                assert o <= (38176 if SPARSE else HALO), o
                s_xb2 = Slot(); s_pb = Slot(); s_pT = Slot(); s_psg = Slot()
                Sched.alias(ple_olds, [s_xb2, s_pb, s_pT, s_pf, s_psg])
                moe_slots = moe_slots + [s_xb2, s_pb, s_pT, s_pf, s_psg]
                load_ln(4)
                for t in range(4):
                    layer_norm(t, EPS_A, p_xb2, s_xb2)
                    transpose_to(p_xb2, s_xb2, lambda c0, n: xT[:, c0:c0 + n, t * 128:(t + 1) * 128], s_xT, pb=0)
                    S.dma("sp", p_pf, pin[(blk * 4 + t) * 128:(blk * 4 + t + 1) * 128, :], writes=[s_pf], dslot=s_pf)
                    act(lambda a: a.copy(out=p_pb, in_=p_pf), [s_pf], [s_pb])
                    transpose_to(p_pb, s_pb, lambda c0, n: p_pT[:, c0:c0 + n, t * 128:(t + 1) * 128], s_pT, nchunks=2, pb=0)
                wpp3 = w_pp.rearrange("(c p) n -> p c n", p=128)
                for nb in range(8):
                    wt, wsl = wload(wcols(w_pg, nb * 256, 256), "")
                    wp, wpl = wload(wpp3[:, :, nb * 256:(nb + 1) * 256], "")
                    for t in range(4):
                        b1, b2 = ((1, 2), (3, 6), (4, 5), (7, 0))[t]
                        for c in range(NCH):
                            pe(lambda tt: tt.matmul(P[b1][:, 0:256], lhsT=xT[:, c, t * 128:(t + 1) * 128], rhs=wt[:, c, :], start=(c == 0), stop=(c == NCH - 1)),
                               [wsl, s_xT], [PS[b1]], inc=(c == NCH - 1))
                        for c in range(2):
                            pe(lambda tt: tt.matmul(P[b2][:, 0:256], lhsT=p_pT[:, c, t * 128:(t + 1) * 128], rhs=wp[:, c, :], start=(c == 0), stop=(c == 1)),
                               [wpl, s_pT], [PS[b2]], inc=(c == 1))
                        act(lambda a: a.activation(out=p_sg, in_=P[b1][:, 0:256], func=AF.Sigmoid), [PS[b1]], [s_psg])
                        dve(lambda v: v.scalar_tensor_tensor(out=p_sg, in0=P[b2][:, 0:256], scalar=IALPHA, in1=p_sg, op0=ALU.mult, op1=ALU.mult),
                            [PS[b2], s_psg], [s_psg])
                        dve(lambda v: v.tensor_tensor(out=R[:, t, nb * 256:(nb + 1) * 256], in0=R[:, t, nb * 256:(nb + 1) * 256], in1=p_sg, op=ALU.add),
                            [s_psg, s_R[t]], [s_R[t]])
                load_ln(6)
                for t in range(4):
                    layer_norm(t, EPS_A, p_xb2, s_xb2)
                    S.dma("sp", out[(blk * 4 + t) * 128:(blk * 4 + t + 1) * 128, :], R[:, t, :], reads=[s_R[t]], dslot=s_xl[t])
            S.dma("sp", flag_o, flagmax[:], reads=[s_flag], dslot=s_flag)
            S.wait_all("sp", s_R + [s_flag])
        print("built: ops", S.nops, "waits", S.nwaits, flush=True)
    return nc


def _consts():
    cst = np.zeros((128, 4, 128), np.float32)
    cst[:, 3, :] = np.arange(128, dtype=np.float32)[None, :]
    cst[:, 0, :] = np.eye(128, dtype=np.float32)
    cst[:, 1, :] = np.triu(np.ones((128, 128), np.float32))
    cst[:, 2, :] = 1.0
    slopes = np.exp2(-8.0 / 16 * np.arange(1, 17, dtype=np.float32)).astype(np.float32)
    j = np.arange(128)[:, None]; i = np.arange(128)[None, :]
    ab = np.zeros((128, 2, 16, 128), np.float32)
    for kt in range(2):
        dist = (i - j + (128 if kt == 0 else 0)).astype(np.float32)
        ok = (dist >= 0) & (dist < 128)
        for h in range(16):
            ab[:, kt, h, :] = np.where(ok, -slopes[h] * dist, NEGBIG)
    return cst, ab


_CACHE = {}


def kernel(x, p, ln_in_g, ln_in_b, w_in, attn_sinks, mlstm_b_i, mlstm_b_f, mlstm_norm_g,
           w_branch_attn, w_branch_mlstm, w_out, ln_mix_g, ln_mix_b, w_router, b_router,
           w_exp_gate, w_exp_up, w_exp_down, w_sh_gate, w_sh_up, w_sh_down, ln_ffn_g, ln_ffn_b,
           w_ple_proj, w_ple_gate, ln_ple_g, ln_ple_b):
    f = lambda a: np.ascontiguousarray(np.asarray(a, dtype=np.float32))
    x = f(x); p = f(p)
    B, SEQ, _ = x.shape
    NQ = 8 // B
    TOK = SEQ // NQ
    NPRE = (SEQ - TOK) // 128
    def get(sparse):
        key = (TOK, NPRE, sparse)
        if key not in _CACHE:
            _CACHE[key] = build(TOK, NPRE, sparse)
        return _CACHE[key]
    nc = get(True)
    cst, ab = _consts()
    shared = {
        "cst": cst, "abias": ab,
        "lng": np.stack([f(ln_in_g), f(ln_in_b), f(ln_mix_g)[0], f(ln_mix_b)[0], f(ln_ffn_g)[0], f(ln_ffn_b)[0],
                         f(ln_ple_g)[0], f(ln_ple_b)[0]]),
        "w_in": f(w_in)[0], "sinks": f(attn_sinks).reshape(1, 16),
        "bif": np.concatenate([f(mlstm_b_i).reshape(-1), f(mlstm_b_f).reshape(-1)]).reshape(1, 8),
        "ng": f(mlstm_norm_g).reshape(1, 1024),
        "w_ba": f(w_branch_attn)[0], "w_bm": f(w_branch_mlstm)[0], "w_out": f(w_out)[0],
        "w_rt": f(w_router)[0], "b_rt": f(b_router).reshape(1, NE),
        "w_eg": np.concatenate([f(w_exp_gate)[0], f(w_sh_gate)], axis=0),
        "w_eu": np.concatenate([f(w_exp_up)[0], f(w_sh_up)], axis=0),
        "w_ed": np.concatenate([f(w_exp_down)[0], f(w_sh_down)], axis=0),
        "w_pp": f(w_ple_proj)[0], "w_pg": f(w_ple_gate)[0],
    }
    in_maps = []
    for c in range(8):
        b, j = c // NQ, c % NQ
        end = (j + 1) * TOK
        xin = np.zeros((SEQ, D), np.float32)
        xin[SEQ - end:] = x[b, :end]
        vm = np.zeros((128, NPRE + 1), np.float32)
        nvalid = (j * TOK) // 128
        if nvalid > 0:
            vm[:, NPRE - nvalid:NPRE] = 1.0
        vm[:, NPRE] = 1.0 if j > 0 else 0.0
        m = dict(shared)
        m["xin"] = xin; m["vmk"] = vm
        m["pin"] = np.ascontiguousarray(p[0, b, j * TOK:(j + 1) * TOK])
        in_maps.append(m)
    res = run_bass_kernel_spmd(nc, in_maps, core_ids=list(range(8)))
    if max(float(np.asarray(r["flag"]).max()) for r in res.results) > 128.5:
        res = run_bass_kernel_spmd(get(False), in_maps, core_ids=list(range(8)))
    outp = np.zeros((B, SEQ, D), np.float32)
    for c in range(8):
        b, j = c // NQ, c % NQ
        outp[b, j * TOK:(j + 1) * TOK] = np.asarray(res.results[c]["out"], dtype=np.float32)
    return outp
```

```python
import numpy as np
from contextlib import ExitStack
import concourse.bass as bass
import concourse.mybir as mybir
from concourse.bass_utils import run_bass_kernel_spmd

F32 = mybir.dt.float32
BF16 = mybir.dt.bfloat16
AF = mybir.ActivationFunctionType
ALU = mybir.AluOpType
AX = mybir.AxisListType

D = 2048
NCH = 16
NE = 64
ALPHA = 2.0 ** 0.25
IALPHA = 1.0 / ALPHA
EPS_A = 1e-5 / (ALPHA * ALPHA)
C_AQ, C_AK, C_AV, C_MQ, C_MK, C_MV, C_MO, C_MI, C_GA, C_GB = 0, 1024, 1152, 1280, 1792, 2304, 3328, 4352, 4360, 6408
NEGBIG = -30000.0


class Slot:
    __slots__ = ("name", "w", "r", "dsem", "dcnt")

    def __init__(self, name=""):
        self.name = name; self.w = None; self.r = {}; self.dsem = None; self.dcnt = 0


class Eng:
    def __init__(self, obj, sem):
        self.obj = obj; self.sem = sem; self.n = 0; self.waited = {}; self.own = {id(sem)}


class Sched:
    def __init__(self, nc, sems):
        self.nc = nc
        self.E = {"pe": Eng(nc.tensor, sems["pe"]), "dve": Eng(nc.vector, sems["dve"]),
                  "act": Eng(nc.scalar, sems["act"]), "pool": Eng(nc.gpsimd, sems["pool"]),
                  "sp": Eng(nc.sync, sems["sp"])}
        self.nops = 0; self.nwaits = 0

    def new_epoch(self, sems):
        for k, e in self.E.items():
            e.sem = sems[k]; e.n = 0; e.own.add(id(e.sem))

    def _wait(self, e, deps):
        best = {}
        for (sem, val, raw) in deps:
            if id(sem) in e.own and not raw:
                continue
            k = id(sem)
            if e.waited.get(k, 0) >= val:
                continue
            if k not in best or best[k][1] < val:
                best[k] = (sem, val)
        for k, (sem, val) in best.items():
            e.obj.wait_ge(sem, val); e.waited[k] = val; self.nwaits += 1

    def _deps(self, reads, writes):
        deps = []
        for s in reads:
            if s.w is not None:
                deps.append((s.w[0], s.w[1], True))
        for s in writes:
            if s.w is not None:
                deps.append((s.w[0], s.w[1], False))
            deps.extend((a, b, False) for (a, b) in s.r.values())
        return deps

    def op(self, eng, fn, reads=(), writes=(), inc=True):
        e = self.E[eng]
        self._wait(e, self._deps(reads, writes))
        inst = fn(e.obj)
        ev = (e.sem, e.n + 1)
        if inc:
            inst.then_inc(e.sem, 1); e.n += 1
        for s in reads:
            s.r[id(e.sem)] = ev
        for s in writes:
            s.w = ev; s.r = {}
        self.nops += 1
        return ev

    def dma(self, eng, out, in_, reads=(), writes=(), dslot=None):
        e = self.E[eng]
        waw = {(id(w.w[0]), w.w[1]) for w in writes if w.w is not None and w.w[0] is dslot.dsem}
        self._wait(e, [d for d in self._deps(reads, writes) if (id(d[0]), d[1]) not in waw])
        dslot.dcnt += 1
        e.obj.dma_start(out=out, in_=in_).then_inc(dslot.dsem, 16)
        ev = (dslot.dsem, 16 * dslot.dcnt)
        for s in reads:
            s.r[id(dslot.dsem)] = ev
        for s in writes:
            s.w = ev; s.r = {}
        return ev

    def wait_all(self, eng, slots):
        e = self.E[eng]
        deps = []
        for s in slots:
            if s.w is not None:
                deps.append((s.w[0], s.w[1], True))
            deps.extend((a, b, True) for (a, b) in s.r.values())
        self._wait(e, deps)

    @staticmethod
    def alias(olds, news):
        m = {}
        for s in olds:
            evs = list(s.r.values()) + ([s.w] if s.w is not None else [])
            for (sem, val) in evs:
                k = id(sem)
                if k not in m or m[k][1] < val:
                    m[k] = (sem, val)
        for s in news:
            s.w = None; s.r = dict(m)


def build(TOK, NPRE, SPARSE=True):
    NT = TOK // 128
    NBLK = TOK // 512
    NTT = NPRE + NT
    nc = bass.Bass("TRN2", target_bir_lowering=False)

    def din(name, shape, dt=F32):
        return nc.dram_tensor(name, list(shape), dt, kind="ExternalInput").ap()

    xin = din("xin", [NTT * 128, D])
    pin = din("pin", [TOK, 256])
    vmk = din("vmk", [128, NPRE + 1])
    cst = din("cst", [128, 4, 128])
    abias_d = din("abias", [128, 2, 16, 128])
    lng = din("lng", [8, D])
    w_in = din("w_in", [D, 8456])
    sinks = din("sinks", [1, 16])
    bif_d = din("bif", [1, 8])
    ng_d = din("ng", [1, 1024])
    w_ba = din("w_ba", [1024, D])
    w_bm = din("w_bm", [1024, D])
    w_out = din("w_out", [D, D])
    w_rt = din("w_rt", [D, NE])
    b_rt = din("b_rt", [1, NE])
    w_eg = din("w_eg", [NE + 1, D, 512])
    w_eu = din("w_eu", [NE + 1, D, 512])
    w_ed = din("w_ed", [NE + 1, 512, D])
    w_pp = din("w_pp", [256, D])
    w_pg = din("w_pg", [D, D])
    out = nc.dram_tensor("out", [TOK, D], F32, kind="ExternalOutput").ap()
    flag_o = nc.dram_tensor("flag", [128, 1], F32, kind="ExternalOutput").ap()
    PRECAST = SPARSE
    if PRECAST:
        wbg = nc.dram_tensor("wbg", [NE + 1, D, 512], BF16, kind="Internal").ap()
        wbu = nc.dram_tensor("wbu", [NE + 1, D, 512], BF16, kind="Internal").ap()
        wbd = nc.dram_tensor("wbd", [NE + 1, 512, D], BF16, kind="Internal").ap()

    with ExitStack() as es:
        def sb(name, shape, dt):
            return es.enter_context(nc.sbuf_tensor(name, list(shape), dt))

        def sem(name):
            return es.enter_context(nc.semaphore(name))

        nsem = [0]

        def engsems():
            nsem[0] += 1
            return {k: sem("e%d_%s" % (nsem[0], k)) for k in ["pe", "dve", "act", "pool", "sp"]}

        def dslot(name):
            s = Slot(name); s.dsem = sem("d_" + name); return s

        S = Sched(nc, engsems())

        WSN = 54816
        R = sb("R", [128, 4, D], F32)
        xT = sb("xT", [128, NCH, 512], BF16)
        WS = sb("WS", [128, WSN], BF16)
        cf = sb("cf", [128, 4, 128], F32)
        idb = sb("idb", [128, 128], BF16)
        onesb = sb("onesb", [128, 128], BF16)
        abias = sb("abias_s", [128, 2, 16, 128], F32)
        esink = sb("esink", [128, 16], F32)
        bif = sb("bifs", [128, 8], F32)
        ngB = sb("ngB", [128, 1024], F32)
        brB = sb("brB", [128, NE], F32)
        vm = sb("vm", [128, NPRE + 1], F32)
        kvb = sb("kvb", [128, 1], F32)
        wrt = sb("wrt", [128, NCH, NE], BF16)
        C32 = sb("C32", [128, 4, 256], F32)
        n32 = sb("n32", [128, 4], F32)
        Cb = sb("Cb", [128, 4, 258], BF16)
        gw = sb("gw", [128, 4, NE + 1], F32)
        sm = sb("sm", [128, 160], F32)
        st6 = sb("st6", [128, 4, 6], F32)
        junk = sb("junk", [128, 256], BF16)
        stri = sb("stri", [128, 128], BF16)
        flagmax = sb("flagmax", [128, 1], F32)
        gB = sb("gB", [128, D], F32)
        bB = sb("bB", [128, D], F32)
        idf = cf[:, 0, :]; tri = cf[:, 1, :]; onesf = cf[:, 2, :]; iotaf = cf[:, 3, :]

        P = [es.enter_context(nc.psum_tensor("ps%d" % i, [128, 512], F32)) for i in range(8)]
        PS = [Slot("ps%d" % i) for i in range(8)]

        s_R = [Slot("R%d" % i) for i in range(4)]
        s_xT = Slot("xT")
        s_c = dslot("c"); s_ab = dslot("ab"); s_es = dslot("es"); s_bif = dslot("bif"); s_ng = dslot("ng")
        s_br = dslot("br"); s_vm = dslot("vm"); s_wrt = dslot("wrt"); s_gB = dslot("gB"); s_bB = dslot("bB")
        s_idb = Slot("idb"); s_C = Slot("C"); s_Cb = Slot("Cb"); s_gw = Slot("gw"); s_sm = Slot("sm")
        s_st6 = Slot("st"); s_junk = Slot("junk"); s_kvb = Slot("kvb")
        s_xl = [dslot("xl%d" % i) for i in range(4)]

        def ws(off, n, dt=BF16):
            if dt == F32:
                return WS[:, off:off + 2 * n].bitcast(F32)
            return WS[:, off:off + n]

        block = es.enter_context(nc.Block())

        @block.sync
        def _(sync):
            dve = lambda fn, r=(), w=(): S.op("dve", fn, r, w)
            act = lambda fn, r=(), w=(): S.op("act", fn, r, w)
            pool = lambda fn, r=(), w=(): S.op("pool", fn, r, w)
            pe = lambda fn, r=(), w=(), inc=True: S.op("pe", fn, r, w, inc)

            S.dma("sp", cf[:], cst, writes=[s_c], dslot=s_c)
            S.dma("sp", abias[:], abias_d, writes=[s_ab], dslot=s_ab)
            S.dma("sp", esink[:], sinks.partition_broadcast(128).rearrange("p a b -> p (a b)"), writes=[s_es], dslot=s_es)
            S.dma("sp", bif[:], bif_d.partition_broadcast(128).rearrange("p a b -> p (a b)"), writes=[s_bif], dslot=s_bif)
            S.dma("sp", ngB[:], ng_d.partition_broadcast(128).rearrange("p a b -> p (a b)"), writes=[s_ng], dslot=s_ng)
            S.dma("sp", brB[:], b_rt.partition_broadcast(128).rearrange("p a b -> p (a b)"), writes=[s_br], dslot=s_br)
            S.dma("sp", vm[:], vmk, writes=[s_vm], dslot=s_vm)
            S.dma("pool", wrt[:], w_rt.rearrange("(c p) n -> p c n", p=128), writes=[s_wrt], dslot=s_wrt)
            dve(lambda v: v.tensor_copy(out=idb[:], in_=idf), [s_c], [s_idb])
            dve(lambda v: v.tensor_copy(out=onesb[:], in_=onesf), [s_c], [s_idb])
            act(lambda a: a.activation(out=esink[:], in_=esink[:], func=AF.Exp), [s_es], [s_es])
            dve(lambda v: v.tensor_scalar(out=kvb[:], in0=vm[:, NPRE:NPRE + 1], scalar1=-1.0, scalar2=-NEGBIG,
                                          op0=ALU.add, op1=ALU.mult), [s_vm], [s_kvb])
            dve(lambda v: v.memset(C32[:], 0.0), [], [s_C])
            dve(lambda v: v.memset(n32[:], 0.0), [], [s_C])
            dve(lambda v: v.memset(Cb[:], 0.0), [], [s_Cb])
            dve(lambda v: v.memset(gw[:], IALPHA), [], [s_gw])
            s_flag = dslot("flag")
            dve(lambda v: v.memset(flagmax[:], 0.0), [], [s_flag])
            dve(lambda v: v.tensor_tensor(out=stri[:], in0=tri, in1=idf, op=ALU.subtract), [s_c], [s_idb])
            s_pc = [dslot("pc%d" % i) for i in range(9)]
            if PRECAST:
                hist = []
                for e in range(NE + 1):
                    for (dst_, src_) in ((wbg, w_eg), (wbu, w_eu), (wbd, w_ed)):
                        if len(hist) >= 6:
                            psem, pval = hist[-6]
                            nc.gpsimd.wait_ge(psem, pval)
                        ev = S.dma("pool", dst_[e], src_[e], writes=[s_pc[e // 8]], dslot=s_pc[e // 8])
                        hist.append(ev)

            def load_ln(idx):
                S.dma("sp", gB[:], lng[idx].partition_broadcast(128), writes=[s_gB], dslot=s_gB)
                S.dma("sp", bB[:], lng[idx + 1].partition_broadcast(128), writes=[s_bB], dslot=s_bB)

            def layer_norm(t, eps, xb, s_xb, use_pool=False, sc0=0, s_st=None):
                s_q = s_sm if s_st is None else s_st
                rt = R[:, t, :]
                c0, c1, c2, c3 = sc0, sc0 + 1, sc0 + 2, sc0 + 3
                for i in range(4):
                    dve(lambda v: v.bn_stats(out=st6[:, i, :], in_=rt[:, i * 512:(i + 1) * 512]), [s_R[t]], [s_st6])
                dve(lambda v: v.bn_aggr(out=sm[:, c0:c0 + 2], in_=st6[:].rearrange("p a b -> p (a b)")), [s_st6], [s_q])
                act(lambda a: a.activation(out=sm[:, c2:c2 + 1], in_=sm[:, c1:c1 + 1], func=AF.Sqrt, bias=float(eps), scale=1.0), [s_q], [s_q])
                dve(lambda v: v.reciprocal(out=sm[:, c2:c2 + 1], in_=sm[:, c2:c2 + 1]), [s_q], [s_q])
                if use_pool:
                    dve(lambda v: v.tensor_scalar(out=sm[:, c3:c3 + 1], in0=sm[:, c0:c0 + 1], scalar1=sm[:, c2:c2 + 1], scalar2=-1.0,
                                                  op0=ALU.mult, op1=ALU.mult), [s_q], [s_q])
                    act(lambda a: a.activation(out=rt, in_=rt, func=AF.Identity, bias=sm[:, c3:c3 + 1], scale=sm[:, c2:c2 + 1]),
                        [s_R[t], s_q], [s_R[t]])
                    pool(lambda g: g.tensor_tensor(out=rt, in0=rt, in1=gB[:], op=ALU.mult), [s_R[t], s_gB], [s_R[t]])
                    pool(lambda g: g.tensor_tensor(out=xb, in0=rt, in1=bB[:], op=ALU.add), [s_R[t], s_bB], [s_xb])
                    return
                dve(lambda v: v.scalar_tensor_tensor(out=rt, in0=rt, scalar=sm[:, c0:c0 + 1], in1=gB[:], op0=ALU.subtract, op1=ALU.mult),
                    [s_R[t], s_q, s_gB], [s_R[t]])
                dve(lambda v: v.scalar_tensor_tensor(out=rt, in0=rt, scalar=sm[:, c2:c2 + 1], in1=bB[:], op0=ALU.mult, op1=ALU.add),
                    [s_R[t], s_q, s_bB], [s_R[t]])
                act(lambda a: a.copy(out=xb, in_=rt), [s_R[t]], [s_xb])

            def transpose_to(xb, s_xb, dst_fn, s_dst, nchunks=NCH, pb=0):
                pbv = P[pb][:].bitcast(BF16)
                for g0 in range(0, nchunks, 8):
                    n = min(8, nchunks - g0)
                    for j in range(n):
                        c = g0 + j
                        pe(lambda t: t.transpose(out=pbv[:, j * 128:(j + 1) * 128], in_=xb[:, c * 128:(c + 1) * 128], identity=idb[:]),
                           [s_xb, s_idb], [PS[pb]], inc=(j == n - 1))
                    act(lambda a: a.copy(out=dst_fn(g0, n), in_=pbv[:, 0:n * 128].rearrange("p (c k) -> p c k", k=128)),
                        [PS[pb]], [s_dst])

            ring = {"slots": [], "aps": [], "i": 0}
            RING_E = 4096

            def wload(src, shape_str, q="pool", xr=()):
                i = ring["i"] % len(ring["slots"]); ring["i"] += 1
                sl = ring["slots"][i]
                n = 1
                for d in src.shape[1:]:
                    n *= d
                dst = ring["aps"][i][0:src.shape[0], 0:n]
                if len(src.shape) == 3:
                    dst = dst.rearrange("p (a b) -> p a b", b=src.shape[2])
                S.dma(q, dst, src, reads=list(xr), writes=[sl], dslot=sl)
                return dst, sl

            def wcols(wd, c0, n):
                return wd.rearrange("(c p) n -> p c n", p=128)[:, :, c0:c0 + n]

            def mlstm_gates(gate_ps, s_gate_ps, pg, vcol, co=0, s_gs=None):
                sq = s_sm if s_gs is None else s_gs
                k = lambda a, b: sm[:, co + a:co + b]
                if gate_ps is not None:
                    dve(lambda v: v.tensor_tensor(out=k(8, 16), in0=gate_ps, in1=bif[:], op=ALU.add), [s_gate_ps, s_bif], [sq])
                act(lambda a: a.activation(out=k(8, 16), in_=k(8, 16), func=AF.Tanh, scale=1.0 / 15.0), [sq], [sq])
                act(lambda a: a.activation(out=k(16, 20), in_=k(12, 16), func=AF.Exp, scale=-15.0), [sq], [sq])
                act(lambda a: a.activation(out=k(16, 20), in_=k(16, 20), func=AF.Ln, bias=1.0, scale=1.0), [sq], [sq])
                pe(lambda t: t.matmul(P[pg][:, 0:4], lhsT=tri, rhs=k(16, 20), start=True, stop=True), [s_c, sq], [PS[pg]], inc=False)
                pe(lambda t: t.matmul(P[pg][:, 4:8], lhsT=onesf, rhs=k(16, 20), start=True, stop=True), [s_c, sq], [PS[pg]])
                dve(lambda v: v.tensor_copy(out=k(20, 24), in_=P[pg][:, 0:4]), [PS[pg]], [sq])
                dve(lambda v: v.scalar_tensor_tensor(out=k(24, 28), in0=k(8, 12), scalar=15.0, in1=k(20, 24),
                                                     op0=ALU.mult, op1=ALU.add), [sq], [sq])
                dve(lambda v: v.tensor_tensor(out=k(28, 32), in0=k(24, 28), in1=P[pg][:, 4:8], op=ALU.subtract), [sq, PS[pg]], [sq])
                act(lambda a: a.activation(out=k(28, 32), in_=k(28, 32), func=AF.Exp), [sq], [sq])
                if vcol is not None:
                    dve(lambda v: v.tensor_scalar(out=k(28, 32), in0=k(28, 32), scalar1=vm[:, vcol:vcol + 1], scalar2=None,
                                                  op0=ALU.mult), [sq, s_vm], [sq])
                act(lambda a: a.activation(out=k(32, 36), in_=P[pg][:, 4:8], func=AF.Exp, scale=-1.0), [PS[pg]], [sq])

            def mlstm_update(ktok_ps, s_ktok, kw, s_kw, vaug, s_vaug, pu0, pu1, pn):
                for h in range(4):
                    act(lambda a: a.activation(out=kw[:, h, :], in_=ktok_ps[:, h, :], func=AF.Copy, scale=sm[:, 28 + h:29 + h]),
                        [s_ktok, s_sm], [s_kw])
                for h in range(4):
                    pb = pu0 if h < 2 else pu1
                    pe(lambda t: t.matmul(P[pb][:, (h % 2) * 256:(h % 2 + 1) * 256], lhsT=kw[:, h, :], rhs=vaug[:, h, 0:256],
                                          start=True, stop=True), [s_kw, s_vaug], [PS[pb]], inc=(h % 2 == 1))
                for h in range(4):
                    pe(lambda t: t.matmul(P[pn][:, 16 + h:17 + h], lhsT=kw[:, h, :], rhs=vaug[:, h, 256:257], start=True, stop=True),
                       [s_kw, s_vaug], [PS[pn]], inc=(h == 3))
                for h in range(4):
                    pb = pu0 if h < 2 else pu1
                    dve(lambda v: v.scalar_tensor_tensor(out=C32[:, h, :], in0=C32[:, h, :], scalar=sm[:, 32 + h:33 + h],
                                                         in1=P[pb][:, (h % 2) * 256:(h % 2 + 1) * 256], op0=ALU.mult, op1=ALU.add),
                        [s_C, s_sm, PS[pb]], [s_C])
                dve(lambda v: v.tensor_tensor(out=n32[:], in0=n32[:], in1=sm[:, 32:36], op=ALU.mult), [s_C, s_sm], [s_C])
                dve(lambda v: v.tensor_tensor(out=n32[:], in0=n32[:], in1=P[pn][:, 16:20], op=ALU.add), [s_C, PS[pn]], [s_C])
                act(lambda a: a.copy(out=Cb[:, :, 0:256], in_=C32[:]), [s_C], [s_Cb])
                act(lambda a: a.copy(out=Cb[:, :, 256:257], in_=n32[:].rearrange("p (h o) -> p h o", o=1)), [s_C], [s_Cb])

            PW = 512 + 1024 + 8 + 128 + 128
            o = 0
            wpre = ws(o, NCH * PW).rearrange("p (c n) -> p c n", n=PW); o += NCH * PW
            p_xb = []; p_hT = []; p_va = []; p_kt = []
            for b in range(2):
                p_xb.append(ws(o, D)); o += D
                p_hT.append(ws(o, NCH * 128).rearrange("p (c k) -> p c k", k=128)); o += NCH * 128
                p_va.append(ws(o, 4 * 258).rearrange("p (h k) -> p h k", k=258)); o += 4 * 258
                p_kt.append(ws(o, 512).rearrange("p (h k) -> p h k", k=128)); o += 512
            p_kw = ws(o, 512).rearrange("p (h k) -> p h k", k=128); o += 512
            HALO = WSN - 256
            assert o <= HALO
            kT_halo = ws(HALO, 128); V_halo = ws(HALO + 128, 128)
            s_wpre = dslot("wpre"); s_pkw = Slot(); s_halo = Slot()
            s_pxb = [Slot(), Slot()]; s_phT = [Slot(), Slot()]; s_pva = [Slot(), Slot()]; s_pkt = [Slot(), Slot()]
            s_g = [Slot(), Slot()]
            GCO = [0, 52]

            def pA(pt):
                b = pt % 2; t = pt % 4
                S.dma("sp", R[:, t, :], xin[pt * 128:(pt + 1) * 128, :], writes=[s_R[t]], dslot=s_xl[t])
                layer_norm(t, 1e-5, p_xb[b], s_pxb[b], use_pool=True, sc0=104 + 4 * b, s_st=s_g[b])

            def pT(pt):
                b = pt % 2
                transpose_to(p_xb[b], s_pxb[b], lambda c0, n: p_hT[b][:, c0:c0 + n, :], s_phT[b], pb=0)

            def pB(pt):
                b = pt % 2; co = GCO[b]
                for (pb, po, n) in [(1, 0, 512), (2, 512, 512), (3, 1024, 512)]:
                    for c in range(NCH):
                        pe(lambda tt: tt.matmul(P[pb][:, 0:n], lhsT=p_hT[b][:, c, :], rhs=wpre[:, c, po:po + n], start=(c == 0), stop=(c == NCH - 1)),
                           [s_phT[b], s_wpre], [PS[pb]], inc=(c == NCH - 1))
                for c in range(NCH):
                    pe(lambda tt: tt.matmul(P[6][:, 0:8], lhsT=p_hT[b][:, c, :], rhs=wpre[:, c, 1536:1544], start=(c == 0), stop=(c == NCH - 1)),
                       [s_phT[b], s_wpre], [PS[6]], inc=(c == NCH - 1))
                act(lambda a: a.activation(out=p_kt[b][:].rearrange("p h k -> p (h k)"), in_=P[1][:], func=AF.Copy, scale=128.0 ** -0.5), [PS[1]], [s_pkt[b]])
                act(lambda a: a.copy(out=p_va[b][:, 0:2, 0:256], in_=P[2][:].rearrange("p (h k) -> p h k", k=256)), [PS[2]], [s_pva[b]])
                act(lambda a: a.copy(out=p_va[b][:, 2:4, 0:256], in_=P[3][:].rearrange("p (h k) -> p h k", k=256)), [PS[3]], [s_pva[b]])
                dve(lambda v: v.tensor_tensor(out=sm[:, co + 8:co + 16], in0=P[6][:, 0:8], in1=bif[:], op=ALU.add), [PS[6], s_bif], [s_g[b]])
                if pt == NPRE - 1:
                    for c in range(NCH):
                        pe(lambda tt: tt.matmul(P[1][:, 0:128], lhsT=wpre[:, c, 1544:1672], rhs=p_hT[b][:, c, :], start=(c == 0), stop=(c == NCH - 1)),
                           [s_phT[b], s_wpre], [PS[1]], inc=(c == NCH - 1))
                    for c in range(NCH):
                        pe(lambda tt: tt.matmul(P[2][:, 0:128], lhsT=p_hT[b][:, c, :], rhs=wpre[:, c, 1672:1800], start=(c == 0), stop=(c == NCH - 1)),
                           [s_phT[b], s_wpre], [PS[2]], inc=(c == NCH - 1))
                    act(lambda a: a.copy(out=kT_halo, in_=P[1][:, 0:128]), [PS[1]], [s_halo])
                    act(lambda a: a.copy(out=V_halo, in_=P[2][:, 0:128]), [PS[2]], [s_halo])

            def pC1(pt):
                b = pt % 2
                mlstm_gates(None, None, 7, pt, co=GCO[b], s_gs=s_g[b])

            def pC2(pt):
                b = pt % 2; co = GCO[b]
                for h in range(4):
                    act(lambda a: a.activation(out=p_kw[:, h, :], in_=p_kt[b][:, h, :], func=AF.Copy, scale=sm[:, co + 28 + h:co + 29 + h]),
                        [s_pkt[b], s_g[b]], [s_pkw])
                for h in range(4):
                    pb = 4 if h < 2 else 5
                    pe(lambda tt: tt.matmul(P[pb][:, (h % 2) * 256:(h % 2 + 1) * 256], lhsT=p_kw[:, h, :], rhs=p_va[b][:, h, 0:256], start=True, stop=True),
                       [s_pkw, s_pva[b]], [PS[pb]], inc=(h % 2 == 1))
                for h in range(4):
                    pe(lambda tt: tt.matmul(P[7][:, 16 + h:17 + h], lhsT=p_kw[:, h, :], rhs=p_va[b][:, h, 256:257], start=True, stop=True),
                       [s_pkw, s_pva[b]], [PS[7]], inc=(h == 3))
                for h in range(4):
                    pb = 4 if h < 2 else 5
                    dve(lambda v: v.scalar_tensor_tensor(out=C32[:, h, :], in0=C32[:, h, :], scalar=sm[:, co + 32 + h:co + 33 + h],
                                                         in1=P[pb][:, (h % 2) * 256:(h % 2 + 1) * 256], op0=ALU.mult, op1=ALU.add),
                        [s_C, s_g[b], PS[pb]], [s_C])
                dve(lambda v: v.tensor_tensor(out=n32[:], in0=n32[:], in1=sm[:, co + 32:co + 36], op=ALU.mult), [s_C, s_g[b]], [s_C])
                dve(lambda v: v.tensor_tensor(out=n32[:], in0=n32[:], in1=P[7][:, 16:20], op=ALU.add), [s_C, PS[7]], [s_C])

            if NPRE > 0:
                segs = [(C_MK, 512), (C_MV, 1024), (C_MI, 8), (C_AK, 128), (C_AV, 128)]
                po = 0
                for (c0, n) in segs:
                    S.dma("pool", wpre[:, :, po:po + n], wcols(w_in, c0, n), writes=[s_wpre], dslot=s_wpre)
                    po += n
                for b in range(2):
                    dve(lambda v: v.memset(p_va[b][:], 1.0), [], [s_pva[b]])
                load_ln(0)
                pA(0); pT(0)
                for pt in range(NPRE):
                    if pt + 1 < NPRE:
                        pA(pt + 1)
                    pB(pt)
                    if pt >= 1:
                        pC2(pt - 1)
                    if pt + 1 < NPRE:
                        pT(pt + 1)
                    pC1(pt)
                pC2(NPRE - 1)
                act(lambda a: a.copy(out=Cb[:, :, 0:256], in_=C32[:]), [s_C], [s_Cb])
                act(lambda a: a.copy(out=Cb[:, :, 256:257], in_=n32[:].rearrange("p (h o) -> p h o", o=1)), [s_C], [s_Cb])
            if NPRE == 0:
                dve(lambda v: v.memset(kT_halo, 0.0), [], [s_halo])
                dve(lambda v: v.memset(V_halo, 0.0), [], [s_halo])

            SB = 512
            RING_E = 4096
            NRM, NRE = 5, 12
            Sched.alias([s_sm] + s_g, [s_sm])
            ring_sl = [dslot("rg%d" % i) for i in range(NRE)]
            s_pf = dslot("pf")
            rot = {"i": 0}

            def nbank(banks=(1, 2, 6, 7)):
                rot["i"] += 1
                return banks[rot["i"] % len(banks)]

            def ring_setup(nslots):
                ring["slots"] = ring_sl[:nslots]
                ring["aps"] = [ws(i * RING_E, RING_E) for i in range(nslots)]
                ring["i"] = 0

            w3 = w_in.rearrange("(c p) n -> p c n", p=128)
            wba4 = w_ba.rearrange("(g j d) n -> g d j n", g=2, d=64)
            wbm3 = w_bm.rearrange("(c p) n -> p c n", p=128)
            moe_slots = None
            for blk in range(NBLK):
                S.new_epoch(engsems())
                o = NRM * RING_E

                def take(n, dt=BF16):
                    nonlocal o
                    v = ws(o, n, dt); o += n * (2 if dt == F32 else 1); return v
                o_mg = o
                m_qT = take(8 * SB).rearrange("p (j k) -> p j k", k=SB)
                m_mqT = take(4 * SB).rearrange("p (h k) -> p h k", k=SB)
                m_mkT = take(4 * SB).rearrange("p (h k) -> p h k", k=SB)
                m_mgT = ws(o_mg, 16 * SB).rearrange("p (c k) -> p c k", k=SB)
                m_kT = take(5 * 128).rearrange("p (t k) -> p t k", k=128)
                m_V = take(5 * 128).rearrange("p (t k) -> p t k", k=128)
                m_va = take(4 * 4 * 258).rearrange("p (t h k) -> p t h k", h=4, k=258)
                m_gs = take(4 * 1024).rearrange("p (t k) -> p t k", k=1024)
                m_attnT = take(8 * SB).rearrange("p (h k) -> p h k", k=SB)
                m_mhT = take(8 * SB).rearrange("p (c k) -> p c k", k=SB)
                m_xb = take(D)
                m_sc = take(512, F32)
                m_PT = take(2 * 512).rearrange("p (t k) -> p t k", k=512)
                m_dn = take(512, F32)
                m_eb = take(512).rearrange("p (h k) -> p h k", k=128)
                m_Sq = take(512).rearrange("p (h k) -> p h k", k=128)
                m_qd = take(512).rearrange("p (h k) -> p h k", k=128)
                m_kw = take(512).rearrange("p (h k) -> p h k", k=128)
                m_mh = take(1024)
                m_Dt = m_dn.rearrange("p (h k) -> p h k", k=128)
                m_rE = m_sc.rearrange("p (h k) -> p h k", k=128)
                m_t1 = m_sc
                m_t2 = m_PT.rearrange("p t k -> p (t k)").bitcast(F32)
                assert o <= HALO, o
                names = "xb V va gs qT kT mqT mkT attnT mhT mgT sc PT eb Sq qd kw mh dn".split()
                sl = {n: Slot(n) for n in names}
                sl["Dt"] = sl["dn"]; sl["rE"] = sl["sc"]; sl["t1"] = sl["sc"]; sl["t2"] = sl["PT"]
                ring_setup(NRM)
                mix_slots = [v for k, v in sl.items() if k != "mgT"] + ring["slots"]
                if blk == 0:
                    Sched.alias([s_wpre, s_pkw] + s_pxb + s_phT + s_pva + s_pkt, mix_slots)
                else:
                    Sched.alias(moe_slots, mix_slots)
                dve(lambda v: v.memset(m_va[:], 1.0), [], [sl["va"]])
                tiles = [0, 1, 2, 3]
                gt0 = NPRE + blk * 4
                load_ln(0)
                for t in tiles:
                    S.dma("sp", R[:, t, :], xin[(gt0 + t) * 128:(gt0 + t + 1) * 128, :], writes=[s_R[t]], dslot=s_xl[t])
                for t in tiles:
                    layer_norm(t, 1e-5, m_xb, sl["xb"])
                    transpose_to(m_xb, sl["xb"], lambda c0, n: xT[:, c0:c0 + n, t * 128:(t + 1) * 128], s_xT, pb=0)
                act(lambda a: a.copy(out=m_kT[:, 0, :], in_=kT_halo), [s_halo], [sl["kT"]])
                act(lambda a: a.copy(out=m_V[:, 0, :], in_=V_halo), [s_halo], [sl["V"]])

                def fmm(wt2, wsl, pb):
                    for c in range(NCH):
                        pe(lambda tt: tt.matmul(P[pb][:], lhsT=wt2[:, c, :], rhs=xT[:, c, :], start=(c == 0), stop=(c == NCH - 1)),
                           [wsl, s_xT], [PS[pb]], inc=(c == NCH - 1))
                for jp in range(4):
                    i = ring["i"] % NRM; ring["i"] += 1
                    wsl = ring["slots"][i]
                    wt = ring["aps"][i].rearrange("p (m c g r) -> p m c g r", m=2, g=2, r=64)
                    for mm in range(2):
                        j = jp * 2 + mm
                        src = w3[:, :, j * 64:j * 64 + 1024].rearrange("p c (g r) -> p c g r", r=512)
                        for gg in range(2):
                            S.dma("pool", wt[:, mm, :, gg, :], src[:, :, gg, 0:64], writes=[wsl], dslot=wsl)
                    wt2 = ring["aps"][i].rearrange("p (m c k) -> p m c k", m=2, k=128)
                    for mm in range(2):
                        j = jp * 2 + mm
                        pb = nbank()
                        fmm(wt2[:, mm], wsl, pb)
                        act(lambda a: a.copy(out=m_qT[:, j, :], in_=P[pb][:]), [PS[pb]], [sl["qT"]])
                wt, wsl = wload(wcols(w_in, C_AK, 128), "")
                pb = nbank(); fmm(wt, wsl, pb)
                act(lambda a: a.copy(out=m_kT[:, 1:5, :], in_=P[pb][:].rearrange("p (t k) -> p t k", k=128)), [PS[pb]], [sl["kT"]])
                for hp in range(2):
                    wt, wsl = wload(wcols(w_in, C_MQ + hp * 256, 256), "")
                    for mm in range(2):
                        h = hp * 2 + mm
                        pb = nbank(); fmm(wt[:, :, mm * 128:(mm + 1) * 128], wsl, pb)
                        act(lambda a: a.copy(out=m_mqT[:, h, :], in_=P[pb][:]), [PS[pb]], [sl["mqT"]])
                for hp in range(2):
                    wt, wsl = wload(wcols(w_in, C_MK + hp * 256, 256), "")
                    for mm in range(2):
                        h = hp * 2 + mm
                        pb = nbank(); fmm(wt[:, :, mm * 128:(mm + 1) * 128], wsl, pb)
                        act(lambda a: a.activation(out=m_mkT[:, h, :], in_=P[pb][:], func=AF.Copy, scale=128.0 ** -0.5), [PS[pb]], [sl["mkT"]])
                gate_sb = sm[:, 112:144].rearrange("p (t k) -> p t k", k=8)

                def tproj(c0, n, evac):
                    wt, wsl = wload(wcols(w_in, c0, n), "")
                    for t in tiles:
                        pb = nbank()
                        for c in range(NCH):
                            pe(lambda tt: tt.matmul(P[pb][:, 0:n], lhsT=xT[:, c, t * 128:(t + 1) * 128], rhs=wt[:, c, :], start=(c == 0), stop=(c == NCH - 1)),
                               [wsl, s_xT], [PS[pb]], inc=(c == NCH - 1))
                        evac(t, pb)
                tproj(C_AV, 128, lambda t, pb: act(lambda a: a.copy(out=m_V[:, 1 + t, :], in_=P[pb][:, 0:128]), [PS[pb]], [sl["V"]]))
                for h in range(4):
                    tproj(C_MV + h * 256, 256, lambda t, pb: act(lambda a: a.copy(out=m_va[:, t, h, 0:256], in_=P[pb][:, 0:256]), [PS[pb]], [sl["va"]]))
                for q in range(4):
                    def ev_mo(t, pb):
                        act(lambda a: a.activation(out=m_gs[:, t, q * 256:(q + 1) * 256], in_=P[pb][:, 0:256], func=AF.Sigmoid), [PS[pb]], [sl["gs"]])
                        dve(lambda v: v.tensor_tensor(out=m_gs[:, t, q * 256:(q + 1) * 256], in0=m_gs[:, t, q * 256:(q + 1) * 256],
                                                      in1=ngB[:, q * 256:(q + 1) * 256], op=ALU.mult), [sl["gs"], s_ng], [sl["gs"]])
                    tproj(C_MO + q * 256, 256, ev_mo)
                tproj(C_MI, 8, lambda t, pb: dve(lambda v: v.tensor_copy(out=gate_sb[:, t, :], in_=P[pb][:, 0:8]), [PS[pb]], [s_sm]))

                for t in tiles:
                    qc = slice(t * 128, (t + 1) * 128)
                    first_tile = (blk == 0 and t == 0)
                    for g in range(2):
                        pr = slice(g * 64, (g + 1) * 64)
                        for half in range(2):
                            hh = g * 8 + half * 4
                            for kt in range(2):
                                pb = nbank((1, 2))
                                pe(lambda tt: tt.matmul(P[pb][:], lhsT=m_kT[pr, t + kt, :], rhs=m_qT[pr, half * 4:half * 4 + 4, qc],
                                                        start=True, stop=True), [sl["kT"], sl["qT"]], [PS[pb]])
                                dve(lambda v: v.scalar_tensor_tensor(out=m_sc, in0=P[pb][:], scalar=0.125,
                                                                     in1=abias[:, kt, hh:hh + 4, :].rearrange("p h k -> p (h k)"),
                                                                     op0=ALU.mult, op1=ALU.add), [PS[pb], s_ab], [sl["sc"]])
                                if kt == 0 and first_tile:
                                    act(lambda a: a.activation(out=m_PT[:, kt, :], in_=m_sc, func=AF.Exp, bias=kvb[:, 0:1], scale=1.0),
                                        [sl["sc"], s_kvb], [sl["PT"]])
                                else:
                                    act(lambda a: a.activation(out=m_PT[:, kt, :], in_=m_sc, func=AF.Exp), [sl["sc"]], [sl["PT"]])
                            for kt in range(2):
                                pe(lambda tt: tt.matmul(P[4][:], lhsT=m_V[:, t + kt, :], rhs=m_PT[:, kt, :], start=(kt == 0), stop=(kt == 1)),
                                   [sl["V"], sl["PT"]], [PS[4]], inc=(kt == 1))
                            for kt in range(2):
                                pe(lambda tt: tt.matmul(P[5][:], lhsT=onesb[:], rhs=m_PT[:, kt, :], start=(kt == 0), stop=(kt == 1)),
                                   [s_idb, sl["PT"]], [PS[5]], inc=(kt == 1))
                            for hq in range(4):
                                dve(lambda v: v.tensor_scalar(out=m_dn[pr, hq * 128:(hq + 1) * 128], in0=P[5][pr, hq * 128:(hq + 1) * 128],
                                                              scalar1=esink[pr, hh + hq:hh + hq + 1], scalar2=None, op0=ALU.add),
                                    [PS[5], s_es], [sl["dn"]])
                            dve(lambda v: v.reciprocal(out=m_dn[pr, :], in_=m_dn[pr, :]), [sl["dn"]], [sl["dn"]])
                            dve(lambda v: v.tensor_tensor(out=m_attnT[pr, half * 4:half * 4 + 4, qc], in0=P[4][pr, :].rearrange("p (h k) -> p h k", k=128),
                                                          in1=m_dn[pr, :].rearrange("p (h k) -> p h k", k=128), op=ALU.mult),
                                [PS[4], sl["dn"]], [sl["attnT"]])

                for t in tiles:
                    qc = slice(t * 128, (t + 1) * 128)
                    mlstm_gates(gate_sb[:, t, :], s_sm, 7, None)
                    for h in range(4):
                        dve(lambda v: v.tensor_scalar(out=m_rE[:, h, :], in0=idf, scalar1=sm[:, 20 + h:21 + h], scalar2=None, op0=ALU.mult),
                            [s_c, s_sm], [sl["rE"]])
                    pe(lambda tt: tt.matmul(P[3][:], lhsT=onesf, rhs=m_rE[:].rearrange("p h k -> p (h k)"), start=True, stop=True),
                       [s_c, sl["rE"]], [PS[3]])
                    act(lambda a: a.activation(out=m_eb[:].rearrange("p h k -> p (h k)"), in_=P[3][:], func=AF.Exp, scale=-1.0), [PS[3]], [sl["eb"]])
                    for h in range(4):
                        act(lambda a: a.activation(out=m_Dt[:, h, :], in_=P[3][:, h * 128:(h + 1) * 128], func=AF.Exp,
                                                   bias=sm[:, 24 + h:25 + h], scale=-1.0), [PS[3], s_sm], [sl["Dt"]])
                    for h in range(4):
                        dve(lambda v: v.tensor_tensor(out=m_Dt[:, h, :], in0=m_Dt[:, h, :], in1=tri, op=ALU.mult), [sl["Dt"], s_c], [sl["Dt"]])
                    for h in range(4):
                        pe(lambda tt: tt.matmul(P[1][:, h * 128:(h + 1) * 128], lhsT=m_mkT[:, h, qc], rhs=m_mqT[:, h, qc], start=True, stop=True),
                           [sl["mkT"], sl["mqT"]], [PS[1]], inc=(h == 3))
                    dve(lambda v: v.tensor_tensor(out=m_Sq[:].rearrange("p h k -> p (h k)"), in0=P[1][:],
                                                  in1=m_Dt[:].rearrange("p h k -> p (h k)"), op=ALU.mult), [PS[1], sl["Dt"]], [sl["Sq"]])
                    dve(lambda v: v.tensor_tensor(out=m_qd[:], in0=m_mqT[:, :, qc], in1=m_eb[:], op=ALU.mult), [sl["mqT"], sl["eb"]], [sl["qd"]])
                    for h in range(4):
                        pb = 4 if h < 2 else 5
                        osl = P[pb][:, (h % 2) * 256:(h % 2 + 1) * 256]
                        pe(lambda tt: tt.matmul(osl, lhsT=m_Sq[:, h, :], rhs=m_va[:, t, h, 0:256], start=True, stop=False),
                           [sl["Sq"], sl["va"]], [PS[pb]], inc=False)
                        pe(lambda tt: tt.matmul(osl, lhsT=m_qd[:, h, :], rhs=Cb[:, h, 0:256], start=False, stop=True),
                           [sl["qd"], s_Cb], [PS[pb]], inc=(h % 2 == 1))
                    for h in range(4):
                        pe(lambda tt: tt.matmul(P[7][:, 32 + h:33 + h], lhsT=m_Sq[:, h, :], rhs=m_va[:, t, h, 256:257], start=True, stop=False),
                           [sl["Sq"], sl["va"]], [PS[7]], inc=False)
                        pe(lambda tt: tt.matmul(P[7][:, 32 + h:33 + h], lhsT=m_qd[:, h, :], rhs=Cb[:, h, 256:257], start=False, stop=True),
                           [sl["qd"], s_Cb], [PS[7]], inc=(h == 3))
                    act(lambda a: a.activation(out=sm[:, 36:40], in_=P[7][:, 32:36], func=AF.Abs), [PS[7]], [s_sm])
                    dve(lambda v: v.tensor_scalar(out=sm[:, 36:40], in0=sm[:, 36:40], scalar1=1.0, scalar2=None, op0=ALU.max), [s_sm], [s_sm])
                    dve(lambda v: v.reciprocal(out=sm[:, 36:40], in_=sm[:, 36:40]), [s_sm], [s_sm])
                    for h in range(4):
                        pb = 4 if h < 2 else 5
                        act(lambda a: a.activation(out=junk[:], in_=P[pb][:, (h % 2) * 256:(h % 2 + 1) * 256], func=AF.Square,
                                                   accum_out=sm[:, 40 + h:41 + h]), [PS[pb]], [s_junk, s_sm])
                    dve(lambda v: v.tensor_tensor(out=sm[:, 48:52], in0=sm[:, 40:44], in1=sm[:, 36:40], op=ALU.mult), [s_sm], [s_sm])
                    dve(lambda v: v.tensor_tensor(out=sm[:, 48:52], in0=sm[:, 48:52], in1=sm[:, 36:40], op=ALU.mult), [s_sm], [s_sm])
                    act(lambda a: a.activation(out=sm[:, 48:52], in_=sm[:, 48:52], func=AF.Sqrt, bias=1e-6, scale=1.0 / 256.0), [s_sm], [s_sm])
                    dve(lambda v: v.reciprocal(out=sm[:, 48:52], in_=sm[:, 48:52]), [s_sm], [s_sm])
                    dve(lambda v: v.tensor_tensor(out=sm[:, 44:48], in0=sm[:, 48:52], in1=sm[:, 36:40], op=ALU.mult), [s_sm], [s_sm])
                    for h in range(4):
                        pb = 4 if h < 2 else 5
                        dve(lambda v: v.scalar_tensor_tensor(out=m_mh[:, h * 256:(h + 1) * 256], in0=P[pb][:, (h % 2) * 256:(h % 2 + 1) * 256],
                                                             scalar=sm[:, 44 + h:45 + h], in1=m_gs[:, t, h * 256:(h + 1) * 256],
                                                             op0=ALU.mult, op1=ALU.mult), [PS[pb], s_sm, sl["gs"]], [sl["mh"]])
                    transpose_to(m_mh, sl["mh"], lambda c0, n: m_mhT[:, c0:c0 + n, qc], sl["mhT"], nchunks=8, pb=0)
                    pbv = P[0][:].bitcast(BF16)
                    for h in range(4):
                        pe(lambda tt: tt.transpose(out=pbv[:, h * 128:(h + 1) * 128], in_=m_mkT[:, h, qc], identity=idb[:]),
                           [sl["mkT"], s_idb], [PS[0]], inc=(h == 3))
                    mlstm_update(pbv[:, 0:512].rearrange("p (h k) -> p h k", k=128), PS[0], m_kw, sl["kw"], m_va[:, t], sl["va"], 4, 5, 7)
                act(lambda a: a.copy(out=kT_halo, in_=m_kT[:, 4, :]), [sl["kT"]], [s_halo])
                act(lambda a: a.copy(out=V_halo, in_=m_V[:, 4, :]), [sl["V"]], [s_halo])

                Sched.alias([sl["qT"], sl["mqT"], sl["mkT"]], [sl["mgT"]])
                for mp in range(8):
                    wga, s1 = wload(wcols(w_in, C_GA + mp * 256, 256), "")
                    wgb, s2 = wload(wcols(w_in, C_GB + mp * 256, 256), "")
                    i = ring["i"] % NRM; ring["i"] += 1
                    s3 = ring["slots"][i]
                    wa = ring["aps"][i][:, 0:2048].rearrange("p (j n) -> p j n", n=256)
                    for gg in range(2):
                        S.dma("pool", wa[gg * 64:(gg + 1) * 64], wba4[gg][:, :, mp * 256:(mp + 1) * 256], writes=[s3], dslot=s3)
                    wb, s4 = wload(wbm3[:, :, mp * 256:(mp + 1) * 256], "")
                    for mm in range(2):
                        m = mp * 2 + mm
                        ms = slice(mm * 128, (mm + 1) * 128)
                        b1, b2, b3, b4 = (1, 2, 3, 6) if m % 2 == 0 else (4, 5, 7, 0)
                        for c in range(NCH):
                            pe(lambda tt: tt.matmul(P[b1][:], lhsT=wga[:, c, ms], rhs=xT[:, c, :], start=(c == 0), stop=(c == NCH - 1)),
                               [s1, s_xT], [PS[b1]], inc=(c == NCH - 1))
                        for c in range(NCH):
                            pe(lambda tt: tt.matmul(P[b2][:], lhsT=wgb[:, c, ms], rhs=xT[:, c, :], start=(c == 0), stop=(c == NCH - 1)),
                               [s2, s_xT], [PS[b2]], inc=(c == NCH - 1))
                        for c in range(8):
                            pe(lambda tt: tt.matmul(P[b3][:], lhsT=wa[:, c, ms], rhs=m_attnT[:, c, :], start=(c == 0), stop=(c == 7)),
                               [s3, sl["attnT"]], [PS[b3]], inc=(c == 7))
                        for c in range(8):
                            pe(lambda tt: tt.matmul(P[b4][:], lhsT=wb[:, c, ms], rhs=m_mhT[:, c, :], start=(c == 0), stop=(c == 7)),
                               [s4, sl["mhT"]], [PS[b4]], inc=(c == 7))
                        act(lambda a: a.activation(out=m_t1, in_=P[b1][:], func=AF.Sigmoid), [PS[b1]], [sl["t1"]])
                        act(lambda a: a.activation(out=m_t2, in_=P[b2][:], func=AF.Sigmoid), [PS[b2]], [sl["t2"]])
                        dve(lambda v: v.tensor_tensor(out=m_t1, in0=m_t1, in1=P[b3][:], op=ALU.mult), [sl["t1"], PS[b3]], [sl["t1"]])
                        dve(lambda v: v.tensor_tensor(out=m_t2, in0=m_t2, in1=P[b4][:], op=ALU.mult), [sl["t2"], PS[b4]], [sl["t2"]])
                        dve(lambda v: v.tensor_tensor(out=m_mgT[:, m, :], in0=m_t1, in1=m_t2, op=ALU.add), [sl["t1"], sl["t2"]], [sl["mgT"]])
                for nb in range(8):
                    wt, wsl = wload(wcols(w_out, nb * 256, 256), "")
                    for t in tiles:
                        pb = nbank()
                        for c in range(NCH):
                            pe(lambda tt: tt.matmul(P[pb][:, 0:256], lhsT=m_mgT[:, c, t * 128:(t + 1) * 128], rhs=wt[:, c, :], start=(c == 0), stop=(c == NCH - 1)),
                               [wsl, sl["mgT"]], [PS[pb]], inc=(c == NCH - 1))
                        dve(lambda v: v.scalar_tensor_tensor(out=R[:, t, nb * 256:(nb + 1) * 256], in0=P[pb][:, 0:256], scalar=IALPHA,
                                                             in1=R[:, t, nb * 256:(nb + 1) * 256], op0=ALU.mult, op1=ALU.add),
                            [PS[pb], s_R[t]], [s_R[t]])
                load_ln(2)
                if SPARSE:
                    X1tok = ws(38176, 4 * D).rearrange("p (t k) -> p t k", k=D)
                    s_x1t = Slot("x1t")
                    Sched.alias([sl["attnT"], sl["mhT"]], [s_x1t])
                for t in tiles:
                    if SPARSE:
                        layer_norm(t, EPS_A, X1tok[:, t, :], s_x1t)
                        transpose_to(X1tok[:, t, :], s_x1t, lambda c0, n: xT[:, c0:c0 + n, t * 128:(t + 1) * 128], s_xT, pb=0)
                    else:
                        layer_norm(t, EPS_A, m_xb, sl["xb"])
                        transpose_to(m_xb, sl["xb"], lambda c0, n: xT[:, c0:c0 + n, t * 128:(t + 1) * 128], s_xT, pb=0)

                if SPARSE:
                    o = 46368
                    x_mskf = take(256, F32).rearrange("p (t k) -> p t k", k=64)
                    x_rank = take(256, F32).rearrange("p (t k) -> p t k", k=64)
                    x_mskb = take(256).rearrange("p (t k) -> p t k", k=64)
                    s_msk = Slot("msk")
                    Sched.alias([sl["xb"]], [s_msk])
                for t in range(4):
                    pb = nbank()
                    for c in range(NCH):
                        pe(lambda tt: tt.matmul(P[pb][:, 0:NE], lhsT=xT[:, c, t * 128:(t + 1) * 128], rhs=wrt[:, c, :], start=(c == 0), stop=(c == NCH - 1)),
                           [s_xT, s_wrt], [PS[pb]], inc=(c == NCH - 1))
                    sc_ = m_sc[:, 0:64]; sel_ = m_sc[:, 64:128]; top_ = m_sc[:, 128:136]; msk_ = m_sc[:, 192:256]
                    act(lambda a: a.activation(out=sc_, in_=P[pb][:, 0:NE], func=AF.Sigmoid), [PS[pb]], [sl["sc"]])
                    dve(lambda v: v.tensor_tensor(out=sel_, in0=sc_, in1=brB[:], op=ALU.add), [sl["sc"], s_br], [sl["sc"]])
                    dve(lambda v: v.max(out=top_, in_=sel_), [sl["sc"]], [sl["sc"]])
                    dve(lambda v: v.tensor_scalar(out=msk_, in0=sel_, scalar1=top_[:, 7:8], scalar2=None, op0=ALU.is_ge), [sl["sc"]], [sl["sc"]])
                    if SPARSE:
                        dve(lambda v: v.tensor_copy(out=x_mskf[:, t, :], in_=msk_), [sl["sc"]], [s_msk])
                        dve(lambda v: v.tensor_copy(out=x_mskb[:, t, :], in_=msk_), [sl["sc"]], [s_msk])
                    dve(lambda v: v.tensor_tensor(out=msk_, in0=msk_, in1=sc_, op=ALU.mult), [sl["sc"]], [sl["sc"]])
                    dve(lambda v: v.reduce_sum(out=sm[:, 150:151], in_=msk_, axis=AX.X), [sl["sc"]], [s_sm])
                    dve(lambda v: v.reciprocal(out=sm[:, 150:151], in_=sm[:, 150:151]), [s_sm], [s_sm])
                    dve(lambda v: v.tensor_scalar(out=gw[:, t, 0:NE], in0=msk_, scalar1=sm[:, 150:151], scalar2=2.5 * IALPHA,
                                                  op0=ALU.mult, op1=ALU.mult), [sl["sc"], s_sm], [s_gw])
                if SPARSE:
                    for t in range(4):
                        pb = nbank()
                        seq = [(stri[:], x_mskb[:, t, :])] + [(onesb[:], x_mskb[:, tp, :]) for tp in range(t)]
                        for i, (lt, rh) in enumerate(seq):
                            pe(lambda tt: tt.matmul(P[pb][:, 0:NE], lhsT=lt, rhs=rh, start=(i == 0), stop=(i == len(seq) - 1)),
                               [s_idb, s_msk], [PS[pb]], inc=(i == len(seq) - 1))
                        dve(lambda v: v.tensor_copy(out=x_rank[:, t, :], in_=P[pb][:, 0:NE]), [PS[pb]], [s_msk])
                    pb = nbank()
                    for t in range(4):
                        pe(lambda tt: tt.matmul(P[pb][:, 0:NE], lhsT=onesb[:], rhs=x_mskb[:, t, :], start=(t == 0), stop=(t == 3)),
                           [s_idb, s_msk], [PS[pb]], inc=(t == 3))
                    dve(lambda v: v.tensor_reduce(out=sm[:, 152:153], in_=P[pb][:, 0:NE], axis=AX.X, op=ALU.max), [PS[pb]], [s_sm])
                    dve(lambda v: v.tensor_tensor(out=flagmax[:], in0=flagmax[:], in1=sm[:, 152:153], op=ALU.max), [s_sm, s_flag], [s_flag])

                wq = "pool"

                def eload(e):
                    if PRECAST:
                        wg3 = wbg[e].rearrange("(c p) n -> p c n", p=128)
                        wu3 = wbu[e].rearrange("(c p) n -> p c n", p=128)
                        wd3 = wbd[e].rearrange("(c p) n -> p c n", p=128)
                        xr = [s_pc[e // 8]]
                    else:
                        wg3 = w_eg[e].rearrange("(c p) n -> p c n", p=128)
                        wu3 = w_eu[e].rearrange("(c p) n -> p c n", p=128)
                        wd3 = w_ed[e].rearrange("(c p) n -> p c n", p=128)
                        xr = []
                    wgs = [wload(wg3[:, :, hp * 256:(hp + 1) * 256], "", q=wq, xr=xr) for hp in range(2)]
                    wus = [wload(wu3[:, :, hp * 256:(hp + 1) * 256], "", q=wq, xr=xr) for hp in range(2)]
                    wds = [wload(wd3[:, hp * 2:hp * 2 + 2, :], "", q=wq, xr=xr) for hp in range(2)]
                    return wgs, wus, wds

                def dense_expert(e, e_h1, e_sg, s_h1, s_sg):
                    wgs, wus, wds = eload(e)
                    for m in range(4):
                        pg_, pu_ = (0, 1) if m % 2 == 0 else (2, 3)
                        wg_, sg_w = wgs[m // 2]; wu_, su_w = wus[m // 2]
                        ms = slice((m % 2) * 128, (m % 2 + 1) * 128)
                        for c in range(NCH):
                            pe(lambda tt: tt.matmul(P[pg_][:], lhsT=wg_[:, c, ms], rhs=xT[:, c, :], start=(c == 0), stop=(c == NCH - 1)),
                               [sg_w, s_xT], [PS[pg_]], inc=(c == NCH - 1))
                        for c in range(NCH):
                            pe(lambda tt: tt.matmul(P[pu_][:], lhsT=wu_[:, c, ms], rhs=xT[:, c, :], start=(c == 0), stop=(c == NCH - 1)),
                               [su_w, s_xT], [PS[pu_]], inc=(c == NCH - 1))
                        act(lambda a: a.activation(out=e_sg, in_=P[pg_][:], func=AF.Silu), [PS[pg_]], [s_sg])
                        dve(lambda v: v.tensor_tensor(out=e_h1[:, m, :], in0=e_sg, in1=P[pu_][:], op=ALU.mult), [s_sg, PS[pu_]], [s_h1])
                    for t in range(4):
                        for nb in range(4):
                            for m in range(4):
                                wd_, sd_w = wds[m // 2]
                                pe(lambda tt: tt.matmul(P[4 + nb][:], lhsT=e_h1[:, m, t * 128:(t + 1) * 128], rhs=wd_[:, m % 2, nb * 512:(nb + 1) * 512],
                                                        start=(m == 0), stop=(m == 3)), [s_h1, sd_w], [PS[4 + nb]], inc=(m == 3))
                            dve(lambda v: v.scalar_tensor_tensor(out=R[:, t, nb * 512:(nb + 1) * 512], in0=P[4 + nb][:], scalar=gw[:, t, e:e + 1],
                                                                 in1=R[:, t, nb * 512:(nb + 1) * 512], op0=ALU.mult, op1=ALU.add),
                                [PS[4 + nb], s_gw, s_R[t]], [s_R[t]])

                if not SPARSE:
                    o = NRE * RING_E
                    e_h1 = take(4 * 512).rearrange("p (m k) -> p m k", k=512)
                    e_sg = take(512, F32)
                    assert o <= HALO
                    s_h1 = Slot("h1"); s_sg = Slot("sg")
                    ring_setup(NRE)
                    moe_slots = [s_h1, s_sg] + ring["slots"]
                    Sched.alias(list(sl.values()) + ring_sl[:NRM], moe_slots)
                    for e in range(NE + 1):
                        dense_expert(e, e_h1, e_sg, s_h1, s_sg)
                    ple_olds = [s_h1, s_sg]
                    PLE_O = NRE * RING_E
                else:
                    NRS = 8
                    ring_setup(NRS)
                    o = NRS * RING_E
                    x_Sel = [take(512).rearrange("p (t k) -> p t k", k=128) for _ in range(2)]
                    x_SelT = take(512).rearrange("p (t k) -> p t k", k=128)
                    x_h1 = take(512); x_h1T = take(512).rearrange("p (m k) -> p m k", k=128)
                    x_yb = take(2048)
                    assert o <= 38176, o
                    o = 49440
                    oB = o
                    x_XeT = [take(2048).rearrange("p (c k) -> p c k", k=128) for _ in range(2)]
                    x_sg = take(512, F32)
                    assert o <= HALO, o
                    e_h1 = ws(oB, 2048).rearrange("p (m k) -> p m k", k=512)
                    e_sg = ws(oB + 2048, 512, F32)
                    s_h1 = Slot("h1"); s_sg = Slot("sg")
                    s_Sel = [Slot(), Slot()]; s_SelT = Slot(); s_xh1 = Slot(); s_xh1T = Slot(); s_yb = Slot()
                    s_XeT = [Slot(), Slot()]; s_xsg = Slot()
                    first = [s_h1, s_sg] + s_Sel + [s_SelT, s_xh1, s_xh1T, s_yb] + ring["slots"]
                    Sched.alias([v for k, v in sl.items() if k not in ("attnT", "mhT", "xb")] + ring_sl[:NRM], first)
                    dense_expert(NE, e_h1, e_sg, s_h1, s_sg)
                    Sched.alias([s_h1, s_sg], s_XeT + [s_xsg])
                    moe_slots = first + s_XeT + [s_xsg, s_x1t, s_msk]

                    def xSel(e):
                        b = e % 2
                        for t in range(4):
                            dve(lambda v: v.tensor_scalar(out=x_Sel[b][:, t, :], in0=iotaf, scalar1=x_rank[:, t, e:e + 1], scalar2=x_mskf[:, t, e:e + 1],
                                                          op0=ALU.is_equal, op1=ALU.mult), [s_c, s_msk], [s_Sel[b]])

                    def xGather(e):
                        b = e % 2
                        for g4 in range(4):
                            pb = g4 % 2
                            for cc in range(4):
                                c = g4 * 4 + cc
                                for t in range(4):
                                    pe(lambda tt: tt.matmul(P[pb][:, cc * 128:(cc + 1) * 128], lhsT=X1tok[:, t, c * 128:(c + 1) * 128], rhs=x_Sel[b][:, t, :],
                                                            start=(t == 0), stop=(t == 3)), [s_x1t, s_Sel[b]], [PS[pb]], inc=(cc == 3 and t == 3))
                            act(lambda a: a.copy(out=x_XeT[b][:, g4 * 4:(g4 + 1) * 4, :], in_=P[pb][:].rearrange("p (c k) -> p c k", k=128)),
                                [PS[pb]], [s_XeT[b]])

                    def xFFN(e, wgs, wus):
                        b = e % 2
                        for (bank, ws_) in ((2, wgs), (3, wus)):
                            for hp in range(2):
                                w_, wsl_ = ws_[hp]
                                for c in range(NCH):
                                    pe(lambda tt: tt.matmul(P[bank][:, hp * 256:(hp + 1) * 256], lhsT=x_XeT[b][:, c, :], rhs=w_[:, c, :],
                                                            start=(c == 0), stop=(c == NCH - 1)), [s_XeT[b], wsl_], [PS[bank]], inc=(c == NCH - 1))
                        act(lambda a: a.activation(out=x_sg, in_=P[2][:], func=AF.Silu), [PS[2]], [s_xsg])
                        dve(lambda v: v.tensor_tensor(out=x_h1, in0=x_sg, in1=P[3][:], op=ALU.mult), [s_xsg, PS[3]], [s_xh1])

                    def xT1(e):
                        transpose_to(x_h1, s_xh1, lambda c0, n: x_h1T[:, c0:c0 + n, :], s_xh1T, nchunks=4, pb=4)

                    def xY(e, wds):
                        for nb in range(4):
                            pb = 5 + nb % 2
                            for m in range(4):
                                wd_, sd_w = wds[m // 2]
                                pe(lambda tt: tt.matmul(P[pb][:], lhsT=x_h1T[:, m, :], rhs=wd_[:, m % 2, nb * 512:(nb + 1) * 512], start=(m == 0), stop=(m == 3)),
                                   [s_xh1T, sd_w], [PS[pb]], inc=(m == 3))
                            act(lambda a: a.copy(out=x_yb[:, nb * 512:(nb + 1) * 512], in_=P[pb][:]), [PS[pb]], [s_yb])

                    def xT2(e):
                        b = e % 2
                        transpose_to(x_Sel[b][:].rearrange("p t k -> p (t k)"), s_Sel[b], lambda c0, n: x_SelT[:, c0:c0 + n, :], s_SelT, nchunks=4, pb=4)

                    def xScatter(e):
                        i = 0
                        for t in range(4):
                            for nb in range(4):
                                pb = (7, 5, 6, 4)[i % 4]; i += 1
                                pe(lambda tt: tt.matmul(P[pb][:], lhsT=x_SelT[:, t, :], rhs=x_yb[:, nb * 512:(nb + 1) * 512], start=True, stop=True),
                                   [s_SelT, s_yb], [PS[pb]])
                                dve(lambda v: v.scalar_tensor_tensor(out=R[:, t, nb * 512:(nb + 1) * 512], in0=P[pb][:], scalar=gw[:, t, e:e + 1],
                                                                     in1=R[:, t, nb * 512:(nb + 1) * 512], op0=ALU.mult, op1=ALU.add),
                                    [PS[pb], s_gw, s_R[t]], [s_R[t]])

                    NEX = NE
                    xSel(0); xGather(0)
                    wcur = eload(0)
                    for e in range(NEX):
                        if e + 1 < NE:
                            xSel(e + 1)
                        xFFN(e, wcur[0], wcur[1])
                        wds = wcur[2]
                        if e + 1 < NE:
                            xGather(e + 1)
                        xT1(e)
                        xY(e, wds)
                        if e + 1 < NE:
                            wcur = eload(e + 1)
                        xT2(e)
                        xScatter(e)
                    ple_olds = [s_Sel[0], s_Sel[1], s_SelT, s_xh1, s_xh1T, s_yb]
                    PLE_O = NRS * RING_E
                o = PLE_O
                p_xb2 = take(D); p_pb = take(256); p_pT = take(2 * 512).rearrange("p (c k) -> p c k", k=512)
                p_pf = take(256, F32); p_sg = take(256, F32)
                assert o <= (38176 if SPARSE else HALO), o
                s_xb2 = Slot(); s_pb = Slot(); s_pT = Slot(); s_psg = Slot()
                Sched.alias(ple_olds, [s_xb2, s_pb, s_pT, s_pf, s_psg])
                moe_slots = moe_slots + [s_xb2, s_pb, s_pT, s_pf, s_psg]
                load_ln(4)
                for t in range(4):
                    layer_norm(t, EPS_A, p_xb2, s_xb2)
                    transpose_to(p_xb2, s_xb2, lambda c0, n: xT[:, c0:c0 + n, t * 128:(t + 1) * 128], s_xT, pb=0)
                    S.dma("sp", p_pf, pin[(blk * 4 + t) * 128:(blk * 4 + t + 1) * 128, :], writes=[s_pf], dslot=s_pf)
                    act(lambda a: a.copy(out=p_pb, in_=p_pf), [s_pf], [s_pb])
                    transpose_to(p_pb, s_pb, lambda c0, n: p_pT[:, c0:c0 + n, t * 128:(t + 1) * 128], s_pT, nchunks=2, pb=0)
                wpp3 = w_pp.rearrange("(c p) n -> p c n", p=128)
                for nb in range(8):
                    wt, wsl = wload(wcols(w_pg, nb * 256, 256), "")
                    wp, wpl = wload(wpp3[:, :, nb * 256:(nb + 1) * 256], "")
                    for t in range(4):
                        b1, b2 = ((1, 2), (3, 6), (4, 5), (7, 0))[t]
                        for c in range(NCH):
                            pe(lambda tt: tt.matmul(P[b1][:, 0:256], lhsT=xT[:, c, t * 128:(t + 1) * 128], rhs=wt[:, c, :], start=(c == 0), stop=(c == NCH - 1)),
                               [wsl, s_xT], [PS[b1]], inc=(c == NCH - 1))
                        for c in range(2):
                            pe(lambda tt: tt.matmul(P[b2][:, 0:256], lhsT=p_pT[:, c, t * 128:(t + 1) * 128], rhs=wp[:, c, :], start=(c == 0), stop=(c == 1)),
                               [wpl, s_pT], [PS[b2]], inc=(c == 1))
                        act(lambda a: a.activation(out=p_sg, in_=P[b1][:, 0:256], func=AF.Sigmoid), [PS[b1]], [s_psg])
                        dve(lambda v: v.scalar_tensor_tensor(out=p_sg, in0=P[b2][:, 0:256], scalar=IALPHA, in1=p_sg, op0=ALU.mult, op1=ALU.mult),
                            [PS[b2], s_psg], [s_psg])
                        dve(lambda v: v.tensor_tensor(out=R[:, t, nb * 256:(nb + 1) * 256], in0=R[:, t, nb * 256:(nb + 1) * 256], in1=p_sg, op=ALU.add),
                            [s_psg, s_R[t]], [s_R[t]])
                load_ln(6)
                for t in range(4):
                    layer_norm(t, EPS_A, p_xb2, s_xb2)
                    S.dma("sp", out[(blk * 4 + t) * 128:(blk * 4 + t + 1) * 128, :], R[:, t, :], reads=[s_R[t]], dslot=s_xl[t])
            S.dma("sp", flag_o, flagmax[:], reads=[s_flag], dslot=s_flag)
            S.wait_all("sp", s_R + [s_flag])
        print("built: ops", S.nops, "waits", S.nwaits, flush=True)
    return nc


def _consts():
    cst = np.zeros((128, 4, 128), np.float32)
    cst[:, 3, :] = np.arange(128, dtype=np.float32)[None, :]
    cst[:, 0, :] = np.eye(128, dtype=np.float32)
    cst[:, 1, :] = np.triu(np.ones((128, 128), np.float32))
    cst[:, 2, :] = 1.0
    slopes = np.exp2(-8.0 / 16 * np.arange(1, 17, dtype=np.float32)).astype(np.float32)
    j = np.arange(128)[:, None]; i = np.arange(128)[None, :]
    ab = np.zeros((128, 2, 16, 128), np.float32)
    for kt in range(2):
        dist = (i - j + (128 if kt == 0 else 0)).astype(np.float32)
        ok = (dist >= 0) & (dist < 128)
        for h in range(16):
            ab[:, kt, h, :] = np.where(ok, -slopes[h] * dist, NEGBIG)
    return cst, ab


_CACHE = {}


def kernel(x, p, ln_in_g, ln_in_b, w_in, attn_sinks, mlstm_b_i, mlstm_b_f, mlstm_norm_g,
           w_branch_attn, w_branch_mlstm, w_out, ln_mix_g, ln_mix_b, w_router, b_router,
           w_exp_gate, w_exp_up, w_exp_down, w_sh_gate, w_sh_up, w_sh_down, ln_ffn_g, ln_ffn_b,
           w_ple_proj, w_ple_gate, ln_ple_g, ln_ple_b):
    f = lambda a: np.ascontiguousarray(np.asarray(a, dtype=np.float32))
    x = f(x); p = f(p)
    B, SEQ, _ = x.shape
    NQ = 8 // B
    TOK = SEQ // NQ
    NPRE = (SEQ - TOK) // 128
    def get(sparse):
        key = (TOK, NPRE, sparse)
        if key not in _CACHE:
            _CACHE[key] = build(TOK, NPRE, sparse)
        return _CACHE[key]
    nc = get(True)
    cst, ab = _consts()
    shared = {
        "cst": cst, "abias": ab,
        "lng": np.stack([f(ln_in_g), f(ln_in_b), f(ln_mix_g)[0], f(ln_mix_b)[0], f(ln_ffn_g)[0], f(ln_ffn_b)[0],
                         f(ln_ple_g)[0], f(ln_ple_b)[0]]),
        "w_in": f(w_in)[0], "sinks": f(attn_sinks).reshape(1, 16),
        "bif": np.concatenate([f(mlstm_b_i).reshape(-1), f(mlstm_b_f).reshape(-1)]).reshape(1, 8),
        "ng": f(mlstm_norm_g).reshape(1, 1024),
        "w_ba": f(w_branch_attn)[0], "w_bm": f(w_branch_mlstm)[0], "w_out": f(w_out)[0],
        "w_rt": f(w_router)[0], "b_rt": f(b_router).reshape(1, NE),
        "w_eg": np.concatenate([f(w_exp_gate)[0], f(w_sh_gate)], axis=0),
        "w_eu": np.concatenate([f(w_exp_up)[0], f(w_sh_up)], axis=0),
        "w_ed": np.concatenate([f(w_exp_down)[0], f(w_sh_down)], axis=0),
        "w_pp": f(w_ple_proj)[0], "w_pg": f(w_ple_gate)[0],
    }
    in_maps = []
    for c in range(8):
        b, j = c // NQ, c % NQ
        end = (j + 1) * TOK
        xin = np.zeros((SEQ, D), np.float32)
        xin[SEQ - end:] = x[b, :end]
        vm = np.zeros((128, NPRE + 1), np.float32)
        nvalid = (j * TOK) // 128
        if nvalid > 0:
            vm[:, NPRE - nvalid:NPRE] = 1.0
        vm[:, NPRE] = 1.0 if j > 0 else 0.0
        m = dict(shared)
        m["xin"] = xin; m["vmk"] = vm
        m["pin"] = np.ascontiguousarray(p[0, b, j * TOK:(j + 1) * TOK])
        in_maps.append(m)
    res = run_bass_kernel_spmd(nc, in_maps, core_ids=list(range(8)))
    if max(float(np.asarray(r["flag"]).max()) for r in res.results) > 128.5:
        res = run_bass_kernel_spmd(get(False), in_maps, core_ids=list(range(8)))
    outp = np.zeros((B, SEQ, D), np.float32)
    for c in range(8):
        b, j = c // NQ, c % NQ
        outp[b, j * TOK:(j + 1) * TOK] = np.asarray(res.results[c]["out"], dtype=np.float32)
    return outp
```

```python
import numpy as np
from contextlib import ExitStack
import concourse.bass as bass
import concourse.mybir as mybir
from concourse.bass_utils import run_bass_kernel_spmd

F32 = mybir.dt.float32
BF16 = mybir.dt.bfloat16
AF = mybir.ActivationFunctionType
ALU = mybir.AluOpType
AX = mybir.AxisListType

D = 2048
NCH = 16
NE = 64
ALPHA = 2.0 ** 0.25
IALPHA = 1.0 / ALPHA
EPS_A = 1e-5 / (ALPHA * ALPHA)
C_AQ, C_AK, C_AV, C_MQ, C_MK, C_MV, C_MO, C_MI, C_GA, C_GB = 0, 1024, 1152, 1280, 1792, 2304, 3328, 4352, 4360, 6408
NEGBIG = -30000.0


class Slot:
    __slots__ = ("name", "w", "r", "dsem", "dcnt")

    def __init__(self, name=""):
        self.name = name; self.w = None; self.r = {}; self.dsem = None; self.dcnt = 0


class Eng:
    def __init__(self, obj, sem):
        self.obj = obj; self.sem = sem; self.n = 0; self.waited = {}; self.own = {id(sem)}


class Sched:
    def __init__(self, nc, sems):
        self.nc = nc
        self.E = {"pe": Eng(nc.tensor, sems["pe"]), "dve": Eng(nc.vector, sems["dve"]),
                  "act": Eng(nc.scalar, sems["act"]), "pool": Eng(nc.gpsimd, sems["pool"]),
                  "sp": Eng(nc.sync, sems["sp"])}
        self.nops = 0; self.nwaits = 0

    def new_epoch(self, sems):
        for k, e in self.E.items():
            e.sem = sems[k]; e.n = 0; e.own.add(id(e.sem))

    def _wait(self, e, deps):
        best = {}
        for (sem, val, raw) in deps:
            if id(sem) in e.own and not raw:
                continue
            k = id(sem)
            if e.waited.get(k, 0) >= val:
                continue
            if k not in best or best[k][1] < val:
                best[k] = (sem, val)
        for k, (sem, val) in best.items():
            e.obj.wait_ge(sem, val); e.waited[k] = val; self.nwaits += 1

    def _deps(self, reads, writes):
        deps = []
        for s in reads:
            if s.w is not None:
                deps.append((s.w[0], s.w[1], True))
        for s in writes:
            if s.w is not None:
                deps.append((s.w[0], s.w[1], False))
            deps.extend((a, b, False) for (a, b) in s.r.values())
        return deps

    def op(self, eng, fn, reads=(), writes=(), inc=True):
        e = self.E[eng]
        self._wait(e, self._deps(reads, writes))
        inst = fn(e.obj)
        ev = (e.sem, e.n + 1)
        if inc:
            inst.then_inc(e.sem, 1); e.n += 1
        for s in reads:
            s.r[id(e.sem)] = ev
        for s in writes:
            s.w = ev; s.r = {}
        self.nops += 1
        return ev

    def dma(self, eng, out, in_, reads=(), writes=(), dslot=None):
        e = self.E[eng]
        waw = {(id(w.w[0]), w.w[1]) for w in writes if w.w is not None and w.w[0] is dslot.dsem}
        self._wait(e, [d for d in self._deps(reads, writes) if (id(d[0]), d[1]) not in waw])
        dslot.dcnt += 1
        e.obj.dma_start(out=out, in_=in_).then_inc(dslot.dsem, 16)
        ev = (dslot.dsem, 16 * dslot.dcnt)
        for s in reads:
            s.r[id(dslot.dsem)] = ev
        for s in writes:
            s.w = ev; s.r = {}
        return ev

    def wait_all(self, eng, slots):
        e = self.E[eng]
        deps = []
        for s in slots:
            if s.w is not None:
                deps.append((s.w[0], s.w[1], True))
            deps.extend((a, b, True) for (a, b) in s.r.values())
        self._wait(e, deps)

    @staticmethod
    def alias(olds, news):
        m = {}
        for s in olds:
            evs = list(s.r.values()) + ([s.w] if s.w is not None else [])
            for (sem, val) in evs:
                k = id(sem)
                if k not in m or m[k][1] < val:
                    m[k] = (sem, val)
        for s in news:
            s.w = None; s.r = dict(m)


def build(TOK, NPRE, SPARSE=True):
    NT = TOK // 128
    NBLK = TOK // 512
    NTT = NPRE + NT
    nc = bass.Bass("TRN2", target_bir_lowering=False)

    def din(name, shape, dt=F32):
        return nc.dram_tensor(name, list(shape), dt, kind="ExternalInput").ap()

    xin = din("xin", [NTT * 128, D])
    pin = din("pin", [TOK, 256])
    vmk = din("vmk", [128, NPRE + 1])
    cst = din("cst", [128, 4, 128])
    abias_d = din("abias", [128, 2, 16, 128])
    lng = din("lng", [8, D])
    w_in = din("w_in", [D, 8456])
    sinks = din("sinks", [1, 16])
    bif_d = din("bif", [1, 8])
    ng_d = din("ng", [1, 1024])
    w_ba = din("w_ba", [1024, D])
    w_bm = din("w_bm", [1024, D])
    w_out = din("w_out", [D, D])
    w_rt = din("w_rt", [D, NE])
    b_rt = din("b_rt", [1, NE])
    w_eg = din("w_eg", [NE + 1, D, 512])
    w_eu = din("w_eu", [NE + 1, D, 512])
    w_ed = din("w_ed", [NE + 1, 512, D])
    w_pp = din("w_pp", [256, D])
    w_pg = din("w_pg", [D, D])
    out = nc.dram_tensor("out", [TOK, D], F32, kind="ExternalOutput").ap()
    flag_o = nc.dram_tensor("flag", [128, 1], F32, kind="ExternalOutput").ap()
    PRECAST = SPARSE
    if PRECAST:
        wbg = nc.dram_tensor("wbg", [NE + 1, D, 512], BF16, kind="Internal").ap()
        wbu = nc.dram_tensor("wbu", [NE + 1, D, 512], BF16, kind="Internal").ap()
        wbd = nc.dram_tensor("wbd", [NE + 1, 512, D], BF16, kind="Internal").ap()

    with ExitStack() as es:
        def sb(name, shape, dt):
            return es.enter_context(nc.sbuf_tensor(name, list(shape), dt))

        def sem(name):
            return es.enter_context(nc.semaphore(name))

        nsem = [0]

        def engsems():
            nsem[0] += 1
            return {k: sem("e%d_%s" % (nsem[0], k)) for k in ["pe", "dve", "act", "pool", "sp"]}

        def dslot(name):
            s = Slot(name); s.dsem = sem("d_" + name); return s

        S = Sched(nc, engsems())

        WSN = 54816
        R = sb("R", [128, 4, D], F32)
        xT = sb("xT", [128, NCH, 512], BF16)
        WS = sb("WS", [128, WSN], BF16)
        cf = sb("cf", [128, 4, 128], F32)
        idb = sb("idb", [128, 128], BF16)
        onesb = sb("onesb", [128, 128], BF16)
        abias = sb("abias_s", [128, 2, 16, 128], F32)
        esink = sb("esink", [128, 16], F32)
        bif = sb("bifs", [128, 8], F32)
        ngB = sb("ngB", [128, 1024], F32)
        brB = sb("brB", [128, NE], F32)
        vm = sb("vm", [128, NPRE + 1], F32)
        kvb = sb("kvb", [128, 1], F32)
        wrt = sb("wrt", [128, NCH, NE], BF16)
        C32 = sb("C32", [128, 4, 256], F32)
        n32 = sb("n32", [128, 4], F32)
        Cb = sb("Cb", [128, 4, 258], BF16)
        gw = sb("gw", [128, 4, NE + 1], F32)
        sm = sb("sm", [128, 160], F32)
        st6 = sb("st6", [128, 4, 6], F32)
        junk = sb("junk", [128, 256], BF16)
        stri = sb("stri", [128, 128], BF16)
        flagmax = sb("flagmax", [128, 1], F32)
        gB = sb("gB", [128, D], F32)
        bB = sb("bB", [128, D], F32)
        idf = cf[:, 0, :]; tri = cf[:, 1, :]; onesf = cf[:, 2, :]; iotaf = cf[:, 3, :]

        P = [es.enter_context(nc.psum_tensor("ps%d" % i, [128, 512], F32)) for i in range(8)]
        PS = [Slot("ps%d" % i) for i in range(8)]

        s_R = [Slot("R%d" % i) for i in range(4)]
        s_xT = Slot("xT")
        s_c = dslot("c"); s_ab = dslot("ab"); s_es = dslot("es"); s_bif = dslot("bif"); s_ng = dslot("ng")
        s_br = dslot("br"); s_vm = dslot("vm"); s_wrt = dslot("wrt"); s_gB = dslot("gB"); s_bB = dslot("bB")
        s_idb = Slot("idb"); s_C = Slot("C"); s_Cb = Slot("Cb"); s_gw = Slot("gw"); s_sm = Slot("sm")
        s_st6 = Slot("st"); s_junk = Slot("junk"); s_kvb = Slot("kvb")
        s_xl = [dslot("xl%d" % i) for i in range(4)]

        def ws(off, n, dt=BF16):
            if dt == F32:
                return WS[:, off:off + 2 * n].bitcast(F32)
            return WS[:, off:off + n]

        block = es.enter_context(nc.Block())

        @block.sync
        def _(sync):
            dve = lambda fn, r=(), w=(): S.op("dve", fn, r, w)
            act = lambda fn, r=(), w=(): S.op("act", fn, r, w)
            pool = lambda fn, r=(), w=(): S.op("pool", fn, r, w)
            pe = lambda fn, r=(), w=(), inc=True: S.op("pe", fn, r, w, inc)

            S.dma("sp", cf[:], cst, writes=[s_c], dslot=s_c)
            S.dma("sp", abias[:], abias_d, writes=[s_ab], dslot=s_ab)
            S.dma("sp", esink[:], sinks.partition_broadcast(128).rearrange("p a b -> p (a b)"), writes=[s_es], dslot=s_es)
            S.dma("sp", bif[:], bif_d.partition_broadcast(128).rearrange("p a b -> p (a b)"), writes=[s_bif], dslot=s_bif)
            S.dma("sp", ngB[:], ng_d.partition_broadcast(128).rearrange("p a b -> p (a b)"), writes=[s_ng], dslot=s_ng)
            S.dma("sp", brB[:], b_rt.partition_broadcast(128).rearrange("p a b -> p (a b)"), writes=[s_br], dslot=s_br)
            S.dma("sp", vm[:], vmk, writes=[s_vm], dslot=s_vm)
            S.dma("pool", wrt[:], w_rt.rearrange("(c p) n -> p c n", p=128), writes=[s_wrt], dslot=s_wrt)
            dve(lambda v: v.tensor_copy(out=idb[:], in_=idf), [s_c], [s_idb])
            dve(lambda v: v.tensor_copy(out=onesb[:], in_=onesf), [s_c], [s_idb])
            act(lambda a: a.activation(out=esink[:], in_=esink[:], func=AF.Exp), [s_es], [s_es])
            dve(lambda v: v.tensor_scalar(out=kvb[:], in0=vm[:, NPRE:NPRE + 1], scalar1=-1.0, scalar2=-NEGBIG,
                                          op0=ALU.add, op1=ALU.mult), [s_vm], [s_kvb])
            dve(lambda v: v.memset(C32[:], 0.0), [], [s_C])
            dve(lambda v: v.memset(n32[:], 0.0), [], [s_C])
            dve(lambda v: v.memset(Cb[:], 0.0), [], [s_Cb])
            dve(lambda v: v.memset(gw[:], IALPHA), [], [s_gw])
            s_flag = dslot("flag")
            dve(lambda v: v.memset(flagmax[:], 0.0), [], [s_flag])
            dve(lambda v: v.tensor_tensor(out=stri[:], in0=tri, in1=idf, op=ALU.subtract), [s_c], [s_idb])
            s_pc = [dslot("pc%d" % i) for i in range(9)]
            pc_list = []
            pc_group = {}
            if PRECAST:
                for e in [NE] + list(range(NE)):
                    for (dst_, src_) in ((wbg, w_eg), (wbu, w_eu), (wbd, w_ed)):
                        pc_group[e] = len(pc_list) // 24
                        pc_list.append((dst_[e], src_[e], len(pc_list) // 24))
            pc_state = {"i": 0, "hist": []}

            def pc_issue(n):
                for _ in range(n):
                    if pc_state["i"] >= len(pc_list):
                        return
                    d_, s_, g_ = pc_list[pc_state["i"]]; pc_state["i"] += 1
                    hist = pc_state["hist"]
                    if len(hist) >= 12:
                        psem, pval = hist[-12]
                        nc.gpsimd.wait_ge(psem, pval)
                    hist.append(S.dma("pool", d_, s_, writes=[s_pc[g_]], dslot=s_pc[g_]))

            def load_ln(idx):
                S.dma("sp", gB[:], lng[idx].partition_broadcast(128), writes=[s_gB], dslot=s_gB)
                S.dma("sp", bB[:], lng[idx + 1].partition_broadcast(128), writes=[s_bB], dslot=s_bB)

            def layer_norm(t, eps, xb, s_xb, use_pool=False, sc0=0, s_st=None):
                s_q = s_sm if s_st is None else s_st
                rt = R[:, t, :]
                c0, c1, c2, c3 = sc0, sc0 + 1, sc0 + 2, sc0 + 3
                for i in range(4):
                    dve(lambda v: v.bn_stats(out=st6[:, i, :], in_=rt[:, i * 512:(i + 1) * 512]), [s_R[t]], [s_st6])
                dve(lambda v: v.bn_aggr(out=sm[:, c0:c0 + 2], in_=st6[:].rearrange("p a b -> p (a b)")), [s_st6], [s_q])
                act(lambda a: a.activation(out=sm[:, c2:c2 + 1], in_=sm[:, c1:c1 + 1], func=AF.Sqrt, bias=float(eps), scale=1.0), [s_q], [s_q])
                dve(lambda v: v.reciprocal(out=sm[:, c2:c2 + 1], in_=sm[:, c2:c2 + 1]), [s_q], [s_q])
                if use_pool:
                    dve(lambda v: v.tensor_scalar(out=sm[:, c3:c3 + 1], in0=sm[:, c0:c0 + 1], scalar1=sm[:, c2:c2 + 1], scalar2=-1.0,
                                                  op0=ALU.mult, op1=ALU.mult), [s_q], [s_q])
                    act(lambda a: a.activation(out=rt, in_=rt, func=AF.Identity, bias=sm[:, c3:c3 + 1], scale=sm[:, c2:c2 + 1]),
                        [s_R[t], s_q], [s_R[t]])
                    pool(lambda g: g.tensor_tensor(out=rt, in0=rt, in1=gB[:], op=ALU.mult), [s_R[t], s_gB], [s_R[t]])
                    pool(lambda g: g.tensor_tensor(out=xb, in0=rt, in1=bB[:], op=ALU.add), [s_R[t], s_bB], [s_xb])
                    return
                dve(lambda v: v.scalar_tensor_tensor(out=rt, in0=rt, scalar=sm[:, c0:c0 + 1], in1=gB[:], op0=ALU.subtract, op1=ALU.mult),
                    [s_R[t], s_q, s_gB], [s_R[t]])
                dve(lambda v: v.scalar_tensor_tensor(out=rt, in0=rt, scalar=sm[:, c2:c2 + 1], in1=bB[:], op0=ALU.mult, op1=ALU.add),
                    [s_R[t], s_q, s_bB], [s_R[t]])
                act(lambda a: a.copy(out=xb, in_=rt), [s_R[t]], [s_xb])

            def transpose_to(xb, s_xb, dst_fn, s_dst, nchunks=NCH, pb=0):
                pbv = P[pb][:].bitcast(BF16)
                for g0 in range(0, nchunks, 8):
                    n = min(8, nchunks - g0)
                    for j in range(n):
                        c = g0 + j
                        pe(lambda t: t.transpose(out=pbv[:, j * 128:(j + 1) * 128], in_=xb[:, c * 128:(c + 1) * 128], identity=idb[:]),
                           [s_xb, s_idb], [PS[pb]], inc=(j == n - 1))
                    act(lambda a: a.copy(out=dst_fn(g0, n), in_=pbv[:, 0:n * 128].rearrange("p (c k) -> p c k", k=128)),
                        [PS[pb]], [s_dst])

            ring = {"slots": [], "aps": [], "i": 0}
            RING_E = 4096

            def wload(src, shape_str, q="pool", xr=()):
                i = ring["i"] % len(ring["slots"]); ring["i"] += 1
                sl = ring["slots"][i]
                n = 1
                for d in src.shape[1:]:
                    n *= d
                dst = ring["aps"][i][0:src.shape[0], 0:n]
                if len(src.shape) == 3:
                    dst = dst.rearrange("p (a b) -> p a b", b=src.shape[2])
                S.dma(q, dst, src, reads=list(xr), writes=[sl], dslot=sl)
                return dst, sl

            def wcols(wd, c0, n):
                return wd.rearrange("(c p) n -> p c n", p=128)[:, :, c0:c0 + n]

            def mlstm_gates(gate_ps, s_gate_ps, pg, vcol, co=0, s_gs=None):
                sq = s_sm if s_gs is None else s_gs
                k = lambda a, b: sm[:, co + a:co + b]
                if gate_ps is not None:
                    dve(lambda v: v.tensor_tensor(out=k(8, 16), in0=gate_ps, in1=bif[:], op=ALU.add), [s_gate_ps, s_bif], [sq])
                act(lambda a: a.activation(out=k(8, 16), in_=k(8, 16), func=AF.Tanh, scale=1.0 / 15.0), [sq], [sq])
                act(lambda a: a.activation(out=k(16, 20), in_=k(12, 16), func=AF.Exp, scale=-15.0), [sq], [sq])
                act(lambda a: a.activation(out=k(16, 20), in_=k(16, 20), func=AF.Ln, bias=1.0, scale=1.0), [sq], [sq])
                pe(lambda t: t.matmul(P[pg][:, 0:4], lhsT=tri, rhs=k(16, 20), start=True, stop=True), [s_c, sq], [PS[pg]], inc=False)
                pe(lambda t: t.matmul(P[pg][:, 4:8], lhsT=onesf, rhs=k(16, 20), start=True, stop=True), [s_c, sq], [PS[pg]])
                dve(lambda v: v.tensor_copy(out=k(20, 24), in_=P[pg][:, 0:4]), [PS[pg]], [sq])
                dve(lambda v: v.scalar_tensor_tensor(out=k(24, 28), in0=k(8, 12), scalar=15.0, in1=k(20, 24),
                                                     op0=ALU.mult, op1=ALU.add), [sq], [sq])
                dve(lambda v: v.tensor_tensor(out=k(28, 32), in0=k(24, 28), in1=P[pg][:, 4:8], op=ALU.subtract), [sq, PS[pg]], [sq])
                act(lambda a: a.activation(out=k(28, 32), in_=k(28, 32), func=AF.Exp), [sq], [sq])
                if vcol is not None:
                    dve(lambda v: v.tensor_scalar(out=k(28, 32), in0=k(28, 32), scalar1=vm[:, vcol:vcol + 1], scalar2=None,
                                                  op0=ALU.mult), [sq, s_vm], [sq])
                act(lambda a: a.activation(out=k(32, 36), in_=P[pg][:, 4:8], func=AF.Exp, scale=-1.0), [PS[pg]], [sq])

            def mlstm_update(ktok_ps, s_ktok, kw, s_kw, vaug, s_vaug, pu0, pu1, pn):
                for h in range(4):
                    act(lambda a: a.activation(out=kw[:, h, :], in_=ktok_ps[:, h, :], func=AF.Copy, scale=sm[:, 28 + h:29 + h]),
                        [s_ktok, s_sm], [s_kw])
                for h in range(4):
                    pb = pu0 if h < 2 else pu1
                    pe(lambda t: t.matmul(P[pb][:, (h % 2) * 256:(h % 2 + 1) * 256], lhsT=kw[:, h, :], rhs=vaug[:, h, 0:256],
                                          start=True, stop=True), [s_kw, s_vaug], [PS[pb]], inc=(h % 2 == 1))
                for h in range(4):
                    pe(lambda t: t.matmul(P[pn][:, 16 + h:17 + h], lhsT=kw[:, h, :], rhs=vaug[:, h, 256:257], start=True, stop=True),
                       [s_kw, s_vaug], [PS[pn]], inc=(h == 3))
                for h in range(4):
                    pb = pu0 if h < 2 else pu1
                    dve(lambda v: v.scalar_tensor_tensor(out=C32[:, h, :], in0=C32[:, h, :], scalar=sm[:, 32 + h:33 + h],
                                                         in1=P[pb][:, (h % 2) * 256:(h % 2 + 1) * 256], op0=ALU.mult, op1=ALU.add),
                        [s_C, s_sm, PS[pb]], [s_C])
                dve(lambda v: v.tensor_tensor(out=n32[:], in0=n32[:], in1=sm[:, 32:36], op=ALU.mult), [s_C, s_sm], [s_C])
                dve(lambda v: v.tensor_tensor(out=n32[:], in0=n32[:], in1=P[pn][:, 16:20], op=ALU.add), [s_C, PS[pn]], [s_C])
                act(lambda a: a.copy(out=Cb[:, :, 0:256], in_=C32[:]), [s_C], [s_Cb])
                act(lambda a: a.copy(out=Cb[:, :, 256:257], in_=n32[:].rearrange("p (h o) -> p h o", o=1)), [s_C], [s_Cb])

            PW = 512 + 1024 + 8 + 128 + 128
            o = 0
            wpre = ws(o, NCH * PW).rearrange("p (c n) -> p c n", n=PW); o += NCH * PW
            p_xb = []; p_hT = []; p_va = []; p_kt = []
            for b in range(2):
                p_xb.append(ws(o, D)); o += D
                p_hT.append(ws(o, NCH * 128).rearrange("p (c k) -> p c k", k=128)); o += NCH * 128
                p_va.append(ws(o, 4 * 258).rearrange("p (h k) -> p h k", k=258)); o += 4 * 258
                p_kt.append(ws(o, 512).rearrange("p (h k) -> p h k", k=128)); o += 512
            p_kw = ws(o, 512).rearrange("p (h k) -> p h k", k=128); o += 512
            HALO = WSN - 256
            assert o <= HALO
            kT_halo = ws(HALO, 128); V_halo = ws(HALO + 128, 128)
            s_wpre = dslot("wpre"); s_pkw = Slot(); s_halo = Slot()
            s_pxb = [Slot(), Slot()]; s_phT = [Slot(), Slot()]; s_pva = [Slot(), Slot()]; s_pkt = [Slot(), Slot()]
            s_g = [Slot(), Slot()]
            GCO = [0, 52]

            def pA(pt):
                b = pt % 2; t = pt % 4
                S.dma("sp", R[:, t, :], xin[pt * 128:(pt + 1) * 128, :], writes=[s_R[t]], dslot=s_xl[t])
                layer_norm(t, 1e-5, p_xb[b], s_pxb[b], use_pool=True, sc0=104 + 4 * b, s_st=s_g[b])

            def pT(pt):
                b = pt % 2
                transpose_to(p_xb[b], s_pxb[b], lambda c0, n: p_hT[b][:, c0:c0 + n, :], s_phT[b], pb=0)

            def pB(pt):
                b = pt % 2; co = GCO[b]
                for (pb, po, n) in [(1, 0, 512), (2, 512, 512), (3, 1024, 512)]:
                    for c in range(NCH):
                        pe(lambda tt: tt.matmul(P[pb][:, 0:n], lhsT=p_hT[b][:, c, :], rhs=wpre[:, c, po:po + n], start=(c == 0), stop=(c == NCH - 1)),
                           [s_phT[b], s_wpre], [PS[pb]], inc=(c == NCH - 1))
                for c in range(NCH):
                    pe(lambda tt: tt.matmul(P[6][:, 0:8], lhsT=p_hT[b][:, c, :], rhs=wpre[:, c, 1536:1544], start=(c == 0), stop=(c == NCH - 1)),
                       [s_phT[b], s_wpre], [PS[6]], inc=(c == NCH - 1))
                act(lambda a: a.activation(out=p_kt[b][:].rearrange("p h k -> p (h k)"), in_=P[1][:], func=AF.Copy, scale=128.0 ** -0.5), [PS[1]], [s_pkt[b]])
                act(lambda a: a.copy(out=p_va[b][:, 0:2, 0:256], in_=P[2][:].rearrange("p (h k) -> p h k", k=256)), [PS[2]], [s_pva[b]])
                act(lambda a: a.copy(out=p_va[b][:, 2:4, 0:256], in_=P[3][:].rearrange("p (h k) -> p h k", k=256)), [PS[3]], [s_pva[b]])
                dve(lambda v: v.tensor_tensor(out=sm[:, co + 8:co + 16], in0=P[6][:, 0:8], in1=bif[:], op=ALU.add), [PS[6], s_bif], [s_g[b]])
                if pt == NPRE - 1:
                    for c in range(NCH):
                        pe(lambda tt: tt.matmul(P[1][:, 0:128], lhsT=wpre[:, c, 1544:1672], rhs=p_hT[b][:, c, :], start=(c == 0), stop=(c == NCH - 1)),
                           [s_phT[b], s_wpre], [PS[1]], inc=(c == NCH - 1))
                    for c in range(NCH):
                        pe(lambda tt: tt.matmul(P[2][:, 0:128], lhsT=p_hT[b][:, c, :], rhs=wpre[:, c, 1672:1800], start=(c == 0), stop=(c == NCH - 1)),
                           [s_phT[b], s_wpre], [PS[2]], inc=(c == NCH - 1))
                    act(lambda a: a.copy(out=kT_halo, in_=P[1][:, 0:128]), [PS[1]], [s_halo])
                    act(lambda a: a.copy(out=V_halo, in_=P[2][:, 0:128]), [PS[2]], [s_halo])

            def pC1(pt):
                b = pt % 2
                mlstm_gates(None, None, 7, pt, co=GCO[b], s_gs=s_g[b])

            def pC2(pt):
                b = pt % 2; co = GCO[b]
                for h in range(4):
                    act(lambda a: a.activation(out=p_kw[:, h, :], in_=p_kt[b][:, h, :], func=AF.Copy, scale=sm[:, co + 28 + h:co + 29 + h]),
                        [s_pkt[b], s_g[b]], [s_pkw])
                for h in range(4):
                    pb = 4 if h < 2 else 5
                    pe(lambda tt: tt.matmul(P[pb][:, (h % 2) * 256:(h % 2 + 1) * 256], lhsT=p_kw[:, h, :], rhs=p_va[b][:, h, 0:256], start=True, stop=True),
                       [s_pkw, s_pva[b]], [PS[pb]], inc=(h % 2 == 1))
                for h in range(4):
                    pe(lambda tt: tt.matmul(P[7][:, 16 + h:17 + h], lhsT=p_kw[:, h, :], rhs=p_va[b][:, h, 256:257], start=True, stop=True),
                       [s_pkw, s_pva[b]], [PS[7]], inc=(h == 3))
                for h in range(4):
                    pb = 4 if h < 2 else 5
                    dve(lambda v: v.scalar_tensor_tensor(out=C32[:, h, :], in0=C32[:, h, :], scalar=sm[:, co + 32 + h:co + 33 + h],
                                                         in1=P[pb][:, (h % 2) * 256:(h % 2 + 1) * 256], op0=ALU.mult, op1=ALU.add),
                        [s_C, s_g[b], PS[pb]], [s_C])
                dve(lambda v: v.tensor_tensor(out=n32[:], in0=n32[:], in1=sm[:, co + 32:co + 36], op=ALU.mult), [s_C, s_g[b]], [s_C])
                dve(lambda v: v.tensor_tensor(out=n32[:], in0=n32[:], in1=P[7][:, 16:20], op=ALU.add), [s_C, PS[7]], [s_C])

            if NPRE > 0:
                segs = [(C_MK, 512), (C_MV, 1024), (C_MI, 8), (C_AK, 128), (C_AV, 128)]
                po = 0
                for (c0, n) in segs:
                    S.dma("pool", wpre[:, :, po:po + n], wcols(w_in, c0, n), writes=[s_wpre], dslot=s_wpre)
                    po += n
                for b in range(2):
                    dve(lambda v: v.memset(p_va[b][:], 1.0), [], [s_pva[b]])
                load_ln(0)
                pA(0); pT(0)
                for pt in range(NPRE):
                    if pt + 1 < NPRE:
                        pA(pt + 1)
                    pc_issue(1 if pt % 2 == 0 else 2)
                    pB(pt)
                    if pt >= 1:
                        pC2(pt - 1)
                    if pt + 1 < NPRE:
                        pT(pt + 1)
                    pC1(pt)
                pC2(NPRE - 1)
                act(lambda a: a.copy(out=Cb[:, :, 0:256], in_=C32[:]), [s_C], [s_Cb])
                act(lambda a: a.copy(out=Cb[:, :, 256:257], in_=n32[:].rearrange("p (h o) -> p h o", o=1)), [s_C], [s_Cb])
            if NPRE == 0:
                dve(lambda v: v.memset(kT_halo, 0.0), [], [s_halo])
                dve(lambda v: v.memset(V_halo, 0.0), [], [s_halo])

            SB = 512
            RING_E = 4096
            NRM, NRE = 5, 12
            Sched.alias([s_sm] + s_g, [s_sm])
            ring_sl = [dslot("rg%d" % i) for i in range(NRE)]
            s_pf = dslot("pf")
            rot = {"i": 0}

            def nbank(banks=(1, 2, 6, 7)):
                rot["i"] += 1
                return banks[rot["i"] % len(banks)]

            def ring_setup(nslots):
                ring["slots"] = ring_sl[:nslots]
                ring["aps"] = [ws(i * RING_E, RING_E) for i in range(nslots)]
                ring["i"] = 0

            w3 = w_in.rearrange("(c p) n -> p c n", p=128)
            wba4 = w_ba.rearrange("(g j d) n -> g d j n", g=2, d=64)
            wbm3 = w_bm.rearrange("(c p) n -> p c n", p=128)
            moe_slots = None
            for blk in range(NBLK):
                S.new_epoch(engsems())
                o = NRM * RING_E

                def take(n, dt=BF16):
                    nonlocal o
                    v = ws(o, n, dt); o += n * (2 if dt == F32 else 1); return v
                o_mg = o
                m_qT = take(8 * SB).rearrange("p (j k) -> p j k", k=SB)
                m_mqT = take(4 * SB).rearrange("p (h k) -> p h k", k=SB)
                m_mkT = take(4 * SB).rearrange("p (h k) -> p h k", k=SB)
                m_mgT = ws(o_mg, 16 * SB).rearrange("p (c k) -> p c k", k=SB)
                m_kT = take(5 * 128).rearrange("p (t k) -> p t k", k=128)
                m_V = take(5 * 128).rearrange("p (t k) -> p t k", k=128)
                m_va = take(4 * 4 * 258).rearrange("p (t h k) -> p t h k", h=4, k=258)
                m_gs = take(4 * 1024).rearrange("p (t k) -> p t k", k=1024)
                m_attnT = take(8 * SB).rearrange("p (h k) -> p h k", k=SB)
                m_mhT = take(8 * SB).rearrange("p (c k) -> p c k", k=SB)
                m_xb = take(D)
                m_sc = take(512, F32)
                m_PT = take(2 * 512).rearrange("p (t k) -> p t k", k=512)
                m_dn = take(512, F32)
                m_eb = take(512).rearrange("p (h k) -> p h k", k=128)
                m_Sq = take(512).rearrange("p (h k) -> p h k", k=128)
                m_qd = take(512).rearrange("p (h k) -> p h k", k=128)
                m_kw = take(512).rearrange("p (h k) -> p h k", k=128)
                m_mh = take(1024)
                m_Dt = m_dn.rearrange("p (h k) -> p h k", k=128)
                m_rE = m_sc.rearrange("p (h k) -> p h k", k=128)
                m_t1 = m_sc
                m_t2 = m_PT.rearrange("p t k -> p (t k)").bitcast(F32)
                assert o <= HALO, o
                names = "xb V va gs qT kT mqT mkT attnT mhT mgT sc PT eb Sq qd kw mh dn".split()
                sl = {n: Slot(n) for n in names}
                sl["Dt"] = sl["dn"]; sl["rE"] = sl["sc"]; sl["t1"] = sl["sc"]; sl["t2"] = sl["PT"]
                ring_setup(NRM)
                mix_slots = [v for k, v in sl.items() if k != "mgT"] + ring["slots"]
                if blk == 0:
                    Sched.alias([s_wpre, s_pkw] + s_pxb + s_phT + s_pva + s_pkt, mix_slots)
                else:
                    Sched.alias(moe_slots, mix_slots)
                dve(lambda v: v.memset(m_va[:], 1.0), [], [sl["va"]])
                tiles = [0, 1, 2, 3]
                gt0 = NPRE + blk * 4
                load_ln(0)
                for t in tiles:
                    S.dma("sp", R[:, t, :], xin[(gt0 + t) * 128:(gt0 + t + 1) * 128, :], writes=[s_R[t]], dslot=s_xl[t])
                for t in tiles:
                    layer_norm(t, 1e-5, m_xb, sl["xb"])
                    transpose_to(m_xb, sl["xb"], lambda c0, n: xT[:, c0:c0 + n, t * 128:(t + 1) * 128], s_xT, pb=0)
                act(lambda a: a.copy(out=m_kT[:, 0, :], in_=kT_halo), [s_halo], [sl["kT"]])
                act(lambda a: a.copy(out=m_V[:, 0, :], in_=V_halo), [s_halo], [sl["V"]])

                def fmm(wt2, wsl, pb):
                    for c in range(NCH):
                        pe(lambda tt: tt.matmul(P[pb][:], lhsT=wt2[:, c, :], rhs=xT[:, c, :], start=(c == 0), stop=(c == NCH - 1)),
                           [wsl, s_xT], [PS[pb]], inc=(c == NCH - 1))
                for jp in range(4):
                    i = ring["i"] % NRM; ring["i"] += 1
                    wsl = ring["slots"][i]
                    wt = ring["aps"][i].rearrange("p (m c g r) -> p m c g r", m=2, g=2, r=64)
                    for mm in range(2):
                        j = jp * 2 + mm
                        src = w3[:, :, j * 64:j * 64 + 1024].rearrange("p c (g r) -> p c g r", r=512)
                        for gg in range(2):
                            S.dma("pool", wt[:, mm, :, gg, :], src[:, :, gg, 0:64], writes=[wsl], dslot=wsl)
                    wt2 = ring["aps"][i].rearrange("p (m c k) -> p m c k", m=2, k=128)
                    for mm in range(2):
                        j = jp * 2 + mm
                        pb = nbank()
                        fmm(wt2[:, mm], wsl, pb)
                        act(lambda a: a.copy(out=m_qT[:, j, :], in_=P[pb][:]), [PS[pb]], [sl["qT"]])
                wt, wsl = wload(wcols(w_in, C_AK, 128), "")
                pb = nbank(); fmm(wt, wsl, pb)
                act(lambda a: a.copy(out=m_kT[:, 1:5, :], in_=P[pb][:].rearrange("p (t k) -> p t k", k=128)), [PS[pb]], [sl["kT"]])
                for hp in range(2):
                    wt, wsl = wload(wcols(w_in, C_MQ + hp * 256, 256), "")
                    for mm in range(2):
                        h = hp * 2 + mm
                        pb = nbank(); fmm(wt[:, :, mm * 128:(mm + 1) * 128], wsl, pb)
                        act(lambda a: a.copy(out=m_mqT[:, h, :], in_=P[pb][:]), [PS[pb]], [sl["mqT"]])
                for hp in range(2):
                    wt, wsl = wload(wcols(w_in, C_MK + hp * 256, 256), "")
                    for mm in range(2):
                        h = hp * 2 + mm
                        pb = nbank(); fmm(wt[:, :, mm * 128:(mm + 1) * 128], wsl, pb)
                        act(lambda a: a.activation(out=m_mkT[:, h, :], in_=P[pb][:], func=AF.Copy, scale=128.0 ** -0.5), [PS[pb]], [sl["mkT"]])
                gate_sb = sm[:, 112:144].rearrange("p (t k) -> p t k", k=8)

                def tproj(c0, n, evac):
                    wt, wsl = wload(wcols(w_in, c0, n), "")
                    for t in tiles:
                        pb = nbank()
                        for c in range(NCH):
                            pe(lambda tt: tt.matmul(P[pb][:, 0:n], lhsT=xT[:, c, t * 128:(t + 1) * 128], rhs=wt[:, c, :], start=(c == 0), stop=(c == NCH - 1)),
                               [wsl, s_xT], [PS[pb]], inc=(c == NCH - 1))
                        evac(t, pb)
                tproj(C_AV, 128, lambda t, pb: act(lambda a: a.copy(out=m_V[:, 1 + t, :], in_=P[pb][:, 0:128]), [PS[pb]], [sl["V"]]))
                for h in range(4):
                    tproj(C_MV + h * 256, 256, lambda t, pb: act(lambda a: a.copy(out=m_va[:, t, h, 0:256], in_=P[pb][:, 0:256]), [PS[pb]], [sl["va"]]))
                for q in range(4):
                    def ev_mo(t, pb):
                        act(lambda a: a.activation(out=m_gs[:, t, q * 256:(q + 1) * 256], in_=P[pb][:, 0:256], func=AF.Sigmoid), [PS[pb]], [sl["gs"]])
                        dve(lambda v: v.tensor_tensor(out=m_gs[:, t, q * 256:(q + 1) * 256], in0=m_gs[:, t, q * 256:(q + 1) * 256],
                                                      in1=ngB[:, q * 256:(q + 1) * 256], op=ALU.mult), [sl["gs"], s_ng], [sl["gs"]])
                    tproj(C_MO + q * 256, 256, ev_mo)
                tproj(C_MI, 8, lambda t, pb: dve(lambda v: v.tensor_copy(out=gate_sb[:, t, :], in_=P[pb][:, 0:8]), [PS[pb]], [s_sm]))

                for t in tiles:
                    qc = slice(t * 128, (t + 1) * 128)
                    first_tile = (blk == 0 and t == 0)
                    for g in range(2):
                        pr = slice(g * 64, (g + 1) * 64)
                        for half in range(2):
                            hh = g * 8 + half * 4
                            for kt in range(2):
                                pb = nbank((1, 2))
                                pe(lambda tt: tt.matmul(P[pb][:], lhsT=m_kT[pr, t + kt, :], rhs=m_qT[pr, half * 4:half * 4 + 4, qc],
                                                        start=True, stop=True), [sl["kT"], sl["qT"]], [PS[pb]])
                                dve(lambda v: v.scalar_tensor_tensor(out=m_sc, in0=P[pb][:], scalar=0.125,
                                                                     in1=abias[:, kt, hh:hh + 4, :].rearrange("p h k -> p (h k)"),
                                                                     op0=ALU.mult, op1=ALU.add), [PS[pb], s_ab], [sl["sc"]])
                                if kt == 0 and first_tile:
                                    act(lambda a: a.activation(out=m_PT[:, kt, :], in_=m_sc, func=AF.Exp, bias=kvb[:, 0:1], scale=1.0),
                                        [sl["sc"], s_kvb], [sl["PT"]])
                                else:
                                    act(lambda a: a.activation(out=m_PT[:, kt, :], in_=m_sc, func=AF.Exp), [sl["sc"]], [sl["PT"]])
                            for kt in range(2):
                                pe(lambda tt: tt.matmul(P[4][:], lhsT=m_V[:, t + kt, :], rhs=m_PT[:, kt, :], start=(kt == 0), stop=(kt == 1)),
                                   [sl["V"], sl["PT"]], [PS[4]], inc=(kt == 1))
                            for kt in range(2):
                                pe(lambda tt: tt.matmul(P[5][:], lhsT=onesb[:], rhs=m_PT[:, kt, :], start=(kt == 0), stop=(kt == 1)),
                                   [s_idb, sl["PT"]], [PS[5]], inc=(kt == 1))
                            for hq in range(4):
                                dve(lambda v: v.tensor_scalar(out=m_dn[pr, hq * 128:(hq + 1) * 128], in0=P[5][pr, hq * 128:(hq + 1) * 128],
                                                              scalar1=esink[pr, hh + hq:hh + hq + 1], scalar2=None, op0=ALU.add),
                                    [PS[5], s_es], [sl["dn"]])
                            dve(lambda v: v.reciprocal(out=m_dn[pr, :], in_=m_dn[pr, :]), [sl["dn"]], [sl["dn"]])
                            dve(lambda v: v.tensor_tensor(out=m_attnT[pr, half * 4:half * 4 + 4, qc], in0=P[4][pr, :].rearrange("p (h k) -> p h k", k=128),
                                                          in1=m_dn[pr, :].rearrange("p (h k) -> p h k", k=128), op=ALU.mult),
                                [PS[4], sl["dn"]], [sl["attnT"]])

                for t in tiles:
                    qc = slice(t * 128, (t + 1) * 128)
                    mlstm_gates(gate_sb[:, t, :], s_sm, 7, None)
                    for h in range(4):
                        dve(lambda v: v.tensor_scalar(out=m_rE[:, h, :], in0=idf, scalar1=sm[:, 20 + h:21 + h], scalar2=None, op0=ALU.mult),
                            [s_c, s_sm], [sl["rE"]])
                    pe(lambda tt: tt.matmul(P[3][:], lhsT=onesf, rhs=m_rE[:].rearrange("p h k -> p (h k)"), start=True, stop=True),
                       [s_c, sl["rE"]], [PS[3]])
                    act(lambda a: a.activation(out=m_eb[:].rearrange("p h k -> p (h k)"), in_=P[3][:], func=AF.Exp, scale=-1.0), [PS[3]], [sl["eb"]])
                    for h in range(4):
                        act(lambda a: a.activation(out=m_Dt[:, h, :], in_=P[3][:, h * 128:(h + 1) * 128], func=AF.Exp,
                                                   bias=sm[:, 24 + h:25 + h], scale=-1.0), [PS[3], s_sm], [sl["Dt"]])
                    for h in range(4):
                        dve(lambda v: v.tensor_tensor(out=m_Dt[:, h, :], in0=m_Dt[:, h, :], in1=tri, op=ALU.mult), [sl["Dt"], s_c], [sl["Dt"]])
                    for h in range(4):
                        pe(lambda tt: tt.matmul(P[1][:, h * 128:(h + 1) * 128], lhsT=m_mkT[:, h, qc], rhs=m_mqT[:, h, qc], start=True, stop=True),
                           [sl["mkT"], sl["mqT"]], [PS[1]], inc=(h == 3))
                    dve(lambda v: v.tensor_tensor(out=m_Sq[:].rearrange("p h k -> p (h k)"), in0=P[1][:],
                                                  in1=m_Dt[:].rearrange("p h k -> p (h k)"), op=ALU.mult), [PS[1], sl["Dt"]], [sl["Sq"]])
                    dve(lambda v: v.tensor_tensor(out=m_qd[:], in0=m_mqT[:, :, qc], in1=m_eb[:], op=ALU.mult), [sl["mqT"], sl["eb"]], [sl["qd"]])
                    for h in range(4):
                        pb = 4 if h < 2 else 5
                        osl = P[pb][:, (h % 2) * 256:(h % 2 + 1) * 256]
                        pe(lambda tt: tt.matmul(osl, lhsT=m_Sq[:, h, :], rhs=m_va[:, t, h, 0:256], start=True, stop=False),
                           [sl["Sq"], sl["va"]], [PS[pb]], inc=False)
                        pe(lambda tt: tt.matmul(osl, lhsT=m_qd[:, h, :], rhs=Cb[:, h, 0:256], start=False, stop=True),
                           [sl["qd"], s_Cb], [PS[pb]], inc=(h % 2 == 1))
                    for h in range(4):
                        pe(lambda tt: tt.matmul(P[7][:, 32 + h:33 + h], lhsT=m_Sq[:, h, :], rhs=m_va[:, t, h, 256:257], start=True, stop=False),
                           [sl["Sq"], sl["va"]], [PS[7]], inc=False)
                        pe(lambda tt: tt.matmul(P[7][:, 32 + h:33 + h], lhsT=m_qd[:, h, :], rhs=Cb[:, h, 256:257], start=False, stop=True),
                           [sl["qd"], s_Cb], [PS[7]], inc=(h == 3))
                    act(lambda a: a.activation(out=sm[:, 36:40], in_=P[7][:, 32:36], func=AF.Abs), [PS[7]], [s_sm])
                    dve(lambda v: v.tensor_scalar(out=sm[:, 36:40], in0=sm[:, 36:40], scalar1=1.0, scalar2=None, op0=ALU.max), [s_sm], [s_sm])
                    dve(lambda v: v.reciprocal(out=sm[:, 36:40], in_=sm[:, 36:40]), [s_sm], [s_sm])
                    for h in range(4):
                        pb = 4 if h < 2 else 5
                        act(lambda a: a.activation(out=junk[:], in_=P[pb][:, (h % 2) * 256:(h % 2 + 1) * 256], func=AF.Square,
                                                   accum_out=sm[:, 40 + h:41 + h]), [PS[pb]], [s_junk, s_sm])
                    dve(lambda v: v.tensor_tensor(out=sm[:, 48:52], in0=sm[:, 40:44], in1=sm[:, 36:40], op=ALU.mult), [s_sm], [s_sm])
                    dve(lambda v: v.tensor_tensor(out=sm[:, 48:52], in0=sm[:, 48:52], in1=sm[:, 36:40], op=ALU.mult), [s_sm], [s_sm])
                    act(lambda a: a.activation(out=sm[:, 48:52], in_=sm[:, 48:52], func=AF.Sqrt, bias=1e-6, scale=1.0 / 256.0), [s_sm], [s_sm])
                    dve(lambda v: v.reciprocal(out=sm[:, 48:52], in_=sm[:, 48:52]), [s_sm], [s_sm])
                    dve(lambda v: v.tensor_tensor(out=sm[:, 44:48], in0=sm[:, 48:52], in1=sm[:, 36:40], op=ALU.mult), [s_sm], [s_sm])
                    for h in range(4):
                        pb = 4 if h < 2 else 5
                        dve(lambda v: v.scalar_tensor_tensor(out=m_mh[:, h * 256:(h + 1) * 256], in0=P[pb][:, (h % 2) * 256:(h % 2 + 1) * 256],
                                                             scalar=sm[:, 44 + h:45 + h], in1=m_gs[:, t, h * 256:(h + 1) * 256],
                                                             op0=ALU.mult, op1=ALU.mult), [PS[pb], s_sm, sl["gs"]], [sl["mh"]])
                    transpose_to(m_mh, sl["mh"], lambda c0, n: m_mhT[:, c0:c0 + n, qc], sl["mhT"], nchunks=8, pb=0)
                    pbv = P[0][:].bitcast(BF16)
                    for h in range(4):
                        pe(lambda tt: tt.transpose(out=pbv[:, h * 128:(h + 1) * 128], in_=m_mkT[:, h, qc], identity=idb[:]),
                           [sl["mkT"], s_idb], [PS[0]], inc=(h == 3))
                    mlstm_update(pbv[:, 0:512].rearrange("p (h k) -> p h k", k=128), PS[0], m_kw, sl["kw"], m_va[:, t], sl["va"], 4, 5, 7)
                act(lambda a: a.copy(out=kT_halo, in_=m_kT[:, 4, :]), [sl["kT"]], [s_halo])
                act(lambda a: a.copy(out=V_halo, in_=m_V[:, 4, :]), [sl["V"]], [s_halo])

                Sched.alias([sl["qT"], sl["mqT"], sl["mkT"]], [sl["mgT"]])
                for mp in range(8):
                    wga, s1 = wload(wcols(w_in, C_GA + mp * 256, 256), "")
                    wgb, s2 = wload(wcols(w_in, C_GB + mp * 256, 256), "")
                    i = ring["i"] % NRM; ring["i"] += 1
                    s3 = ring["slots"][i]
                    wa = ring["aps"][i][:, 0:2048].rearrange("p (j n) -> p j n", n=256)
                    for gg in range(2):
                        S.dma("pool", wa[gg * 64:(gg + 1) * 64], wba4[gg][:, :, mp * 256:(mp + 1) * 256], writes=[s3], dslot=s3)
                    wb, s4 = wload(wbm3[:, :, mp * 256:(mp + 1) * 256], "")
                    for mm in range(2):
                        m = mp * 2 + mm
                        ms = slice(mm * 128, (mm + 1) * 128)
                        b1, b2, b3, b4 = (1, 2, 3, 6) if m % 2 == 0 else (4, 5, 7, 0)
                        for c in range(NCH):
                            pe(lambda tt: tt.matmul(P[b1][:], lhsT=wga[:, c, ms], rhs=xT[:, c, :], start=(c == 0), stop=(c == NCH - 1)),
                               [s1, s_xT], [PS[b1]], inc=(c == NCH - 1))
                        for c in range(NCH):
                            pe(lambda tt: tt.matmul(P[b2][:], lhsT=wgb[:, c, ms], rhs=xT[:, c, :], start=(c == 0), stop=(c == NCH - 1)),
                               [s2, s_xT], [PS[b2]], inc=(c == NCH - 1))
                        for c in range(8):
                            pe(lambda tt: tt.matmul(P[b3][:], lhsT=wa[:, c, ms], rhs=m_attnT[:, c, :], start=(c == 0), stop=(c == 7)),
                               [s3, sl["attnT"]], [PS[b3]], inc=(c == 7))
                        for c in range(8):
                            pe(lambda tt: tt.matmul(P[b4][:], lhsT=wb[:, c, ms], rhs=m_mhT[:, c, :], start=(c == 0), stop=(c == 7)),
                               [s4, sl["mhT"]], [PS[b4]], inc=(c == 7))
                        act(lambda a: a.activation(out=m_t1, in_=P[b1][:], func=AF.Sigmoid), [PS[b1]], [sl["t1"]])
                        act(lambda a: a.activation(out=m_t2, in_=P[b2][:], func=AF.Sigmoid), [PS[b2]], [sl["t2"]])
                        dve(lambda v: v.tensor_tensor(out=m_t1, in0=m_t1, in1=P[b3][:], op=ALU.mult), [sl["t1"], PS[b3]], [sl["t1"]])
                        dve(lambda v: v.tensor_tensor(out=m_t2, in0=m_t2, in1=P[b4][:], op=ALU.mult), [sl["t2"], PS[b4]], [sl["t2"]])
                        dve(lambda v: v.tensor_tensor(out=m_mgT[:, m, :], in0=m_t1, in1=m_t2, op=ALU.add), [sl["t1"], sl["t2"]], [sl["mgT"]])
                for nb in range(8):
                    wt, wsl = wload(wcols(w_out, nb * 256, 256), "")
                    for t in tiles:
                        pb = nbank()
                        for c in range(NCH):
                            pe(lambda tt: tt.matmul(P[pb][:, 0:256], lhsT=m_mgT[:, c, t * 128:(t + 1) * 128], rhs=wt[:, c, :], start=(c == 0), stop=(c == NCH - 1)),
                               [wsl, sl["mgT"]], [PS[pb]], inc=(c == NCH - 1))
                        dve(lambda v: v.scalar_tensor_tensor(out=R[:, t, nb * 256:(nb + 1) * 256], in0=P[pb][:, 0:256], scalar=IALPHA,
                                                             in1=R[:, t, nb * 256:(nb + 1) * 256], op0=ALU.mult, op1=ALU.add),
                            [PS[pb], s_R[t]], [s_R[t]])
                load_ln(2)
                if SPARSE:
                    X1tok = ws(38176, 4 * D).rearrange("p (t k) -> p t k", k=D)
                    s_x1t = Slot("x1t")
                    Sched.alias([sl["attnT"], sl["mhT"]], [s_x1t])
                for t in tiles:
                    if SPARSE:
                        layer_norm(t, EPS_A, X1tok[:, t, :], s_x1t)
                        transpose_to(X1tok[:, t, :], s_x1t, lambda c0, n: xT[:, c0:c0 + n, t * 128:(t + 1) * 128], s_xT, pb=0)
                    else:
                        layer_norm(t, EPS_A, m_xb, sl["xb"])
                        transpose_to(m_xb, sl["xb"], lambda c0, n: xT[:, c0:c0 + n, t * 128:(t + 1) * 128], s_xT, pb=0)

                if SPARSE:
                    o = 46368
                    x_mskf = take(256, F32).rearrange("p (t k) -> p t k", k=64)
                    x_rank = take(256, F32).rearrange("p (t k) -> p t k", k=64)
                    x_mskb = take(256).rearrange("p (t k) -> p t k", k=64)
                    s_msk = Slot("msk")
                    Sched.alias([sl["xb"]], [s_msk])
                for t in range(4):
                    pb = nbank()
                    for c in range(NCH):
                        pe(lambda tt: tt.matmul(P[pb][:, 0:NE], lhsT=xT[:, c, t * 128:(t + 1) * 128], rhs=wrt[:, c, :], start=(c == 0), stop=(c == NCH - 1)),
                           [s_xT, s_wrt], [PS[pb]], inc=(c == NCH - 1))
                    sc_ = m_sc[:, 0:64]; sel_ = m_sc[:, 64:128]; top_ = m_sc[:, 128:136]; msk_ = m_sc[:, 192:256]
                    act(lambda a: a.activation(out=sc_, in_=P[pb][:, 0:NE], func=AF.Sigmoid), [PS[pb]], [sl["sc"]])
                    dve(lambda v: v.tensor_tensor(out=sel_, in0=sc_, in1=brB[:], op=ALU.add), [sl["sc"], s_br], [sl["sc"]])
                    dve(lambda v: v.max(out=top_, in_=sel_), [sl["sc"]], [sl["sc"]])
                    dve(lambda v: v.tensor_scalar(out=msk_, in0=sel_, scalar1=top_[:, 7:8], scalar2=None, op0=ALU.is_ge), [sl["sc"]], [sl["sc"]])
                    if SPARSE:
                        dve(lambda v: v.tensor_copy(out=x_mskf[:, t, :], in_=msk_), [sl["sc"]], [s_msk])
                        dve(lambda v: v.tensor_copy(out=x_mskb[:, t, :], in_=msk_), [sl["sc"]], [s_msk])
                    dve(lambda v: v.tensor_tensor(out=msk_, in0=msk_, in1=sc_, op=ALU.mult), [sl["sc"]], [sl["sc"]])
                    dve(lambda v: v.reduce_sum(out=sm[:, 150:151], in_=msk_, axis=AX.X), [sl["sc"]], [s_sm])
                    dve(lambda v: v.reciprocal(out=sm[:, 150:151], in_=sm[:, 150:151]), [s_sm], [s_sm])
                    dve(lambda v: v.tensor_scalar(out=gw[:, t, 0:NE], in0=msk_, scalar1=sm[:, 150:151], scalar2=2.5 * IALPHA,
                                                  op0=ALU.mult, op1=ALU.mult), [sl["sc"], s_sm], [s_gw])
                if SPARSE:
                    for t in range(4):
                        pb = nbank()
                        seq = [(stri[:], x_mskb[:, t, :])] + [(onesb[:], x_mskb[:, tp, :]) for tp in range(t)]
                        for i, (lt, rh) in enumerate(seq):
                            pe(lambda tt: tt.matmul(P[pb][:, 0:NE], lhsT=lt, rhs=rh, start=(i == 0), stop=(i == len(seq) - 1)),
                               [s_idb, s_msk], [PS[pb]], inc=(i == len(seq) - 1))
                        dve(lambda v: v.tensor_copy(out=x_rank[:, t, :], in_=P[pb][:, 0:NE]), [PS[pb]], [s_msk])
                    pb = nbank()
                    for t in range(4):
                        pe(lambda tt: tt.matmul(P[pb][:, 0:NE], lhsT=onesb[:], rhs=x_mskb[:, t, :], start=(t == 0), stop=(t == 3)),
                           [s_idb, s_msk], [PS[pb]], inc=(t == 3))
                    dve(lambda v: v.tensor_reduce(out=sm[:, 152:153], in_=P[pb][:, 0:NE], axis=AX.X, op=ALU.max), [PS[pb]], [s_sm])
                    dve(lambda v: v.tensor_tensor(out=flagmax[:], in0=flagmax[:], in1=sm[:, 152:153], op=ALU.max), [s_sm, s_flag], [s_flag])

                wq = "pool"

                def eload(e):
                    if PRECAST and blk >= 1:
                        wg3 = wbg[e].rearrange("(c p) n -> p c n", p=128)
                        wu3 = wbu[e].rearrange("(c p) n -> p c n", p=128)
                        wd3 = wbd[e].rearrange("(c p) n -> p c n", p=128)
                        xr = [s_pc[pc_group[e]]]
                    else:
                        wg3 = w_eg[e].rearrange("(c p) n -> p c n", p=128)
                        wu3 = w_eu[e].rearrange("(c p) n -> p c n", p=128)
                        wd3 = w_ed[e].rearrange("(c p) n -> p c n", p=128)
                        xr = []
                    wgs = [wload(wg3[:, :, hp * 256:(hp + 1) * 256], "", q=wq, xr=xr) for hp in range(2)]
                    wus = [wload(wu3[:, :, hp * 256:(hp + 1) * 256], "", q=wq, xr=xr) for hp in range(2)]
                    wds = [wload(wd3[:, hp * 2:hp * 2 + 2, :], "", q=wq, xr=xr) for hp in range(2)]
                    return wgs, wus, wds

                def dense_expert(e, e_h1, e_sg, s_h1, s_sg):
                    wgs, wus, wds = eload(e)
                    for m in range(4):
                        pg_, pu_ = (0, 1) if m % 2 == 0 else (2, 3)
                        wg_, sg_w = wgs[m // 2]; wu_, su_w = wus[m // 2]
                        ms = slice((m % 2) * 128, (m % 2 + 1) * 128)
                        for c in range(NCH):
                            pe(lambda tt: tt.matmul(P[pg_][:], lhsT=wg_[:, c, ms], rhs=xT[:, c, :], start=(c == 0), stop=(c == NCH - 1)),
                               [sg_w, s_xT], [PS[pg_]], inc=(c == NCH - 1))
                        for c in range(NCH):
                            pe(lambda tt: tt.matmul(P[pu_][:], lhsT=wu_[:, c, ms], rhs=xT[:, c, :], start=(c == 0), stop=(c == NCH - 1)),
                               [su_w, s_xT], [PS[pu_]], inc=(c == NCH - 1))
                        act(lambda a: a.activation(out=e_sg, in_=P[pg_][:], func=AF.Silu), [PS[pg_]], [s_sg])
                        dve(lambda v: v.tensor_tensor(out=e_h1[:, m, :], in0=e_sg, in1=P[pu_][:], op=ALU.mult), [s_sg, PS[pu_]], [s_h1])
                    for t in range(4):
                        for nb in range(4):
                            for m in range(4):
                                wd_, sd_w = wds[m // 2]
                                pe(lambda tt: tt.matmul(P[4 + nb][:], lhsT=e_h1[:, m, t * 128:(t + 1) * 128], rhs=wd_[:, m % 2, nb * 512:(nb + 1) * 512],
                                                        start=(m == 0), stop=(m == 3)), [s_h1, sd_w], [PS[4 + nb]], inc=(m == 3))
                            dve(lambda v: v.scalar_tensor_tensor(out=R[:, t, nb * 512:(nb + 1) * 512], in0=P[4 + nb][:], scalar=gw[:, t, e:e + 1],
                                                                 in1=R[:, t, nb * 512:(nb + 1) * 512], op0=ALU.mult, op1=ALU.add),
                                [PS[4 + nb], s_gw, s_R[t]], [s_R[t]])

                if not SPARSE:
                    o = NRE * RING_E
                    e_h1 = take(4 * 512).rearrange("p (m k) -> p m k", k=512)
                    e_sg = take(512, F32)
                    assert o <= HALO
                    s_h1 = Slot("h1"); s_sg = Slot("sg")
                    ring_setup(NRE)
                    moe_slots = [s_h1, s_sg] + ring["slots"]
                    Sched.alias(list(sl.values()) + ring_sl[:NRM], moe_slots)
                    for e in range(NE + 1):
                        dense_expert(e, e_h1, e_sg, s_h1, s_sg)
                    ple_olds = [s_h1, s_sg]
                    PLE_O = NRE * RING_E
                else:
                    NRS = 8
                    ring_setup(NRS)
                    o = NRS * RING_E
                    x_Sel = [take(512).rearrange("p (t k) -> p t k", k=128) for _ in range(2)]
                    x_SelT = take(512).rearrange("p (t k) -> p t k", k=128)
                    x_h1 = take(512); x_h1T = take(512).rearrange("p (m k) -> p m k", k=128)
                    x_yb = take(2048)
                    assert o <= 38176, o
                    o = 49440
                    oB = o
                    x_XeT = [take(2048).rearrange("p (c k) -> p c k", k=128) for _ in range(2)]
                    x_sg = take(512, F32)
                    assert o <= HALO, o
                    e_h1 = ws(oB, 2048).rearrange("p (m k) -> p m k", k=512)
                    e_sg = ws(oB + 2048, 512, F32)
                    s_h1 = Slot("h1"); s_sg = Slot("sg")
                    s_Sel = [Slot(), Slot()]; s_SelT = Slot(); s_xh1 = Slot(); s_xh1T = Slot(); s_yb = Slot()
                    s_XeT = [Slot(), Slot()]; s_xsg = Slot()
                    first = [s_h1, s_sg] + s_Sel + [s_SelT, s_xh1, s_xh1T, s_yb] + ring["slots"]
                    Sched.alias([v for k, v in sl.items() if k not in ("attnT", "mhT", "xb")] + ring_sl[:NRM], first)
                    dense_expert(NE, e_h1, e_sg, s_h1, s_sg)
                    Sched.alias([s_h1, s_sg], s_XeT + [s_xsg])
                    moe_slots = first + s_XeT + [s_xsg, s_x1t, s_msk]

                    def xSel(e):
                        b = e % 2
                        for t in range(4):
                            dve(lambda v: v.tensor_scalar(out=x_Sel[b][:, t, :], in0=iotaf, scalar1=x_rank[:, t, e:e + 1], scalar2=x_mskf[:, t, e:e + 1],
                                                          op0=ALU.is_equal, op1=ALU.mult), [s_c, s_msk], [s_Sel[b]])

                    def xGather(e):
                        b = e % 2
                        for g4 in range(4):
                            pb = g4 % 2
                            for cc in range(4):
                                c = g4 * 4 + cc
                                for t in range(4):
                                    pe(lambda tt: tt.matmul(P[pb][:, cc * 128:(cc + 1) * 128], lhsT=X1tok[:, t, c * 128:(c + 1) * 128], rhs=x_Sel[b][:, t, :],
                                                            start=(t == 0), stop=(t == 3)), [s_x1t, s_Sel[b]], [PS[pb]], inc=(cc == 3 and t == 3))
                            act(lambda a: a.copy(out=x_XeT[b][:, g4 * 4:(g4 + 1) * 4, :], in_=P[pb][:].rearrange("p (c k) -> p c k", k=128)),
                                [PS[pb]], [s_XeT[b]])

                    def xFFN(e, wgs, wus):
                        b = e % 2
                        for (bank, ws_) in ((2, wgs), (3, wus)):
                            for hp in range(2):
                                w_, wsl_ = ws_[hp]
                                for c in range(NCH):
                                    pe(lambda tt: tt.matmul(P[bank][:, hp * 256:(hp + 1) * 256], lhsT=x_XeT[b][:, c, :], rhs=w_[:, c, :],
                                                            start=(c == 0), stop=(c == NCH - 1)), [s_XeT[b], wsl_], [PS[bank]], inc=(c == NCH - 1))
                        act(lambda a: a.activation(out=x_sg, in_=P[2][:], func=AF.Silu), [PS[2]], [s_xsg])
                        dve(lambda v: v.tensor_tensor(out=x_h1, in0=x_sg, in1=P[3][:], op=ALU.mult), [s_xsg, PS[3]], [s_xh1])

                    def xT1(e):
                        transpose_to(x_h1, s_xh1, lambda c0, n: x_h1T[:, c0:c0 + n, :], s_xh1T, nchunks=4, pb=4)

                    def xY(e, wds):
                        for nb in range(4):
                            pb = 5 + nb % 2
                            for m in range(4):
                                wd_, sd_w = wds[m // 2]
                                pe(lambda tt: tt.matmul(P[pb][:], lhsT=x_h1T[:, m, :], rhs=wd_[:, m % 2, nb * 512:(nb + 1) * 512], start=(m == 0), stop=(m == 3)),
                                   [s_xh1T, sd_w], [PS[pb]], inc=(m == 3))
                            act(lambda a: a.copy(out=x_yb[:, nb * 512:(nb + 1) * 512], in_=P[pb][:]), [PS[pb]], [s_yb])

                    def xT2(e):
                        b = e % 2
                        transpose_to(x_Sel[b][:].rearrange("p t k -> p (t k)"), s_Sel[b], lambda c0, n: x_SelT[:, c0:c0 + n, :], s_SelT, nchunks=4, pb=4)

                    def xScatter(e):
                        i = 0
                        for t in range(4):
                            for nb in range(4):
                                pb = (7, 5, 6, 4)[i % 4]; i += 1
                                pe(lambda tt: tt.matmul(P[pb][:], lhsT=x_SelT[:, t, :], rhs=x_yb[:, nb * 512:(nb + 1) * 512], start=True, stop=True),
                                   [s_SelT, s_yb], [PS[pb]])
                                dve(lambda v: v.scalar_tensor_tensor(out=R[:, t, nb * 512:(nb + 1) * 512], in0=P[pb][:], scalar=gw[:, t, e:e + 1],
                                                                     in1=R[:, t, nb * 512:(nb + 1) * 512], op0=ALU.mult, op1=ALU.add),
                                    [PS[pb], s_gw, s_R[t]], [s_R[t]])

                    NEX = NE
                    xSel(0); xGather(0)
                    wcur = eload(0)
                    for e in range(NEX):
                        if e + 1 < NE:
                            xSel(e + 1)
                        xFFN(e, wcur[0], wcur[1])
                        wds = wcur[2]
                        if e + 1 < NE:
                            xGather(e + 1)
                        xT1(e)
                        xY(e, wds)
                        if e + 1 < NE:
                            wcur = eload(e + 1)
                        if blk == 0:
                            pc_issue(1)
                        xT2(e)
                        xScatter(e)
                    if blk == 0:
                        pc_issue(len(pc_list))
                    ple_olds = [s_Sel[0], s_Sel[1], s_SelT, s_xh1, s_xh1T, s_yb]
                    PLE_O = NRS * RING_E
                o = PLE_O
                p_xb2 = take(D); p_pb = take(256); p_pT = take(2 * 512).rearrange("p (c k) -> p c k", k=512)
                p_pf = take(256, F32); p_sg = take(256, F32)
                assert o <= (38176 if SPARSE else HALO), o
                s_xb2 = Slot(); s_pb = Slot(); s_pT = Slot(); s_psg = Slot()
                Sched.alias(ple_olds, [s_xb2, s_pb, s_pT, s_pf, s_psg])
                moe_slots = moe_slots + [s_xb2, s_pb, s_pT, s_pf, s_psg]
                load_ln(4)
                for t in range(4):
                    layer_norm(t, EPS_A, p_xb2, s_xb2)
                    transpose_to(p_xb2, s_xb2, lambda c0, n: xT[:, c0:c0 + n, t * 128:(t + 1) * 128], s_xT, pb=0)
                    S.dma("sp", p_pf, pin[(blk * 4 + t) * 128:(blk * 4 + t + 1) * 128, :], writes=[s_pf], dslot=s_pf)
                    act(lambda a: a.copy(out=p_pb, in_=p_pf), [s_pf], [s_pb])
                    transpose_to(p_pb, s_pb, lambda c0, n: p_pT[:, c0:c0 + n, t * 128:(t + 1) * 128], s_pT, nchunks=2, pb=0)
                wpp3 = w_pp.rearrange("(c p) n -> p c n", p=128)
                for nb in range(8):
                    wt, wsl = wload(wcols(w_pg, nb * 256, 256), "")
                    wp, wpl = wload(wpp3[:, :, nb * 256:(nb + 1) * 256], "")
                    for t in range(4):
                        b1, b2 = ((1, 2), (3, 6), (4, 5), (7, 0))[t]
                        for c in range(NCH):
                            pe(lambda tt: tt.matmul(P[b1][:, 0:256], lhsT=xT[:, c, t * 128:(t + 1) * 128], rhs=wt[:, c, :], start=(c == 0), stop=(c == NCH - 1)),
                               [wsl, s_xT], [PS[b1]], inc=(c == NCH - 1))
                        for c in range(2):
                            pe(lambda tt: tt.matmul(P[b2][:, 0:256], lhsT=p_pT[:, c, t * 128:(t + 1) * 128], rhs=wp[:, c, :], start=(c == 0), stop=(c == 1)),
                               [wpl, s_pT], [PS[b2]], inc=(c == 1))
                        act(lambda a: a.activation(out=p_sg, in_=P[b1][:, 0:256], func=AF.Sigmoid), [PS[b1]], [s_psg])
                        dve(lambda v: v.scalar_tensor_tensor(out=p_sg, in0=P[b2][:, 0:256], scalar=IALPHA, in1=p_sg, op0=ALU.mult, op1=ALU.mult),
                            [PS[b2], s_psg], [s_psg])
                        dve(lambda v: v.tensor_tensor(out=R[:, t, nb * 256:(nb + 1) * 256], in0=R[:, t, nb * 256:(nb + 1) * 256], in1=p_sg, op=ALU.add),
                            [s_psg, s_R[t]], [s_R[t]])
                load_ln(6)
                for t in range(4):
                    layer_norm(t, EPS_A, p_xb2, s_xb2)
                    S.dma("sp", out[(blk * 4 + t) * 128:(blk * 4 + t + 1) * 128, :], R[:, t, :], reads=[s_R[t]], dslot=s_xl[t])
            S.dma("sp", flag_o, flagmax[:], reads=[s_flag], dslot=s_flag)
            S.wait_all("sp", s_R + [s_flag])
        print("built: ops", S.nops, "waits", S.nwaits, flush=True)
    return nc


def _consts():
    cst = np.zeros((128, 4, 128), np.float32)
    cst[:, 3, :] = np.arange(128, dtype=np.float32)[None, :]
    cst[:, 0, :] = np.eye(128, dtype=np.float32)
    cst[:, 1, :] = np.triu(np.ones((128, 128), np.float32))
    cst[:, 2, :] = 1.0
    slopes = np.exp2(-8.0 / 16 * np.arange(1, 17, dtype=np.float32)).astype(np.float32)
    j = np.arange(128)[:, None]; i = np.arange(128)[None, :]
    ab = np.zeros((128, 2, 16, 128), np.float32)
    for kt in range(2):
        dist = (i - j + (128 if kt == 0 else 0)).astype(np.float32)
        ok = (dist >= 0) & (dist < 128)
        for h in range(16):
            ab[:, kt, h, :] = np.where(ok, -slopes[h] * dist, NEGBIG)
    return cst, ab


_CACHE = {}


def kernel(x, p, ln_in_g, ln_in_b, w_in, attn_sinks, mlstm_b_i, mlstm_b_f, mlstm_norm_g,
           w_branch_attn, w_branch_mlstm, w_out, ln_mix_g, ln_mix_b, w_router, b_router,
           w_exp_gate, w_exp_up, w_exp_down, w_sh_gate, w_sh_up, w_sh_down, ln_ffn_g, ln_ffn_b,
           w_ple_proj, w_ple_gate, ln_ple_g, ln_ple_b):
    f = lambda a: np.ascontiguousarray(np.asarray(a, dtype=np.float32))
    x = f(x); p = f(p)
    B, SEQ, _ = x.shape
    NQ = 8 // B
    TOK = SEQ // NQ
    NPRE = (SEQ - TOK) // 128
    def get(sparse):
        key = (TOK, NPRE, sparse)
        if key not in _CACHE:
            _CACHE[key] = build(TOK, NPRE, sparse)
        return _CACHE[key]
    nc = get(True)
    cst, ab = _consts()
    shared = {
        "cst": cst, "abias": ab,
        "lng": np.stack([f(ln_in_g), f(ln_in_b), f(ln_mix_g)[0], f(ln_mix_b)[0], f(ln_ffn_g)[0], f(ln_ffn_b)[0],
                         f(ln_ple_g)[0], f(ln_ple_b)[0]]),
        "w_in": f(w_in)[0], "sinks": f(attn_sinks).reshape(1, 16),
        "bif": np.concatenate([f(mlstm_b_i).reshape(-1), f(mlstm_b_f).reshape(-1)]).reshape(1, 8),
        "ng": f(mlstm_norm_g).reshape(1, 1024),
        "w_ba": f(w_branch_attn)[0], "w_bm": f(w_branch_mlstm)[0], "w_out": f(w_out)[0],
        "w_rt": f(w_router)[0], "b_rt": f(b_router).reshape(1, NE),
        "w_eg": np.concatenate([f(w_exp_gate)[0], f(w_sh_gate)], axis=0),
        "w_eu": np.concatenate([f(w_exp_up)[0], f(w_sh_up)], axis=0),
        "w_ed": np.concatenate([f(w_exp_down)[0], f(w_sh_down)], axis=0),
        "w_pp": f(w_ple_proj)[0], "w_pg": f(w_ple_gate)[0],
    }
    in_maps = []
    for c in range(8):
        b, j = c // NQ, c % NQ
        end = (j + 1) * TOK
        xin = np.zeros((SEQ, D), np.float32)
        xin[SEQ - end:] = x[b, :end]
        vm = np.zeros((128, NPRE + 1), np.float32)
        nvalid = (j * TOK) // 128
        if nvalid > 0:
            vm[:, NPRE - nvalid:NPRE] = 1.0
        vm[:, NPRE] = 1.0 if j > 0 else 0.0
        m = dict(shared)
        m["xin"] = xin; m["vmk"] = vm
        m["pin"] = np.ascontiguousarray(p[0, b, j * TOK:(j + 1) * TOK])
        in_maps.append(m)
    res = run_bass_kernel_spmd(nc, in_maps, core_ids=list(range(8)))
    if max(float(np.asarray(r["flag"]).max()) for r in res.results) > 128.5:
        res = run_bass_kernel_spmd(get(False), in_maps, core_ids=list(range(8)))
    outp = np.zeros((B, SEQ, D), np.float32)
    for c in range(8):
        b, j = c // NQ, c % NQ
        outp[b, j * TOK:(j + 1) * TOK] = np.asarray(res.results[c]["out"], dtype=np.float32)
    return outp
```

```python
import numpy as np
from contextlib import ExitStack
import concourse.bass as bass
import concourse.mybir as mybir
from concourse.bass_utils import run_bass_kernel_spmd

F32 = mybir.dt.float32
BF16 = mybir.dt.bfloat16
AF = mybir.ActivationFunctionType
ALU = mybir.AluOpType
AX = mybir.AxisListType

D = 2048
NCH = 16
NE = 64
ALPHA = 2.0 ** 0.25
IALPHA = 1.0 / ALPHA
EPS_A = 1e-5 / (ALPHA * ALPHA)
C_AQ, C_AK, C_AV, C_MQ, C_MK, C_MV, C_MO, C_MI, C_GA, C_GB = 0, 1024, 1152, 1280, 1792, 2304, 3328, 4352, 4360, 6408
NEGBIG = -30000.0


class Slot:
    __slots__ = ("name", "w", "r", "dsem", "dcnt")

    def __init__(self, name=""):
        self.name = name; self.w = None; self.r = {}; self.dsem = None; self.dcnt = 0


class Eng:
    def __init__(self, obj, sem):
        self.obj = obj; self.sem = sem; self.n = 0; self.waited = {}; self.own = {id(sem)}


class Sched:
    def __init__(self, nc, sems):
        self.nc = nc
        self.E = {"pe": Eng(nc.tensor, sems["pe"]), "dve": Eng(nc.vector, sems["dve"]),
                  "act": Eng(nc.scalar, sems["act"]), "pool": Eng(nc.gpsimd, sems["pool"]),
                  "sp": Eng(nc.sync, sems["sp"])}
        self.nops = 0; self.nwaits = 0

    def new_epoch(self, sems):
        for k, e in self.E.items():
            e.sem = sems[k]; e.n = 0; e.own.add(id(e.sem))

    def _wait(self, e, deps):
        best = {}
        for (sem, val, raw) in deps:
            if id(sem) in e.own and not raw:
                continue
            k = id(sem)
            if e.waited.get(k, 0) >= val:
                continue
            if k not in best or best[k][1] < val:
                best[k] = (sem, val)
        for k, (sem, val) in best.items():
            e.obj.wait_ge(sem, val); e.waited[k] = val; self.nwaits += 1

    def _deps(self, reads, writes):
        deps = []
        for s in reads:
            if s.w is not None:
                deps.append((s.w[0], s.w[1], True))
        for s in writes:
            if s.w is not None:
                deps.append((s.w[0], s.w[1], False))
            deps.extend((a, b, False) for (a, b) in s.r.values())
        return deps

    def op(self, eng, fn, reads=(), writes=(), inc=True):
        e = self.E[eng]
        self._wait(e, self._deps(reads, writes))
        inst = fn(e.obj)
        ev = (e.sem, e.n + 1)
        if inc:
            inst.then_inc(e.sem, 1); e.n += 1
        for s in reads:
            s.r[id(e.sem)] = ev
        for s in writes:
            s.w = ev; s.r = {}
        self.nops += 1
        return ev

    def dma(self, eng, out, in_, reads=(), writes=(), dslot=None):
        e = self.E[eng]
        waw = {(id(w.w[0]), w.w[1]) for w in writes if w.w is not None and w.w[0] is dslot.dsem}
        self._wait(e, [d for d in self._deps(reads, writes) if (id(d[0]), d[1]) not in waw])
        dslot.dcnt += 1
        e.obj.dma_start(out=out, in_=in_).then_inc(dslot.dsem, 16)
        ev = (dslot.dsem, 16 * dslot.dcnt)
        for s in reads:
            s.r[id(dslot.dsem)] = ev
        for s in writes:
            s.w = ev; s.r = {}
        return ev

    def wait_all(self, eng, slots):
        e = self.E[eng]
        deps = []
        for s in slots:
            if s.w is not None:
                deps.append((s.w[0], s.w[1], True))
            deps.extend((a, b, True) for (a, b) in s.r.values())
        self._wait(e, deps)

    @staticmethod
    def alias(olds, news):
        m = {}
        for s in olds:
            evs = list(s.r.values()) + ([s.w] if s.w is not None else [])
            for (sem, val) in evs:
                k = id(sem)
                if k not in m or m[k][1] < val:
                    m[k] = (sem, val)
        for s in news:
            s.w = None; s.r = dict(m)


def build(TOK, NPRE, SPARSE=True):
    NT = TOK // 128
    NBLK = TOK // 512
    NTT = NPRE + NT
    nc = bass.Bass("TRN2", target_bir_lowering=False)

    def din(name, shape, dt=F32):
        return nc.dram_tensor(name, list(shape), dt, kind="ExternalInput").ap()

    xin = din("xin", [NTT * 128, D])
    pin = din("pin", [TOK, 256])
    vmk = din("vmk", [128, NPRE + 1])
    cst = din("cst", [128, 4, 128])
    abias_d = din("abias", [128, 2, 16, 128])
    lng = din("lng", [8, D])
    w_in = din("w_in", [D, 8456])
    sinks = din("sinks", [1, 16])
    bif_d = din("bif", [1, 8])
    ng_d = din("ng", [1, 1024])
    w_ba = din("w_ba", [1024, D])
    w_bm = din("w_bm", [1024, D])
    w_out = din("w_out", [D, D])
    w_rt = din("w_rt", [D, NE])
    b_rt = din("b_rt", [1, NE])
    w_eg = din("w_eg", [NE + 1, D, 512])
    w_eu = din("w_eu", [NE + 1, D, 512])
    w_ed = din("w_ed", [NE + 1, 512, D])
    w_pp = din("w_pp", [256, D])
    w_pg = din("w_pg", [D, D])
    out = nc.dram_tensor("out", [TOK, D], F32, kind="ExternalOutput").ap()
    flag_o = nc.dram_tensor("flag", [128, 1], F32, kind="ExternalOutput").ap()
    PRECAST = SPARSE
    if PRECAST:
        wbg = nc.dram_tensor("wbg", [NE + 1, D, 512], BF16, kind="Internal").ap()
        wbu = nc.dram_tensor("wbu", [NE + 1, D, 512], BF16, kind="Internal").ap()
        wbd = nc.dram_tensor("wbd", [NE + 1, 512, D], BF16, kind="Internal").ap()

    with ExitStack() as es:
        def sb(name, shape, dt):
            return es.enter_context(nc.sbuf_tensor(name, list(shape), dt))

        def sem(name):
            return es.enter_context(nc.semaphore(name))

        nsem = [0]

        def engsems():
            nsem[0] += 1
            return {k: sem("e%d_%s" % (nsem[0], k)) for k in ["pe", "dve", "act", "pool", "sp"]}

        def dslot(name):
            s = Slot(name); s.dsem = sem("d_" + name); return s

        S = Sched(nc, engsems())

        WSN = 54816
        R = sb("R", [128, 4, D], F32)
        xT = sb("xT", [128, NCH, 512], BF16)
        WS = sb("WS", [128, WSN], BF16)
        cf = sb("cf", [128, 4, 128], F32)
        idb = sb("idb", [128, 128], BF16)
        onesb = sb("onesb", [128, 128], BF16)
        abias = sb("abias_s", [128, 2, 16, 128], F32)
        esink = sb("esink", [128, 16], F32)
        bif = sb("bifs", [128, 8], F32)
        ngB = sb("ngB", [128, 1024], F32)
        brB = sb("brB", [128, NE], F32)
        vm = sb("vm", [128, NPRE + 1], F32)
        kvb = sb("kvb", [128, 1], F32)
        wrt = sb("wrt", [128, NCH, NE], BF16)
        C32 = sb("C32", [128, 4, 256], F32)
        n32 = sb("n32", [128, 4], F32)
        Cb = sb("Cb", [128, 4, 258], BF16)
        gw = sb("gw", [128, 4, NE + 1], F32)
        sm = sb("sm", [128, 160], F32)
        st6 = sb("st6", [128, 4, 6], F32)
        junk = sb("junk", [128, 256], BF16)
        stri = sb("stri", [128, 128], BF16)
        flagmax = sb("flagmax", [128, 1], F32)
        gB = sb("gB", [128, D], F32)
        bB = sb("bB", [128, D], F32)
        idf = cf[:, 0, :]; tri = cf[:, 1, :]; onesf = cf[:, 2, :]; iotaf = cf[:, 3, :]

        P = [es.enter_context(nc.psum_tensor("ps%d" % i, [128, 512], F32)) for i in range(8)]
        PS = [Slot("ps%d" % i) for i in range(8)]

        s_R = [Slot("R%d" % i) for i in range(4)]
        s_xT = Slot("xT")
        s_c = dslot("c"); s_ab = dslot("ab"); s_es = dslot("es"); s_bif = dslot("bif"); s_ng = dslot("ng")
        s_br = dslot("br"); s_vm = dslot("vm"); s_wrt = dslot("wrt"); s_gB = dslot("gB"); s_bB = dslot("bB")
        s_idb = Slot("idb"); s_C = Slot("C"); s_Cb = Slot("Cb"); s_gw = Slot("gw"); s_sm = Slot("sm")
        s_st6 = Slot("st"); s_junk = Slot("junk"); s_kvb = Slot("kvb")
        s_xl = [dslot("xl%d" % i) for i in range(4)]

        def ws(off, n, dt=BF16):
            if dt == F32:
                return WS[:, off:off + 2 * n].bitcast(F32)
            return WS[:, off:off + n]

        block = es.enter_context(nc.Block())

        @block.sync
        def _(sync):
            dve = lambda fn, r=(), w=(): S.op("dve", fn, r, w)
            act = lambda fn, r=(), w=(): S.op("act", fn, r, w)
            pool = lambda fn, r=(), w=(): S.op("pool", fn, r, w)
            pe = lambda fn, r=(), w=(), inc=True: S.op("pe", fn, r, w, inc)

            S.dma("sp", cf[:], cst, writes=[s_c], dslot=s_c)
            S.dma("sp", abias[:], abias_d, writes=[s_ab], dslot=s_ab)
            S.dma("sp", esink[:], sinks.partition_broadcast(128).rearrange("p a b -> p (a b)"), writes=[s_es], dslot=s_es)
            S.dma("sp", bif[:], bif_d.partition_broadcast(128).rearrange("p a b -> p (a b)"), writes=[s_bif], dslot=s_bif)
            S.dma("sp", ngB[:], ng_d.partition_broadcast(128).rearrange("p a b -> p (a b)"), writes=[s_ng], dslot=s_ng)
            S.dma("sp", brB[:], b_rt.partition_broadcast(128).rearrange("p a b -> p (a b)"), writes=[s_br], dslot=s_br)
            S.dma("sp", vm[:], vmk, writes=[s_vm], dslot=s_vm)
            S.dma("pool", wrt[:], w_rt.rearrange("(c p) n -> p c n", p=128), writes=[s_wrt], dslot=s_wrt)
            dve(lambda v: v.tensor_copy(out=idb[:], in_=idf), [s_c], [s_idb])
            dve(lambda v: v.tensor_copy(out=onesb[:], in_=onesf), [s_c], [s_idb])
            act(lambda a: a.activation(out=esink[:], in_=esink[:], func=AF.Exp), [s_es], [s_es])
            dve(lambda v: v.tensor_scalar(out=kvb[:], in0=vm[:, NPRE:NPRE + 1], scalar1=-1.0, scalar2=-NEGBIG,
                                          op0=ALU.add, op1=ALU.mult), [s_vm], [s_kvb])
            dve(lambda v: v.memset(C32[:], 0.0), [], [s_C])
            dve(lambda v: v.memset(n32[:], 0.0), [], [s_C])
            dve(lambda v: v.memset(Cb[:], 0.0), [], [s_Cb])
            dve(lambda v: v.memset(gw[:], IALPHA), [], [s_gw])
            s_flag = dslot("flag")
            dve(lambda v: v.memset(flagmax[:], 0.0), [], [s_flag])
            dve(lambda v: v.tensor_tensor(out=stri[:], in0=tri, in1=idf, op=ALU.subtract), [s_c], [s_idb])
            s_pc = [dslot("pc%d" % i) for i in range(9)]
            pc_list = []
            pc_group = {}
            if PRECAST:
                for e in [NE] + list(range(NE)):
                    for (dst_, src_) in ((wbg, w_eg), (wbu, w_eu), (wbd, w_ed)):
                        pc_group[e] = len(pc_list) // 24
                        pc_list.append((dst_[e], src_[e], len(pc_list) // 24))
            pc_state = {"i": 0, "hist": []}
            pc_prefix_issued = [0]

            def pc_issue(n):
                for _ in range(n):
                    if pc_state["i"] >= len(pc_list):
                        return
                    d_, s_, g_ = pc_list[pc_state["i"]]; pc_state["i"] += 1
                    hist = pc_state["hist"]
                    if len(hist) >= 12:
                        psem, pval = hist[-12]
                        nc.gpsimd.wait_ge(psem, pval)
                    hist.append(S.dma("pool", d_, s_, writes=[s_pc[g_]], dslot=s_pc[g_]))

            def load_ln(idx):
                S.dma("sp", gB[:], lng[idx].partition_broadcast(128), writes=[s_gB], dslot=s_gB)
                S.dma("sp", bB[:], lng[idx + 1].partition_broadcast(128), writes=[s_bB], dslot=s_bB)

            def layer_norm(t, eps, xb, s_xb, use_pool=False, sc0=0, s_st=None):
                s_q = s_sm if s_st is None else s_st
                rt = R[:, t, :]
                c0, c1, c2, c3 = sc0, sc0 + 1, sc0 + 2, sc0 + 3
                for i in range(4):
                    dve(lambda v: v.bn_stats(out=st6[:, i, :], in_=rt[:, i * 512:(i + 1) * 512]), [s_R[t]], [s_st6])
                dve(lambda v: v.bn_aggr(out=sm[:, c0:c0 + 2], in_=st6[:].rearrange("p a b -> p (a b)")), [s_st6], [s_q])
                act(lambda a: a.activation(out=sm[:, c2:c2 + 1], in_=sm[:, c1:c1 + 1], func=AF.Sqrt, bias=float(eps), scale=1.0), [s_q], [s_q])
                dve(lambda v: v.reciprocal(out=sm[:, c2:c2 + 1], in_=sm[:, c2:c2 + 1]), [s_q], [s_q])
                if use_pool:
                    dve(lambda v: v.tensor_scalar(out=sm[:, c3:c3 + 1], in0=sm[:, c0:c0 + 1], scalar1=sm[:, c2:c2 + 1], scalar2=-1.0,
                                                  op0=ALU.mult, op1=ALU.mult), [s_q], [s_q])
                    act(lambda a: a.activation(out=rt, in_=rt, func=AF.Identity, bias=sm[:, c3:c3 + 1], scale=sm[:, c2:c2 + 1]),
                        [s_R[t], s_q], [s_R[t]])
                    pool(lambda g: g.tensor_tensor(out=rt, in0=rt, in1=gB[:], op=ALU.mult), [s_R[t], s_gB], [s_R[t]])
                    pool(lambda g: g.tensor_tensor(out=xb, in0=rt, in1=bB[:], op=ALU.add), [s_R[t], s_bB], [s_xb])
                    return
                dve(lambda v: v.scalar_tensor_tensor(out=rt, in0=rt, scalar=sm[:, c0:c0 + 1], in1=gB[:], op0=ALU.subtract, op1=ALU.mult),
                    [s_R[t], s_q, s_gB], [s_R[t]])
                dve(lambda v: v.scalar_tensor_tensor(out=rt, in0=rt, scalar=sm[:, c2:c2 + 1], in1=bB[:], op0=ALU.mult, op1=ALU.add),
                    [s_R[t], s_q, s_bB], [s_R[t]])
                act(lambda a: a.copy(out=xb, in_=rt), [s_R[t]], [s_xb])

            def transpose_to(xb, s_xb, dst_fn, s_dst, nchunks=NCH, pb=0):
                pbv = P[pb][:].bitcast(BF16)
                for g0 in range(0, nchunks, 8):
                    n = min(8, nchunks - g0)
                    for j in range(n):
                        c = g0 + j
                        pe(lambda t: t.transpose(out=pbv[:, j * 128:(j + 1) * 128], in_=xb[:, c * 128:(c + 1) * 128], identity=idb[:]),
                           [s_xb, s_idb], [PS[pb]], inc=(j == n - 1))
                    act(lambda a: a.copy(out=dst_fn(g0, n), in_=pbv[:, 0:n * 128].rearrange("p (c k) -> p c k", k=128)),
                        [PS[pb]], [s_dst])

            ring = {"slots": [], "aps": [], "i": 0}
            RING_E = 4096

            def wload(src, shape_str, q="pool", xr=()):
                i = ring["i"] % len(ring["slots"]); ring["i"] += 1
                sl = ring["slots"][i]
                n = 1
                for d in src.shape[1:]:
                    n *= d
                dst = ring["aps"][i][0:src.shape[0], 0:n]
                if len(src.shape) == 3:
                    dst = dst.rearrange("p (a b) -> p a b", b=src.shape[2])
                S.dma(q, dst, src, reads=list(xr), writes=[sl], dslot=sl)
                return dst, sl

            def wcols(wd, c0, n):
                return wd.rearrange("(c p) n -> p c n", p=128)[:, :, c0:c0 + n]

            def mlstm_gates(gate_ps, s_gate_ps, pg, vcol, co=0, s_gs=None):
                sq = s_sm if s_gs is None else s_gs
                k = lambda a, b: sm[:, co + a:co + b]
                if gate_ps is not None:
                    dve(lambda v: v.tensor_tensor(out=k(8, 16), in0=gate_ps, in1=bif[:], op=ALU.add), [s_gate_ps, s_bif], [sq])
                act(lambda a: a.activation(out=k(8, 16), in_=k(8, 16), func=AF.Tanh, scale=1.0 / 15.0), [sq], [sq])
                act(lambda a: a.activation(out=k(16, 20), in_=k(12, 16), func=AF.Exp, scale=-15.0), [sq], [sq])
                act(lambda a: a.activation(out=k(16, 20), in_=k(16, 20), func=AF.Ln, bias=1.0, scale=1.0), [sq], [sq])
                pe(lambda t: t.matmul(P[pg][:, 0:4], lhsT=tri, rhs=k(16, 20), start=True, stop=True), [s_c, sq], [PS[pg]], inc=False)
                pe(lambda t: t.matmul(P[pg][:, 4:8], lhsT=onesf, rhs=k(16, 20), start=True, stop=True), [s_c, sq], [PS[pg]])
                dve(lambda v: v.tensor_copy(out=k(20, 24), in_=P[pg][:, 0:4]), [PS[pg]], [sq])
                dve(lambda v: v.scalar_tensor_tensor(out=k(24, 28), in0=k(8, 12), scalar=15.0, in1=k(20, 24),
                                                     op0=ALU.mult, op1=ALU.add), [sq], [sq])
                dve(lambda v: v.tensor_tensor(out=k(28, 32), in0=k(24, 28), in1=P[pg][:, 4:8], op=ALU.subtract), [sq, PS[pg]], [sq])
                act(lambda a: a.activation(out=k(28, 32), in_=k(28, 32), func=AF.Exp), [sq], [sq])
                if vcol is not None:
                    dve(lambda v: v.tensor_scalar(out=k(28, 32), in0=k(28, 32), scalar1=vm[:, vcol:vcol + 1], scalar2=None,
                                                  op0=ALU.mult), [sq, s_vm], [sq])
                act(lambda a: a.activation(out=k(32, 36), in_=P[pg][:, 4:8], func=AF.Exp, scale=-1.0), [PS[pg]], [sq])

            def mlstm_update(ktok_ps, s_ktok, kw, s_kw, vaug, s_vaug, pu0, pu1, pn):
                for h in range(4):
                    act(lambda a: a.activation(out=kw[:, h, :], in_=ktok_ps[:, h, :], func=AF.Copy, scale=sm[:, 28 + h:29 + h]),
                        [s_ktok, s_sm], [s_kw])
                for h in range(4):
                    pb = pu0 if h < 2 else pu1
                    pe(lambda t: t.matmul(P[pb][:, (h % 2) * 256:(h % 2 + 1) * 256], lhsT=kw[:, h, :], rhs=vaug[:, h, 0:256],
                                          start=True, stop=True), [s_kw, s_vaug], [PS[pb]], inc=(h % 2 == 1))
                for h in range(4):
                    pe(lambda t: t.matmul(P[pn][:, 16 + h:17 + h], lhsT=kw[:, h, :], rhs=vaug[:, h, 256:257], start=True, stop=True),
                       [s_kw, s_vaug], [PS[pn]], inc=(h == 3))
                for h in range(4):
                    pb = pu0 if h < 2 else pu1
                    dve(lambda v: v.scalar_tensor_tensor(out=C32[:, h, :], in0=C32[:, h, :], scalar=sm[:, 32 + h:33 + h],
                                                         in1=P[pb][:, (h % 2) * 256:(h % 2 + 1) * 256], op0=ALU.mult, op1=ALU.add),
                        [s_C, s_sm, PS[pb]], [s_C])
                dve(lambda v: v.tensor_tensor(out=n32[:], in0=n32[:], in1=sm[:, 32:36], op=ALU.mult), [s_C, s_sm], [s_C])
                dve(lambda v: v.tensor_tensor(out=n32[:], in0=n32[:], in1=P[pn][:, 16:20], op=ALU.add), [s_C, PS[pn]], [s_C])
                act(lambda a: a.copy(out=Cb[:, :, 0:256], in_=C32[:]), [s_C], [s_Cb])
                act(lambda a: a.copy(out=Cb[:, :, 256:257], in_=n32[:].rearrange("p (h o) -> p h o", o=1)), [s_C], [s_Cb])

            PW = 512 + 1024 + 8 + 128 + 128
            o = 0
            wpre = ws(o, NCH * PW).rearrange("p (c n) -> p c n", n=PW); o += NCH * PW
            p_xb = []; p_hT = []; p_va = []; p_kt = []
            for b in range(2):
                p_xb.append(ws(o, D)); o += D
                p_hT.append(ws(o, NCH * 128).rearrange("p (c k) -> p c k", k=128)); o += NCH * 128
                p_va.append(ws(o, 4 * 258).rearrange("p (h k) -> p h k", k=258)); o += 4 * 258
                p_kt.append(ws(o, 512).rearrange("p (h k) -> p h k", k=128)); o += 512
            p_kw = ws(o, 512).rearrange("p (h k) -> p h k", k=128); o += 512
            HALO = WSN - 256
            assert o <= HALO
            kT_halo = ws(HALO, 128); V_halo = ws(HALO + 128, 128)
            s_wpre = dslot("wpre"); s_pkw = Slot(); s_halo = Slot()
            s_pxb = [Slot(), Slot()]; s_phT = [Slot(), Slot()]; s_pva = [Slot(), Slot()]; s_pkt = [Slot(), Slot()]
            s_g = [Slot(), Slot()]
            GCO = [0, 52]

            def pA(pt):
                b = pt % 2; t = pt % 4
                S.dma("sp", R[:, t, :], xin[pt * 128:(pt + 1) * 128, :], writes=[s_R[t]], dslot=s_xl[t])
                layer_norm(t, 1e-5, p_xb[b], s_pxb[b], use_pool=True, sc0=104 + 4 * b, s_st=s_g[b])

            def pT(pt):
                b = pt % 2
                transpose_to(p_xb[b], s_pxb[b], lambda c0, n: p_hT[b][:, c0:c0 + n, :], s_phT[b], pb=0)

            def pB(pt):
                b = pt % 2; co = GCO[b]
                for (pb, po, n) in [(1, 0, 512), (2, 512, 512), (3, 1024, 512)]:
                    for c in range(NCH):
                        pe(lambda tt: tt.matmul(P[pb][:, 0:n], lhsT=p_hT[b][:, c, :], rhs=wpre[:, c, po:po + n], start=(c == 0), stop=(c == NCH - 1)),
                           [s_phT[b], s_wpre], [PS[pb]], inc=(c == NCH - 1))
                for c in range(NCH):
                    pe(lambda tt: tt.matmul(P[6][:, 0:8], lhsT=p_hT[b][:, c, :], rhs=wpre[:, c, 1536:1544], start=(c == 0), stop=(c == NCH - 1)),
                       [s_phT[b], s_wpre], [PS[6]], inc=(c == NCH - 1))
                act(lambda a: a.activation(out=p_kt[b][:].rearrange("p h k -> p (h k)"), in_=P[1][:], func=AF.Copy, scale=128.0 ** -0.5), [PS[1]], [s_pkt[b]])
                act(lambda a: a.copy(out=p_va[b][:, 0:2, 0:256], in_=P[2][:].rearrange("p (h k) -> p h k", k=256)), [PS[2]], [s_pva[b]])
                act(lambda a: a.copy(out=p_va[b][:, 2:4, 0:256], in_=P[3][:].rearrange("p (h k) -> p h k", k=256)), [PS[3]], [s_pva[b]])
                dve(lambda v: v.tensor_tensor(out=sm[:, co + 8:co + 16], in0=P[6][:, 0:8], in1=bif[:], op=ALU.add), [PS[6], s_bif], [s_g[b]])
                if pt == NPRE - 1:
                    for c in range(NCH):
                        pe(lambda tt: tt.matmul(P[1][:, 0:128], lhsT=wpre[:, c, 1544:1672], rhs=p_hT[b][:, c, :], start=(c == 0), stop=(c == NCH - 1)),
                           [s_phT[b], s_wpre], [PS[1]], inc=(c == NCH - 1))
                    for c in range(NCH):
                        pe(lambda tt: tt.matmul(P[2][:, 0:128], lhsT=p_hT[b][:, c, :], rhs=wpre[:, c, 1672:1800], start=(c == 0), stop=(c == NCH - 1)),
                           [s_phT[b], s_wpre], [PS[2]], inc=(c == NCH - 1))
                    act(lambda a: a.copy(out=kT_halo, in_=P[1][:, 0:128]), [PS[1]], [s_halo])
                    act(lambda a: a.copy(out=V_halo, in_=P[2][:, 0:128]), [PS[2]], [s_halo])

            def pC1(pt):
                b = pt % 2
                mlstm_gates(None, None, 7, pt, co=GCO[b], s_gs=s_g[b])

            def pC2(pt):
                b = pt % 2; co = GCO[b]
                for h in range(4):
                    act(lambda a: a.activation(out=p_kw[:, h, :], in_=p_kt[b][:, h, :], func=AF.Copy, scale=sm[:, co + 28 + h:co + 29 + h]),
                        [s_pkt[b], s_g[b]], [s_pkw])
                for h in range(4):
                    pb = 4 if h < 2 else 5
                    pe(lambda tt: tt.matmul(P[pb][:, (h % 2) * 256:(h % 2 + 1) * 256], lhsT=p_kw[:, h, :], rhs=p_va[b][:, h, 0:256], start=True, stop=True),
                       [s_pkw, s_pva[b]], [PS[pb]], inc=(h % 2 == 1))
                for h in range(4):
                    pe(lambda tt: tt.matmul(P[7][:, 16 + h:17 + h], lhsT=p_kw[:, h, :], rhs=p_va[b][:, h, 256:257], start=True, stop=True),
                       [s_pkw, s_pva[b]], [PS[7]], inc=(h == 3))
                for h in range(4):
                    pb = 4 if h < 2 else 5
                    dve(lambda v: v.scalar_tensor_tensor(out=C32[:, h, :], in0=C32[:, h, :], scalar=sm[:, co + 32 + h:co + 33 + h],
                                                         in1=P[pb][:, (h % 2) * 256:(h % 2 + 1) * 256], op0=ALU.mult, op1=ALU.add),
                        [s_C, s_g[b], PS[pb]], [s_C])
                dve(lambda v: v.tensor_tensor(out=n32[:], in0=n32[:], in1=sm[:, co + 32:co + 36], op=ALU.mult), [s_C, s_g[b]], [s_C])
                dve(lambda v: v.tensor_tensor(out=n32[:], in0=n32[:], in1=P[7][:, 16:20], op=ALU.add), [s_C, PS[7]], [s_C])

            if NPRE > 0:
                segs = [(C_MK, 512), (C_MV, 1024), (C_MI, 8), (C_AK, 128), (C_AV, 128)]
                po = 0
                for (c0, n) in segs:
                    S.dma("pool", wpre[:, :, po:po + n], wcols(w_in, c0, n), writes=[s_wpre], dslot=s_wpre)
                    po += n
                for b in range(2):
                    dve(lambda v: v.memset(p_va[b][:], 1.0), [], [s_pva[b]])
                load_ln(0)
                pA(0); pT(0)
                for pt in range(NPRE):
                    if pt + 1 < NPRE:
                        pA(pt + 1)
                    pc_issue(1 if pt % 2 == 0 else 2)
                    pB(pt)
                    if pt >= 1:
                        pC2(pt - 1)
                    if pt + 1 < NPRE:
                        pT(pt + 1)
                    pC1(pt)
                pC2(NPRE - 1)
                pc_prefix_issued[0] = pc_state["i"]
                act(lambda a: a.copy(out=Cb[:, :, 0:256], in_=C32[:]), [s_C], [s_Cb])
                act(lambda a: a.copy(out=Cb[:, :, 256:257], in_=n32[:].rearrange("p (h o) -> p h o", o=1)), [s_C], [s_Cb])
            if NPRE == 0:
                dve(lambda v: v.memset(kT_halo, 0.0), [], [s_halo])
                dve(lambda v: v.memset(V_halo, 0.0), [], [s_halo])

            SB = 512
            RING_E = 4096
            NRM, NRE = 5, 12
            Sched.alias([s_sm] + s_g, [s_sm])
            ring_sl = [dslot("rg%d" % i) for i in range(NRE)]
            s_pf = dslot("pf")
            rot = {"i": 0}

            def nbank(banks=(1, 2, 6, 7)):
                rot["i"] += 1
                return banks[rot["i"] % len(banks)]

            def ring_setup(nslots):
                ring["slots"] = ring_sl[:nslots]
                ring["aps"] = [ws(i * RING_E, RING_E) for i in range(nslots)]
                ring["i"] = 0

            w3 = w_in.rearrange("(c p) n -> p c n", p=128)
            wba4 = w_ba.rearrange("(g j d) n -> g d j n", g=2, d=64)
            wbm3 = w_bm.rearrange("(c p) n -> p c n", p=128)
            moe_slots = None
            for blk in range(NBLK):
                S.new_epoch(engsems())
                o = NRM * RING_E

                def take(n, dt=BF16):
                    nonlocal o
                    v = ws(o, n, dt); o += n * (2 if dt == F32 else 1); return v
                o_mg = o
                m_qT = take(8 * SB).rearrange("p (j k) -> p j k", k=SB)
                m_mqT = take(4 * SB).rearrange("p (h k) -> p h k", k=SB)
                m_mkT = take(4 * SB).rearrange("p (h k) -> p h k", k=SB)
                m_mgT = ws(o_mg, 16 * SB).rearrange("p (c k) -> p c k", k=SB)
                m_kT = take(5 * 128).rearrange("p (t k) -> p t k", k=128)
                m_V = take(5 * 128).rearrange("p (t k) -> p t k", k=128)
                m_va = take(4 * 4 * 258).rearrange("p (t h k) -> p t h k", h=4, k=258)
                m_gs = take(4 * 1024).rearrange("p (t k) -> p t k", k=1024)
                m_attnT = take(8 * SB).rearrange("p (h k) -> p h k", k=SB)
                m_mhT = take(8 * SB).rearrange("p (c k) -> p c k", k=SB)
                m_xb = take(D)
                m_sc = take(512, F32)
                m_PT = take(2 * 512).rearrange("p (t k) -> p t k", k=512)
                m_dn = take(512, F32)
                m_eb = take(512).rearrange("p (h k) -> p h k", k=128)
                m_Sq = take(512).rearrange("p (h k) -> p h k", k=128)
                m_qd = take(512).rearrange("p (h k) -> p h k", k=128)
                m_kw = take(512).rearrange("p (h k) -> p h k", k=128)
                m_mh = take(1024)
                m_Dt = m_dn.rearrange("p (h k) -> p h k", k=128)
                m_rE = m_sc.rearrange("p (h k) -> p h k", k=128)
                m_t1 = m_sc
                m_t2 = m_PT.rearrange("p t k -> p (t k)").bitcast(F32)
                assert o <= HALO, o
                names = "xb V va gs qT kT mqT mkT attnT mhT mgT sc PT eb Sq qd kw mh dn".split()
                sl = {n: Slot(n) for n in names}
                sl["Dt"] = sl["dn"]; sl["rE"] = sl["sc"]; sl["t1"] = sl["sc"]; sl["t2"] = sl["PT"]
                ring_setup(NRM)
                mix_slots = [v for k, v in sl.items() if k != "mgT"] + ring["slots"]
                if blk == 0:
                    Sched.alias([s_wpre, s_pkw] + s_pxb + s_phT + s_pva + s_pkt, mix_slots)
                else:
                    Sched.alias(moe_slots, mix_slots)
                dve(lambda v: v.memset(m_va[:], 1.0), [], [sl["va"]])
                tiles = [0, 1, 2, 3]
                gt0 = NPRE + blk * 4
                load_ln(0)
                for t in tiles:
                    S.dma("sp", R[:, t, :], xin[(gt0 + t) * 128:(gt0 + t + 1) * 128, :], writes=[s_R[t]], dslot=s_xl[t])
                for t in tiles:
                    layer_norm(t, 1e-5, m_xb, sl["xb"])
                    transpose_to(m_xb, sl["xb"], lambda c0, n: xT[:, c0:c0 + n, t * 128:(t + 1) * 128], s_xT, pb=0)
                act(lambda a: a.copy(out=m_kT[:, 0, :], in_=kT_halo), [s_halo], [sl["kT"]])
                act(lambda a: a.copy(out=m_V[:, 0, :], in_=V_halo), [s_halo], [sl["V"]])

                def fmm(wt2, wsl, pb):
                    for c in range(NCH):
                        pe(lambda tt: tt.matmul(P[pb][:], lhsT=wt2[:, c, :], rhs=xT[:, c, :], start=(c == 0), stop=(c == NCH - 1)),
                           [wsl, s_xT], [PS[pb]], inc=(c == NCH - 1))
                for jp in range(4):
                    i = ring["i"] % NRM; ring["i"] += 1
                    wsl = ring["slots"][i]
                    wt = ring["aps"][i].rearrange("p (m c g r) -> p m c g r", m=2, g=2, r=64)
                    for mm in range(2):
                        j = jp * 2 + mm
                        src = w3[:, :, j * 64:j * 64 + 1024].rearrange("p c (g r) -> p c g r", r=512)
                        for gg in range(2):
                            S.dma("pool", wt[:, mm, :, gg, :], src[:, :, gg, 0:64], writes=[wsl], dslot=wsl)
                    wt2 = ring["aps"][i].rearrange("p (m c k) -> p m c k", m=2, k=128)
                    for mm in range(2):
                        j = jp * 2 + mm
                        pb = nbank()
                        fmm(wt2[:, mm], wsl, pb)
                        act(lambda a: a.copy(out=m_qT[:, j, :], in_=P[pb][:]), [PS[pb]], [sl["qT"]])
                wt, wsl = wload(wcols(w_in, C_AK, 128), "")
                pb = nbank(); fmm(wt, wsl, pb)
                act(lambda a: a.copy(out=m_kT[:, 1:5, :], in_=P[pb][:].rearrange("p (t k) -> p t k", k=128)), [PS[pb]], [sl["kT"]])
                for hp in range(2):
                    wt, wsl = wload(wcols(w_in, C_MQ + hp * 256, 256), "")
                    for mm in range(2):
                        h = hp * 2 + mm
                        pb = nbank(); fmm(wt[:, :, mm * 128:(mm + 1) * 128], wsl, pb)
                        act(lambda a: a.copy(out=m_mqT[:, h, :], in_=P[pb][:]), [PS[pb]], [sl["mqT"]])
                for hp in range(2):
                    wt, wsl = wload(wcols(w_in, C_MK + hp * 256, 256), "")
                    for mm in range(2):
                        h = hp * 2 + mm
                        pb = nbank(); fmm(wt[:, :, mm * 128:(mm + 1) * 128], wsl, pb)
                        act(lambda a: a.activation(out=m_mkT[:, h, :], in_=P[pb][:], func=AF.Copy, scale=128.0 ** -0.5), [PS[pb]], [sl["mkT"]])
                gate_sb = sm[:, 112:144].rearrange("p (t k) -> p t k", k=8)

                def tproj(c0, n, evac):
                    wt, wsl = wload(wcols(w_in, c0, n), "")
                    for t in tiles:
                        pb = nbank()
                        for c in range(NCH):
                            pe(lambda tt: tt.matmul(P[pb][:, 0:n], lhsT=xT[:, c, t * 128:(t + 1) * 128], rhs=wt[:, c, :], start=(c == 0), stop=(c == NCH - 1)),
                               [wsl, s_xT], [PS[pb]], inc=(c == NCH - 1))
                        evac(t, pb)
                tproj(C_AV, 128, lambda t, pb: act(lambda a: a.copy(out=m_V[:, 1 + t, :], in_=P[pb][:, 0:128]), [PS[pb]], [sl["V"]]))
                for h in range(4):
                    tproj(C_MV + h * 256, 256, lambda t, pb: act(lambda a: a.copy(out=m_va[:, t, h, 0:256], in_=P[pb][:, 0:256]), [PS[pb]], [sl["va"]]))
                for q in range(4):
                    def ev_mo(t, pb):
                        act(lambda a: a.activation(out=m_gs[:, t, q * 256:(q + 1) * 256], in_=P[pb][:, 0:256], func=AF.Sigmoid), [PS[pb]], [sl["gs"]])
                        dve(lambda v: v.tensor_tensor(out=m_gs[:, t, q * 256:(q + 1) * 256], in0=m_gs[:, t, q * 256:(q + 1) * 256],
                                                      in1=ngB[:, q * 256:(q + 1) * 256], op=ALU.mult), [sl["gs"], s_ng], [sl["gs"]])
                    tproj(C_MO + q * 256, 256, ev_mo)
                tproj(C_MI, 8, lambda t, pb: dve(lambda v: v.tensor_copy(out=gate_sb[:, t, :], in_=P[pb][:, 0:8]), [PS[pb]], [s_sm]))

                for t in tiles:
                    qc = slice(t * 128, (t + 1) * 128)
                    first_tile = (blk == 0 and t == 0)
                    for g in range(2):
                        pr = slice(g * 64, (g + 1) * 64)
                        for half in range(2):
                            hh = g * 8 + half * 4
                            for kt in range(2):
                                pb = nbank((1, 2))
                                pe(lambda tt: tt.matmul(P[pb][:], lhsT=m_kT[pr, t + kt, :], rhs=m_qT[pr, half * 4:half * 4 + 4, qc],
                                                        start=True, stop=True), [sl["kT"], sl["qT"]], [PS[pb]])
                                dve(lambda v: v.scalar_tensor_tensor(out=m_sc, in0=P[pb][:], scalar=0.125,
                                                                     in1=abias[:, kt, hh:hh + 4, :].rearrange("p h k -> p (h k)"),
                                                                     op0=ALU.mult, op1=ALU.add), [PS[pb], s_ab], [sl["sc"]])
                                if kt == 0 and first_tile:
                                    act(lambda a: a.activation(out=m_PT[:, kt, :], in_=m_sc, func=AF.Exp, bias=kvb[:, 0:1], scale=1.0),
                                        [sl["sc"], s_kvb], [sl["PT"]])
                                else:
                                    act(lambda a: a.activation(out=m_PT[:, kt, :], in_=m_sc, func=AF.Exp), [sl["sc"]], [sl["PT"]])
                            for kt in range(2):
                                pe(lambda tt: tt.matmul(P[4][:], lhsT=m_V[:, t + kt, :], rhs=m_PT[:, kt, :], start=(kt == 0), stop=(kt == 1)),
                                   [sl["V"], sl["PT"]], [PS[4]], inc=(kt == 1))
                            for kt in range(2):
                                pe(lambda tt: tt.matmul(P[5][:], lhsT=onesb[:], rhs=m_PT[:, kt, :], start=(kt == 0), stop=(kt == 1)),
                                   [s_idb, sl["PT"]], [PS[5]], inc=(kt == 1))
                            for hq in range(4):
                                dve(lambda v: v.tensor_scalar(out=m_dn[pr, hq * 128:(hq + 1) * 128], in0=P[5][pr, hq * 128:(hq + 1) * 128],
                                                              scalar1=esink[pr, hh + hq:hh + hq + 1], scalar2=None, op0=ALU.add),
                                    [PS[5], s_es], [sl["dn"]])
                            dve(lambda v: v.reciprocal(out=m_dn[pr, :], in_=m_dn[pr, :]), [sl["dn"]], [sl["dn"]])
                            dve(lambda v: v.tensor_tensor(out=m_attnT[pr, half * 4:half * 4 + 4, qc], in0=P[4][pr, :].rearrange("p (h k) -> p h k", k=128),
                                                          in1=m_dn[pr, :].rearrange("p (h k) -> p h k", k=128), op=ALU.mult),
                                [PS[4], sl["dn"]], [sl["attnT"]])

                for t in tiles:
                    qc = slice(t * 128, (t + 1) * 128)
                    mlstm_gates(gate_sb[:, t, :], s_sm, 7, None)
                    for h in range(4):
                        dve(lambda v: v.tensor_scalar(out=m_rE[:, h, :], in0=idf, scalar1=sm[:, 20 + h:21 + h], scalar2=None, op0=ALU.mult),
                            [s_c, s_sm], [sl["rE"]])
                    pe(lambda tt: tt.matmul(P[3][:], lhsT=onesf, rhs=m_rE[:].rearrange("p h k -> p (h k)"), start=True, stop=True),
                       [s_c, sl["rE"]], [PS[3]])
                    act(lambda a: a.activation(out=m_eb[:].rearrange("p h k -> p (h k)"), in_=P[3][:], func=AF.Exp, scale=-1.0), [PS[3]], [sl["eb"]])
                    for h in range(4):
                        act(lambda a: a.activation(out=m_Dt[:, h, :], in_=P[3][:, h * 128:(h + 1) * 128], func=AF.Exp,
                                                   bias=sm[:, 24 + h:25 + h], scale=-1.0), [PS[3], s_sm], [sl["Dt"]])
                    for h in range(4):
                        dve(lambda v: v.tensor_tensor(out=m_Dt[:, h, :], in0=m_Dt[:, h, :], in1=tri, op=ALU.mult), [sl["Dt"], s_c], [sl["Dt"]])
                    for h in range(4):
                        pe(lambda tt: tt.matmul(P[1][:, h * 128:(h + 1) * 128], lhsT=m_mkT[:, h, qc], rhs=m_mqT[:, h, qc], start=True, stop=True),
                           [sl["mkT"], sl["mqT"]], [PS[1]], inc=(h == 3))
                    dve(lambda v: v.tensor_tensor(out=m_Sq[:].rearrange("p h k -> p (h k)"), in0=P[1][:],
                                                  in1=m_Dt[:].rearrange("p h k -> p (h k)"), op=ALU.mult), [PS[1], sl["Dt"]], [sl["Sq"]])
                    dve(lambda v: v.tensor_tensor(out=m_qd[:], in0=m_mqT[:, :, qc], in1=m_eb[:], op=ALU.mult), [sl["mqT"], sl["eb"]], [sl["qd"]])
                    for h in range(4):
                        pb = 4 if h < 2 else 5
                        osl = P[pb][:, (h % 2) * 256:(h % 2 + 1) * 256]
                        pe(lambda tt: tt.matmul(osl, lhsT=m_Sq[:, h, :], rhs=m_va[:, t, h, 0:256], start=True, stop=False),
                           [sl["Sq"], sl["va"]], [PS[pb]], inc=False)
                        pe(lambda tt: tt.matmul(osl, lhsT=m_qd[:, h, :], rhs=Cb[:, h, 0:256], start=False, stop=True),
                           [sl["qd"], s_Cb], [PS[pb]], inc=(h % 2 == 1))
                    for h in range(4):
                        pe(lambda tt: tt.matmul(P[7][:, 32 + h:33 + h], lhsT=m_Sq[:, h, :], rhs=m_va[:, t, h, 256:257], start=True, stop=False),
                           [sl["Sq"], sl["va"]], [PS[7]], inc=False)
                        pe(lambda tt: tt.matmul(P[7][:, 32 + h:33 + h], lhsT=m_qd[:, h, :], rhs=Cb[:, h, 256:257], start=False, stop=True),
                           [sl["qd"], s_Cb], [PS[7]], inc=(h == 3))
                    act(lambda a: a.activation(out=sm[:, 36:40], in_=P[7][:, 32:36], func=AF.Abs), [PS[7]], [s_sm])
                    dve(lambda v: v.tensor_scalar(out=sm[:, 36:40], in0=sm[:, 36:40], scalar1=1.0, scalar2=None, op0=ALU.max), [s_sm], [s_sm])
                    dve(lambda v: v.reciprocal(out=sm[:, 36:40], in_=sm[:, 36:40]), [s_sm], [s_sm])
                    for h in range(4):
                        pb = 4 if h < 2 else 5
                        act(lambda a: a.activation(out=junk[:], in_=P[pb][:, (h % 2) * 256:(h % 2 + 1) * 256], func=AF.Square,
                                                   accum_out=sm[:, 40 + h:41 + h]), [PS[pb]], [s_junk, s_sm])
                    dve(lambda v: v.tensor_tensor(out=sm[:, 48:52], in0=sm[:, 40:44], in1=sm[:, 36:40], op=ALU.mult), [s_sm], [s_sm])
                    dve(lambda v: v.tensor_tensor(out=sm[:, 48:52], in0=sm[:, 48:52], in1=sm[:, 36:40], op=ALU.mult), [s_sm], [s_sm])
                    act(lambda a: a.activation(out=sm[:, 48:52], in_=sm[:, 48:52], func=AF.Sqrt, bias=1e-6, scale=1.0 / 256.0), [s_sm], [s_sm])
                    dve(lambda v: v.reciprocal(out=sm[:, 48:52], in_=sm[:, 48:52]), [s_sm], [s_sm])
                    dve(lambda v: v.tensor_tensor(out=sm[:, 44:48], in0=sm[:, 48:52], in1=sm[:, 36:40], op=ALU.mult), [s_sm], [s_sm])
                    for h in range(4):
                        pb = 4 if h < 2 else 5
                        dve(lambda v: v.scalar_tensor_tensor(out=m_mh[:, h * 256:(h + 1) * 256], in0=P[pb][:, (h % 2) * 256:(h % 2 + 1) * 256],
                                                             scalar=sm[:, 44 + h:45 + h], in1=m_gs[:, t, h * 256:(h + 1) * 256],
                                                             op0=ALU.mult, op1=ALU.mult), [PS[pb], s_sm, sl["gs"]], [sl["mh"]])
                    transpose_to(m_mh, sl["mh"], lambda c0, n: m_mhT[:, c0:c0 + n, qc], sl["mhT"], nchunks=8, pb=0)
                    pbv = P[0][:].bitcast(BF16)
                    for h in range(4):
                        pe(lambda tt: tt.transpose(out=pbv[:, h * 128:(h + 1) * 128], in_=m_mkT[:, h, qc], identity=idb[:]),
                           [sl["mkT"], s_idb], [PS[0]], inc=(h == 3))
                    mlstm_update(pbv[:, 0:512].rearrange("p (h k) -> p h k", k=128), PS[0], m_kw, sl["kw"], m_va[:, t], sl["va"], 4, 5, 7)
                act(lambda a: a.copy(out=kT_halo, in_=m_kT[:, 4, :]), [sl["kT"]], [s_halo])
                act(lambda a: a.copy(out=V_halo, in_=m_V[:, 4, :]), [sl["V"]], [s_halo])

                Sched.alias([sl["qT"], sl["mqT"], sl["mkT"]], [sl["mgT"]])
                for mp in range(8):
                    wga, s1 = wload(wcols(w_in, C_GA + mp * 256, 256), "")
                    wgb, s2 = wload(wcols(w_in, C_GB + mp * 256, 256), "")
                    i = ring["i"] % NRM; ring["i"] += 1
                    s3 = ring["slots"][i]
                    wa = ring["aps"][i][:, 0:2048].rearrange("p (j n) -> p j n", n=256)
                    for gg in range(2):
                        S.dma("pool", wa[gg * 64:(gg + 1) * 64], wba4[gg][:, :, mp * 256:(mp + 1) * 256], writes=[s3], dslot=s3)
                    wb, s4 = wload(wbm3[:, :, mp * 256:(mp + 1) * 256], "")
                    for mm in range(2):
                        m = mp * 2 + mm
                        ms = slice(mm * 128, (mm + 1) * 128)
                        b1, b2, b3, b4 = (1, 2, 3, 6) if m % 2 == 0 else (4, 5, 7, 0)
                        for c in range(NCH):
                            pe(lambda tt: tt.matmul(P[b1][:], lhsT=wga[:, c, ms], rhs=xT[:, c, :], start=(c == 0), stop=(c == NCH - 1)),
                               [s1, s_xT], [PS[b1]], inc=(c == NCH - 1))
                        for c in range(NCH):
                            pe(lambda tt: tt.matmul(P[b2][:], lhsT=wgb[:, c, ms], rhs=xT[:, c, :], start=(c == 0), stop=(c == NCH - 1)),
                               [s2, s_xT], [PS[b2]], inc=(c == NCH - 1))
                        for c in range(8):
                            pe(lambda tt: tt.matmul(P[b3][:], lhsT=wa[:, c, ms], rhs=m_attnT[:, c, :], start=(c == 0), stop=(c == 7)),
                               [s3, sl["attnT"]], [PS[b3]], inc=(c == 7))
                        for c in range(8):
                            pe(lambda tt: tt.matmul(P[b4][:], lhsT=wb[:, c, ms], rhs=m_mhT[:, c, :], start=(c == 0), stop=(c == 7)),
                               [s4, sl["mhT"]], [PS[b4]], inc=(c == 7))
                        act(lambda a: a.activation(out=m_t1, in_=P[b1][:], func=AF.Sigmoid), [PS[b1]], [sl["t1"]])
                        act(lambda a: a.activation(out=m_t2, in_=P[b2][:], func=AF.Sigmoid), [PS[b2]], [sl["t2"]])
                        dve(lambda v: v.tensor_tensor(out=m_t1, in0=m_t1, in1=P[b3][:], op=ALU.mult), [sl["t1"], PS[b3]], [sl["t1"]])
                        dve(lambda v: v.tensor_tensor(out=m_t2, in0=m_t2, in1=P[b4][:], op=ALU.mult), [sl["t2"], PS[b4]], [sl["t2"]])
                        dve(lambda v: v.tensor_tensor(out=m_mgT[:, m, :], in0=m_t1, in1=m_t2, op=ALU.add), [sl["t1"], sl["t2"]], [sl["mgT"]])
                for nb in range(8):
                    wt, wsl = wload(wcols(w_out, nb * 256, 256), "")
                    for t in tiles:
                        pb = nbank()
                        for c in range(NCH):
                            pe(lambda tt: tt.matmul(P[pb][:, 0:256], lhsT=m_mgT[:, c, t * 128:(t + 1) * 128], rhs=wt[:, c, :], start=(c == 0), stop=(c == NCH - 1)),
                               [wsl, sl["mgT"]], [PS[pb]], inc=(c == NCH - 1))
                        dve(lambda v: v.scalar_tensor_tensor(out=R[:, t, nb * 256:(nb + 1) * 256], in0=P[pb][:, 0:256], scalar=IALPHA,
                                                             in1=R[:, t, nb * 256:(nb + 1) * 256], op0=ALU.mult, op1=ALU.add),
                            [PS[pb], s_R[t]], [s_R[t]])
                load_ln(2)
                if SPARSE:
                    X1tok = ws(38176, 4 * D).rearrange("p (t k) -> p t k", k=D)
                    s_x1t = Slot("x1t")
                    Sched.alias([sl["attnT"], sl["mhT"]], [s_x1t])
                for t in tiles:
                    if SPARSE:
                        layer_norm(t, EPS_A, X1tok[:, t, :], s_x1t)
                        transpose_to(X1tok[:, t, :], s_x1t, lambda c0, n: xT[:, c0:c0 + n, t * 128:(t + 1) * 128], s_xT, pb=0)
                    else:
                        layer_norm(t, EPS_A, m_xb, sl["xb"])
                        transpose_to(m_xb, sl["xb"], lambda c0, n: xT[:, c0:c0 + n, t * 128:(t + 1) * 128], s_xT, pb=0)

                if SPARSE:
                    o = 46368
                    x_mskf = take(256, F32).rearrange("p (t k) -> p t k", k=64)
                    x_rank = take(256, F32).rearrange("p (t k) -> p t k", k=64)
                    x_mskb = take(256).rearrange("p (t k) -> p t k", k=64)
                    s_msk = Slot("msk")
                    Sched.alias([sl["xb"]], [s_msk])
                for t in range(4):
                    pb = nbank()
                    for c in range(NCH):
                        pe(lambda tt: tt.matmul(P[pb][:, 0:NE], lhsT=xT[:, c, t * 128:(t + 1) * 128], rhs=wrt[:, c, :], start=(c == 0), stop=(c == NCH - 1)),
                           [s_xT, s_wrt], [PS[pb]], inc=(c == NCH - 1))
                    sc_ = m_sc[:, 0:64]; sel_ = m_sc[:, 64:128]; top_ = m_sc[:, 128:136]; msk_ = m_sc[:, 192:256]
                    act(lambda a: a.activation(out=sc_, in_=P[pb][:, 0:NE], func=AF.Sigmoid), [PS[pb]], [sl["sc"]])
                    dve(lambda v: v.tensor_tensor(out=sel_, in0=sc_, in1=brB[:], op=ALU.add), [sl["sc"], s_br], [sl["sc"]])
                    dve(lambda v: v.max(out=top_, in_=sel_), [sl["sc"]], [sl["sc"]])
                    dve(lambda v: v.tensor_scalar(out=msk_, in0=sel_, scalar1=top_[:, 7:8], scalar2=None, op0=ALU.is_ge), [sl["sc"]], [sl["sc"]])
                    if SPARSE:
                        dve(lambda v: v.tensor_copy(out=x_mskf[:, t, :], in_=msk_), [sl["sc"]], [s_msk])
                        dve(lambda v: v.tensor_copy(out=x_mskb[:, t, :], in_=msk_), [sl["sc"]], [s_msk])
                    dve(lambda v: v.tensor_tensor(out=msk_, in0=msk_, in1=sc_, op=ALU.mult), [sl["sc"]], [sl["sc"]])
                    dve(lambda v: v.reduce_sum(out=sm[:, 150:151], in_=msk_, axis=AX.X), [sl["sc"]], [s_sm])
                    dve(lambda v: v.reciprocal(out=sm[:, 150:151], in_=sm[:, 150:151]), [s_sm], [s_sm])
                    dve(lambda v: v.tensor_scalar(out=gw[:, t, 0:NE], in0=msk_, scalar1=sm[:, 150:151], scalar2=2.5 * IALPHA,
                                                  op0=ALU.mult, op1=ALU.mult), [sl["sc"], s_sm], [s_gw])
                if SPARSE:
                    for t in range(4):
                        pb = nbank()
                        seq = [(stri[:], x_mskb[:, t, :])] + [(onesb[:], x_mskb[:, tp, :]) for tp in range(t)]
                        for i, (lt, rh) in enumerate(seq):
                            pe(lambda tt: tt.matmul(P[pb][:, 0:NE], lhsT=lt, rhs=rh, start=(i == 0), stop=(i == len(seq) - 1)),
                               [s_idb, s_msk], [PS[pb]], inc=(i == len(seq) - 1))
                        dve(lambda v: v.tensor_copy(out=x_rank[:, t, :], in_=P[pb][:, 0:NE]), [PS[pb]], [s_msk])
                    pb = nbank()
                    for t in range(4):
                        pe(lambda tt: tt.matmul(P[pb][:, 0:NE], lhsT=onesb[:], rhs=x_mskb[:, t, :], start=(t == 0), stop=(t == 3)),
                           [s_idb, s_msk], [PS[pb]], inc=(t == 3))
                    dve(lambda v: v.tensor_reduce(out=sm[:, 152:153], in_=P[pb][:, 0:NE], axis=AX.X, op=ALU.max), [PS[pb]], [s_sm])
                    dve(lambda v: v.tensor_tensor(out=flagmax[:], in0=flagmax[:], in1=sm[:, 152:153], op=ALU.max), [s_sm, s_flag], [s_flag])

                wq = "pool"

                def eload(e):
                    if PRECAST and (blk >= 1 or (pc_group[e] + 1) * 24 <= pc_prefix_issued[0]):
                        wg3 = wbg[e].rearrange("(c p) n -> p c n", p=128)
                        wu3 = wbu[e].rearrange("(c p) n -> p c n", p=128)
                        wd3 = wbd[e].rearrange("(c p) n -> p c n", p=128)
                        xr = [s_pc[pc_group[e]]]
                    else:
                        wg3 = w_eg[e].rearrange("(c p) n -> p c n", p=128)
                        wu3 = w_eu[e].rearrange("(c p) n -> p c n", p=128)
                        wd3 = w_ed[e].rearrange("(c p) n -> p c n", p=128)
                        xr = []
                    wgs = [wload(wg3[:, :, hp * 256:(hp + 1) * 256], "", q=wq, xr=xr) for hp in range(2)]
                    wus = [wload(wu3[:, :, hp * 256:(hp + 1) * 256], "", q=wq, xr=xr) for hp in range(2)]
                    wds = [wload(wd3[:, hp * 2:hp * 2 + 2, :], "", q=wq, xr=xr) for hp in range(2)]
                    return wgs, wus, wds

                def dense_expert(e, e_h1, e_sg, s_h1, s_sg):
                    wgs, wus, wds = eload(e)
                    for m in range(4):
                        pg_, pu_ = (0, 1) if m % 2 == 0 else (2, 3)
                        wg_, sg_w = wgs[m // 2]; wu_, su_w = wus[m // 2]
                        ms = slice((m % 2) * 128, (m % 2 + 1) * 128)
                        for c in range(NCH):
                            pe(lambda tt: tt.matmul(P[pg_][:], lhsT=wg_[:, c, ms], rhs=xT[:, c, :], start=(c == 0), stop=(c == NCH - 1)),
                               [sg_w, s_xT], [PS[pg_]], inc=(c == NCH - 1))
                        for c in range(NCH):
                            pe(lambda tt: tt.matmul(P[pu_][:], lhsT=wu_[:, c, ms], rhs=xT[:, c, :], start=(c == 0), stop=(c == NCH - 1)),
                               [su_w, s_xT], [PS[pu_]], inc=(c == NCH - 1))
                        act(lambda a: a.activation(out=e_sg, in_=P[pg_][:], func=AF.Silu), [PS[pg_]], [s_sg])
                        dve(lambda v: v.tensor_tensor(out=e_h1[:, m, :], in0=e_sg, in1=P[pu_][:], op=ALU.mult), [s_sg, PS[pu_]], [s_h1])
                    for t in range(4):
                        for nb in range(4):
                            for m in range(4):
                                wd_, sd_w = wds[m // 2]
                                pe(lambda tt: tt.matmul(P[4 + nb][:], lhsT=e_h1[:, m, t * 128:(t + 1) * 128], rhs=wd_[:, m % 2, nb * 512:(nb + 1) * 512],
                                                        start=(m == 0), stop=(m == 3)), [s_h1, sd_w], [PS[4 + nb]], inc=(m == 3))
                            dve(lambda v: v.scalar_tensor_tensor(out=R[:, t, nb * 512:(nb + 1) * 512], in0=P[4 + nb][:], scalar=gw[:, t, e:e + 1],
                                                                 in1=R[:, t, nb * 512:(nb + 1) * 512], op0=ALU.mult, op1=ALU.add),
                                [PS[4 + nb], s_gw, s_R[t]], [s_R[t]])

                if not SPARSE:
                    o = NRE * RING_E
                    e_h1 = take(4 * 512).rearrange("p (m k) -> p m k", k=512)
                    e_sg = take(512, F32)
                    assert o <= HALO
                    s_h1 = Slot("h1"); s_sg = Slot("sg")
                    ring_setup(NRE)
                    moe_slots = [s_h1, s_sg] + ring["slots"]
                    Sched.alias(list(sl.values()) + ring_sl[:NRM], moe_slots)
                    for e in range(NE + 1):
                        dense_expert(e, e_h1, e_sg, s_h1, s_sg)
                    ple_olds = [s_h1, s_sg]
                    PLE_O = NRE * RING_E
                else:
                    NRS = 8
                    ring_setup(NRS)
                    o = NRS * RING_E
                    x_Sel = [take(512).rearrange("p (t k) -> p t k", k=128) for _ in range(2)]
                    x_SelT = take(512).rearrange("p (t k) -> p t k", k=128)
                    x_h1 = take(512); x_h1T = take(512).rearrange("p (m k) -> p m k", k=128)
                    x_yb = take(2048)
                    assert o <= 38176, o
                    o = 49440
                    oB = o
                    x_XeT = [take(2048).rearrange("p (c k) -> p c k", k=128) for _ in range(2)]
                    x_sg = take(512, F32)
                    assert o <= HALO, o
                    e_h1 = ws(oB, 2048).rearrange("p (m k) -> p m k", k=512)
                    e_sg = ws(oB + 2048, 512, F32)
                    s_h1 = Slot("h1"); s_sg = Slot("sg")
                    s_Sel = [Slot(), Slot()]; s_SelT = Slot(); s_xh1 = Slot(); s_xh1T = Slot(); s_yb = Slot()
                    s_XeT = [Slot(), Slot()]; s_xsg = Slot()
                    first = [s_h1, s_sg] + s_Sel + [s_SelT, s_xh1, s_xh1T, s_yb] + ring["slots"]
                    Sched.alias([v for k, v in sl.items() if k not in ("attnT", "mhT", "xb")] + ring_sl[:NRM], first)
                    dense_expert(NE, e_h1, e_sg, s_h1, s_sg)
                    Sched.alias([s_h1, s_sg], s_XeT + [s_xsg])
                    moe_slots = first + s_XeT + [s_xsg, s_x1t, s_msk]

                    def xSel(e):
                        b = e % 2
                        for t in range(4):
                            dve(lambda v: v.tensor_scalar(out=x_Sel[b][:, t, :], in0=iotaf, scalar1=x_rank[:, t, e:e + 1], scalar2=x_mskf[:, t, e:e + 1],
                                                          op0=ALU.is_equal, op1=ALU.mult), [s_c, s_msk], [s_Sel[b]])

                    def xGather(e):
                        b = e % 2
                        for g4 in range(4):
                            pb = g4 % 2
                            for cc in range(4):
                                c = g4 * 4 + cc
                                for t in range(4):
                                    pe(lambda tt: tt.matmul(P[pb][:, cc * 128:(cc + 1) * 128], lhsT=X1tok[:, t, c * 128:(c + 1) * 128], rhs=x_Sel[b][:, t, :],
                                                            start=(t == 0), stop=(t == 3)), [s_x1t, s_Sel[b]], [PS[pb]], inc=(cc == 3 and t == 3))
                            act(lambda a: a.copy(out=x_XeT[b][:, g4 * 4:(g4 + 1) * 4, :], in_=P[pb][:].rearrange("p (c k) -> p c k", k=128)),
                                [PS[pb]], [s_XeT[b]])

                    def xFFN(e, wgs, wus, between=None):
                        b = e % 2
                        q = 0
                        for (bank, ws_) in ((2, wgs), (3, wus)):
                            for hp in range(2):
                                w_, wsl_ = ws_[hp]
                                for c in range(NCH):
                                    pe(lambda tt: tt.matmul(P[bank][:, hp * 256:(hp + 1) * 256], lhsT=x_XeT[b][:, c, :], rhs=w_[:, c, :],
                                                            start=(c == 0), stop=(c == NCH - 1)), [s_XeT[b], wsl_], [PS[bank]], inc=(c == NCH - 1))
                                if between is not None:
                                    between(q)
                                q += 1
                        act(lambda a: a.activation(out=x_sg, in_=P[2][:], func=AF.Silu), [PS[2]], [s_xsg])
                        dve(lambda v: v.tensor_tensor(out=x_h1, in0=x_sg, in1=P[3][:], op=ALU.mult), [s_xsg, PS[3]], [s_xh1])

                    def xT1(e):
                        transpose_to(x_h1, s_xh1, lambda c0, n: x_h1T[:, c0:c0 + n, :], s_xh1T, nchunks=4, pb=4)

                    def xY(e, wds):
                        for nb in range(4):
                            pb = 5 + nb % 2
                            for m in range(4):
                                wd_, sd_w = wds[m // 2]
                                pe(lambda tt: tt.matmul(P[pb][:], lhsT=x_h1T[:, m, :], rhs=wd_[:, m % 2, nb * 512:(nb + 1) * 512], start=(m == 0), stop=(m == 3)),
                                   [s_xh1T, sd_w], [PS[pb]], inc=(m == 3))
                            act(lambda a: a.copy(out=x_yb[:, nb * 512:(nb + 1) * 512], in_=P[pb][:]), [PS[pb]], [s_yb])

                    def xT2(e):
                        b = e % 2
                        transpose_to(x_Sel[b][:].rearrange("p t k -> p (t k)"), s_Sel[b], lambda c0, n: x_SelT[:, c0:c0 + n, :], s_SelT, nchunks=4, pb=4)

                    sc_rot = {"i": 0}

                    def xScatterT(e, t):
                        for nb in range(4):
                            pb = (7, 5, 6, 4)[sc_rot["i"] % 4]; sc_rot["i"] += 1
                            pe(lambda tt: tt.matmul(P[pb][:], lhsT=x_SelT[:, t, :], rhs=x_yb[:, nb * 512:(nb + 1) * 512], start=True, stop=True),
                               [s_SelT, s_yb], [PS[pb]])
                            dve(lambda v: v.scalar_tensor_tensor(out=R[:, t, nb * 512:(nb + 1) * 512], in0=P[pb][:], scalar=gw[:, t, e:e + 1],
                                                                 in1=R[:, t, nb * 512:(nb + 1) * 512], op0=ALU.mult, op1=ALU.add),
                                [PS[pb], s_gw, s_R[t]], [s_R[t]])

                    xSel(0); xGather(0)
                    wcur = eload(0)
                    for e in range(NE):
                        if e + 1 < NE:
                            xSel(e + 1)
                        xFFN(e, wcur[0], wcur[1], between=(lambda q, ep=e - 1: xScatterT(ep, q)) if e >= 1 else None)
                        wds = wcur[2]
                        if e + 1 < NE:
                            xGather(e + 1)
                        xT1(e)
                        xY(e, wds)
                        if e + 1 < NE:
                            wcur = eload(e + 1)
                        if blk == 0:
                            pc_issue(1)
                        xT2(e)
                    for t in range(4):
                        xScatterT(NE - 1, t)
                    if blk == 0:
                        pc_issue(len(pc_list))
                    ple_olds = [s_Sel[0], s_Sel[1], s_SelT, s_xh1, s_xh1T, s_yb]
                    PLE_O = NRS * RING_E
                o = PLE_O
                p_xb2 = take(D); p_pb = take(256); p_pT = take(2 * 512).rearrange("p (c k) -> p c k", k=512)
                p_pf = take(256, F32); p_sg = take(256, F32)
                assert o <= (38176 if SPARSE else HALO), o
                s_xb2 = Slot(); s_pb = Slot(); s_pT = Slot(); s_psg = Slot()
                Sched.alias(ple_olds, [s_xb2, s_pb, s_pT, s_pf, s_psg])
                moe_slots = moe_slots + [s_xb2, s_pb, s_pT, s_pf, s_psg]
                load_ln(4)
                for t in range(4):
                    layer_norm(t, EPS_A, p_xb2, s_xb2)
                    transpose_to(p_xb2, s_xb2, lambda c0, n: xT[:, c0:c0 + n, t * 128:(t + 1) * 128], s_xT, pb=0)
                    S.dma("sp", p_pf, pin[(blk * 4 + t) * 128:(blk * 4 + t + 1) * 128, :], writes=[s_pf], dslot=s_pf)
                    act(lambda a: a.copy(out=p_pb, in_=p_pf), [s_pf], [s_pb])
                    transpose_to(p_pb, s_pb, lambda c0, n: p_pT[:, c0:c0 + n, t * 128:(t + 1) * 128], s_pT, nchunks=2, pb=0)
                wpp3 = w_pp.rearrange("(c p) n -> p c n", p=128)
                for nb in range(8):
                    wt, wsl = wload(wcols(w_pg, nb * 256, 256), "")
                    wp, wpl = wload(wpp3[:, :, nb * 256:(nb + 1) * 256], "")
                    for t in range(4):
                        b1, b2 = ((1, 2), (3, 6), (4, 5), (7, 0))[t]
                        for c in range(NCH):
                            pe(lambda tt: tt.matmul(P[b1][:, 0:256], lhsT=xT[:, c, t * 128:(t + 1) * 128], rhs=wt[:, c, :], start=(c == 0), stop=(c == NCH - 1)),
                               [wsl, s_xT], [PS[b1]], inc=(c == NCH - 1))
                        for c in range(2):
                            pe(lambda tt: tt.matmul(P[b2][:, 0:256], lhsT=p_pT[:, c, t * 128:(t + 1) * 128], rhs=wp[:, c, :], start=(c == 0), stop=(c == 1)),
                               [wpl, s_pT], [PS[b2]], inc=(c == 1))
                        act(lambda a: a.activation(out=p_sg, in_=P[b1][:, 0:256], func=AF.Sigmoid), [PS[b1]], [s_psg])
                        dve(lambda v: v.scalar_tensor_tensor(out=p_sg, in0=P[b2][:, 0:256], scalar=IALPHA, in1=p_sg, op0=ALU.mult, op1=ALU.mult),
                            [PS[b2], s_psg], [s_psg])
                        dve(lambda v: v.tensor_tensor(out=R[:, t, nb * 256:(nb + 1) * 256], in0=R[:, t, nb * 256:(nb + 1) * 256], in1=p_sg, op=ALU.add),
                            [s_psg, s_R[t]], [s_R[t]])
                load_ln(6)
                for t in range(4):
                    layer_norm(t, EPS_A, p_xb2, s_xb2)
                    S.dma("sp", out[(blk * 4 + t) * 128:(blk * 4 + t + 1) * 128, :], R[:, t, :], reads=[s_R[t]], dslot=s_xl[t])
            S.dma("sp", flag_o, flagmax[:], reads=[s_flag], dslot=s_flag)
            S.wait_all("sp", s_R + [s_flag])
        print("built: ops", S.nops, "waits", S.nwaits, flush=True)
    return nc


def _consts():
    cst = np.zeros((128, 4, 128), np.float32)
    cst[:, 3, :] = np.arange(128, dtype=np.float32)[None, :]
    cst[:, 0, :] = np.eye(128, dtype=np.float32)
    cst[:, 1, :] = np.triu(np.ones((128, 128), np.float32))
    cst[:, 2, :] = 1.0
    slopes = np.exp2(-8.0 / 16 * np.arange(1, 17, dtype=np.float32)).astype(np.float32)
    j = np.arange(128)[:, None]; i = np.arange(128)[None, :]
    ab = np.zeros((128, 2, 16, 128), np.float32)
    for kt in range(2):
        dist = (i - j + (128 if kt == 0 else 0)).astype(np.float32)
        ok = (dist >= 0) & (dist < 128)
        for h in range(16):
            ab[:, kt, h, :] = np.where(ok, -slopes[h] * dist, NEGBIG)
    return cst, ab


_CACHE = {}


def kernel(x, p, ln_in_g, ln_in_b, w_in, attn_sinks, mlstm_b_i, mlstm_b_f, mlstm_norm_g,
           w_branch_attn, w_branch_mlstm, w_out, ln_mix_g, ln_mix_b, w_router, b_router,
           w_exp_gate, w_exp_up, w_exp_down, w_sh_gate, w_sh_up, w_sh_down, ln_ffn_g, ln_ffn_b,
           w_ple_proj, w_ple_gate, ln_ple_g, ln_ple_b):
    f = lambda a: np.ascontiguousarray(np.asarray(a, dtype=np.float32))
    x = f(x); p = f(p)
    B, SEQ, _ = x.shape
    NQ = 8 // B
    TOK = SEQ // NQ
    NPRE = (SEQ - TOK) // 128
    def get(sparse):
        key = (TOK, NPRE, sparse)
        if key not in _CACHE:
            _CACHE[key] = build(TOK, NPRE, sparse)
        return _CACHE[key]
    nc = get(True)
    cst, ab = _consts()
    shared = {
        "cst": cst, "abias": ab,
        "lng": np.stack([f(ln_in_g), f(ln_in_b), f(ln_mix_g)[0], f(ln_mix_b)[0], f(ln_ffn_g)[0], f(ln_ffn_b)[0],
                         f(ln_ple_g)[0], f(ln_ple_b)[0]]),
        "w_in": f(w_in)[0], "sinks": f(attn_sinks).reshape(1, 16),
        "bif": np.concatenate([f(mlstm_b_i).reshape(-1), f(mlstm_b_f).reshape(-1)]).reshape(1, 8),
        "ng": f(mlstm_norm_g).reshape(1, 1024),
        "w_ba": f(w_branch_attn)[0], "w_bm": f(w_branch_mlstm)[0], "w_out": f(w_out)[0],
        "w_rt": f(w_router)[0], "b_rt": f(b_router).reshape(1, NE),
        "w_eg": np.concatenate([f(w_exp_gate)[0], f(w_sh_gate)], axis=0),
        "w_eu": np.concatenate([f(w_exp_up)[0], f(w_sh_up)], axis=0),
        "w_ed": np.concatenate([f(w_exp_down)[0], f(w_sh_down)], axis=0),
        "w_pp": f(w_ple_proj)[0], "w_pg": f(w_ple_gate)[0],
    }
    in_maps = []
    for c in range(8):
        b, j = c // NQ, c % NQ
        end = (j + 1) * TOK
        xin = np.zeros((SEQ, D), np.float32)
        xin[SEQ - end:] = x[b, :end]
        vm = np.zeros((128, NPRE + 1), np.float32)
        nvalid = (j * TOK) // 128
        if nvalid > 0:
            vm[:, NPRE - nvalid:NPRE] = 1.0
        vm[:, NPRE] = 1.0 if j > 0 else 0.0
        m = dict(shared)
        m["xin"] = xin; m["vmk"] = vm
        m["pin"] = np.ascontiguousarray(p[0, b, j * TOK:(j + 1) * TOK])
        in_maps.append(m)
    res = run_bass_kernel_spmd(nc, in_maps, core_ids=list(range(8)))
    if max(float(np.asarray(r["flag"]).max()) for r in res.results) > 128.5:
        res = run_bass_kernel_spmd(get(False), in_maps, core_ids=list(range(8)))
    outp = np.zeros((B, SEQ, D), np.float32)
    for c in range(8):
        b, j = c // NQ, c % NQ
        outp[b, j * TOK:(j + 1) * TOK] = np.asarray(res.results[c]["out"], dtype=np.float32)
    return outp
```

```python
import numpy as np
from contextlib import ExitStack
import concourse.bass as bass
import concourse.mybir as mybir
from concourse.bass_utils import run_bass_kernel_spmd

F32 = mybir.dt.float32
BF16 = mybir.dt.bfloat16
AF = mybir.ActivationFunctionType
ALU = mybir.AluOpType
AX = mybir.AxisListType

D = 2048
NCH = 16
NE = 64
ALPHA = 2.0 ** 0.25
IALPHA = 1.0 / ALPHA
EPS_A = 1e-5 / (ALPHA * ALPHA)
C_AQ, C_AK, C_AV, C_MQ, C_MK, C_MV, C_MO, C_MI, C_GA, C_GB = 0, 1024, 1152, 1280, 1792, 2304, 3328, 4352, 4360, 6408
NEGBIG = -30000.0


class Slot:
    __slots__ = ("name", "w", "r", "dsem", "dcnt")

    def __init__(self, name=""):
        self.name = name; self.w = None; self.r = {}; self.dsem = None; self.dcnt = 0


class Eng:
    def __init__(self, obj, sem):
        self.obj = obj; self.sem = sem; self.n = 0; self.waited = {}; self.own = {id(sem)}


class Sched:
    def __init__(self, nc, sems):
        self.nc = nc
        self.E = {"pe": Eng(nc.tensor, sems["pe"]), "dve": Eng(nc.vector, sems["dve"]),
                  "act": Eng(nc.scalar, sems["act"]), "pool": Eng(nc.gpsimd, sems["pool"]),
                  "sp": Eng(nc.sync, sems["sp"])}
        self.nops = 0; self.nwaits = 0

    def new_epoch(self, sems):
        for k, e in self.E.items():
            e.sem = sems[k]; e.n = 0; e.own.add(id(e.sem))

    def _wait(self, e, deps):
        best = {}
        for (sem, val, raw) in deps:
            if id(sem) in e.own and not raw:
                continue
            k = id(sem)
            if e.waited.get(k, 0) >= val:
                continue
            if k not in best or best[k][1] < val:
                best[k] = (sem, val)
        for k, (sem, val) in best.items():
            e.obj.wait_ge(sem, val); e.waited[k] = val; self.nwaits += 1

    def _deps(self, reads, writes):
        deps = []
        for s in reads:
            if s.w is not None:
                deps.append((s.w[0], s.w[1], True))
        for s in writes:
            if s.w is not None:
                deps.append((s.w[0], s.w[1], False))
            deps.extend((a, b, False) for (a, b) in s.r.values())
        return deps

    def op(self, eng, fn, reads=(), writes=(), inc=True):
        e = self.E[eng]
        self._wait(e, self._deps(reads, writes))
        inst = fn(e.obj)
        ev = (e.sem, e.n + 1)
        if inc:
            inst.then_inc(e.sem, 1); e.n += 1
        for s in reads:
            s.r[id(e.sem)] = ev
        for s in writes:
            s.w = ev; s.r = {}
        self.nops += 1
        return ev

    def dma(self, eng, out, in_, reads=(), writes=(), dslot=None):
        e = self.E[eng]
        waw = {(id(w.w[0]), w.w[1]) for w in writes if w.w is not None and w.w[0] is dslot.dsem}
        self._wait(e, [d for d in self._deps(reads, writes) if (id(d[0]), d[1]) not in waw])
        dslot.dcnt += 1
        e.obj.dma_start(out=out, in_=in_).then_inc(dslot.dsem, 16)
        ev = (dslot.dsem, 16 * dslot.dcnt)
        for s in reads:
            s.r[id(dslot.dsem)] = ev
        for s in writes:
            s.w = ev; s.r = {}
        return ev

    def wait_all(self, eng, slots):
        e = self.E[eng]
        deps = []
        for s in slots:
            if s.w is not None:
                deps.append((s.w[0], s.w[1], True))
            deps.extend((a, b, True) for (a, b) in s.r.values())
        self._wait(e, deps)

    @staticmethod
    def alias(olds, news):
        m = {}
        for s in olds:
            evs = list(s.r.values()) + ([s.w] if s.w is not None else [])
            for (sem, val) in evs:
                k = id(sem)
                if k not in m or m[k][1] < val:
                    m[k] = (sem, val)
        for s in news:
            s.w = None; s.r = dict(m)


def build(TOK, NPRE, SPARSE=True):
    NT = TOK // 128
    NBLK = TOK // 512
    NTT = NPRE + NT
    nc = bass.Bass("TRN2", target_bir_lowering=False)

    def din(name, shape, dt=F32):
        return nc.dram_tensor(name, list(shape), dt, kind="ExternalInput").ap()

    xin = din("xin", [NTT * 128, D])
    pin = din("pin", [TOK, 256])
    vmk = din("vmk", [128, NPRE + 1])
    cst = din("cst", [128, 4, 128])
    abias_d = din("abias", [128, 2, 16, 128])
    lng = din("lng", [8, D])
    w_in = din("w_in", [D, 8456])
    sinks = din("sinks", [1, 16])
    bif_d = din("bif", [1, 8])
    ng_d = din("ng", [1, 1024])
    w_ba = din("w_ba", [1024, D])
    w_bm = din("w_bm", [1024, D])
    w_out = din("w_out", [D, D])
    w_rt = din("w_rt", [D, NE])
    b_rt = din("b_rt", [1, NE])
    w_eg = din("w_eg", [NE + 1, D, 512])
    w_eu = din("w_eu", [NE + 1, D, 512])
    w_ed = din("w_ed", [NE + 1, 512, D])
    w_pp = din("w_pp", [256, D])
    w_pg = din("w_pg", [D, D])
    out = nc.dram_tensor("out", [TOK, D], F32, kind="ExternalOutput").ap()
    flag_o = nc.dram_tensor("flag", [128, 1], F32, kind="ExternalOutput").ap()
    PRECAST = SPARSE
    if PRECAST:
        wbg = nc.dram_tensor("wbg", [NE + 1, D, 512], BF16, kind="Internal").ap()
        wbu = nc.dram_tensor("wbu", [NE + 1, D, 512], BF16, kind="Internal").ap()
        wbd = nc.dram_tensor("wbd", [NE + 1, 512, D], BF16, kind="Internal").ap()
        wb_in = nc.dram_tensor("wb_in", [D, 8456], BF16, kind="Internal").ap()
        wb_ba = nc.dram_tensor("wb_ba", [1024, D], BF16, kind="Internal").ap()
        wb_bm = nc.dram_tensor("wb_bm", [1024, D], BF16, kind="Internal").ap()
        wb_out = nc.dram_tensor("wb_out", [D, D], BF16, kind="Internal").ap()
        wb_pg = nc.dram_tensor("wb_pg", [D, D], BF16, kind="Internal").ap()
        wb_pp = nc.dram_tensor("wb_pp", [256, D], BF16, kind="Internal").ap()

    with ExitStack() as es:
        def sb(name, shape, dt):
            return es.enter_context(nc.sbuf_tensor(name, list(shape), dt))

        def sem(name):
            return es.enter_context(nc.semaphore(name))

        nsem = [0]

        def engsems():
            nsem[0] += 1
            return {k: sem("e%d_%s" % (nsem[0], k)) for k in ["pe", "dve", "act", "pool", "sp"]}

        def dslot(name):
            s = Slot(name); s.dsem = sem("d_" + name); return s

        S = Sched(nc, engsems())

        WSN = 54816
        R = sb("R", [128, 4, D], F32)
        xT = sb("xT", [128, NCH, 512], BF16)
        WS = sb("WS", [128, WSN], BF16)
        cf = sb("cf", [128, 4, 128], F32)
        idb = sb("idb", [128, 128], BF16)
        onesb = sb("onesb", [128, 128], BF16)
        abias = sb("abias_s", [128, 2, 16, 128], F32)
        esink = sb("esink", [128, 16], F32)
        bif = sb("bifs", [128, 8], F32)
        ngB = sb("ngB", [128, 1024], F32)
        brB = sb("brB", [128, NE], F32)
        vm = sb("vm", [128, NPRE + 1], F32)
        kvb = sb("kvb", [128, 1], F32)
        wrt = sb("wrt", [128, NCH, NE], BF16)
        C32 = sb("C32", [128, 4, 256], F32)
        n32 = sb("n32", [128, 4], F32)
        Cb = sb("Cb", [128, 4, 258], BF16)
        gw = sb("gw", [128, 4, NE + 1], F32)
        sm = sb("sm", [128, 160], F32)
        st6 = sb("st6", [128, 4, 6], F32)
        junk = sb("junk", [128, 256], BF16)
        stri = sb("stri", [128, 128], BF16)
        flagmax = sb("flagmax", [128, 1], F32)
        gB = sb("gB", [128, D], F32)
        bB = sb("bB", [128, D], F32)
        idf = cf[:, 0, :]; tri = cf[:, 1, :]; onesf = cf[:, 2, :]; iotaf = cf[:, 3, :]

        P = [es.enter_context(nc.psum_tensor("ps%d" % i, [128, 512], F32)) for i in range(8)]
        PS = [Slot("ps%d" % i) for i in range(8)]

        s_R = [Slot("R%d" % i) for i in range(4)]
        s_xT = Slot("xT")
        s_c = dslot("c"); s_ab = dslot("ab"); s_es = dslot("es"); s_bif = dslot("bif"); s_ng = dslot("ng")
        s_br = dslot("br"); s_vm = dslot("vm"); s_wrt = dslot("wrt"); s_gB = dslot("gB"); s_bB = dslot("bB")
        s_idb = Slot("idb"); s_C = Slot("C"); s_Cb = Slot("Cb"); s_gw = Slot("gw"); s_sm = Slot("sm")
        s_st6 = Slot("st"); s_junk = Slot("junk"); s_kvb = Slot("kvb")
        s_xl = [dslot("xl%d" % i) for i in range(4)]

        def ws(off, n, dt=BF16):
            if dt == F32:
                return WS[:, off:off + 2 * n].bitcast(F32)
            return WS[:, off:off + n]

        block = es.enter_context(nc.Block())

        @block.sync
        def _(sync):
            nonlocal w_in, w_ba, w_bm, w_out, w_pg, w_pp
            dve = lambda fn, r=(), w=(): S.op("dve", fn, r, w)
            act = lambda fn, r=(), w=(): S.op("act", fn, r, w)
            pool = lambda fn, r=(), w=(): S.op("pool", fn, r, w)
            pe = lambda fn, r=(), w=(), inc=True: S.op("pe", fn, r, w, inc)

            S.dma("sp", cf[:], cst, writes=[s_c], dslot=s_c)
            S.dma("sp", abias[:], abias_d, writes=[s_ab], dslot=s_ab)
            S.dma("sp", esink[:], sinks.partition_broadcast(128).rearrange("p a b -> p (a b)"), writes=[s_es], dslot=s_es)
            S.dma("sp", bif[:], bif_d.partition_broadcast(128).rearrange("p a b -> p (a b)"), writes=[s_bif], dslot=s_bif)
            S.dma("sp", ngB[:], ng_d.partition_broadcast(128).rearrange("p a b -> p (a b)"), writes=[s_ng], dslot=s_ng)
            S.dma("sp", brB[:], b_rt.partition_broadcast(128).rearrange("p a b -> p (a b)"), writes=[s_br], dslot=s_br)
            S.dma("sp", vm[:], vmk, writes=[s_vm], dslot=s_vm)
            S.dma("pool", wrt[:], w_rt.rearrange("(c p) n -> p c n", p=128), writes=[s_wrt], dslot=s_wrt)
            dve(lambda v: v.tensor_copy(out=idb[:], in_=idf), [s_c], [s_idb])
            dve(lambda v: v.tensor_copy(out=onesb[:], in_=onesf), [s_c], [s_idb])
            act(lambda a: a.activation(out=esink[:], in_=esink[:], func=AF.Exp), [s_es], [s_es])
            dve(lambda v: v.tensor_scalar(out=kvb[:], in0=vm[:, NPRE:NPRE + 1], scalar1=-1.0, scalar2=-NEGBIG,
                                          op0=ALU.add, op1=ALU.mult), [s_vm], [s_kvb])
            dve(lambda v: v.memset(C32[:], 0.0), [], [s_C])
            dve(lambda v: v.memset(n32[:], 0.0), [], [s_C])
            dve(lambda v: v.memset(Cb[:], 0.0), [], [s_Cb])
            dve(lambda v: v.memset(gw[:], IALPHA), [], [s_gw])
            s_flag = dslot("flag")
            dve(lambda v: v.memset(flagmax[:], 0.0), [], [s_flag])
            dve(lambda v: v.tensor_tensor(out=stri[:], in0=tri, in1=idf, op=ALU.subtract), [s_c], [s_idb])
            s_pc = [dslot("pc%d" % i) for i in range(9)]
            pc_list = []
            pc_group = {}
            if PRECAST:
                for c0 in range(0, 8456, 2048):
                    n_ = min(2048, 8456 - c0)
                    pc_list.append((wb_in[:, c0:c0 + n_], w_in[:, c0:c0 + n_], len(pc_list) // 24))
                for (dst_, src_) in ((wb_ba, w_ba), (wb_bm, w_bm), (wb_out, w_out), (wb_pg, w_pg), (wb_pp, w_pp)):
                    pc_list.append((dst_, src_, len(pc_list) // 24))
                n_mix_pc = len(pc_list)
                for e in [NE] + list(range(NE)):
                    for (dst_, src_) in ((wbg, w_eg), (wbu, w_eu), (wbd, w_ed)):
                        pc_group[e] = len(pc_list) // 24
                        pc_list.append((dst_[e], src_[e], len(pc_list) // 24))
            pc_state = {"i": 0, "hist": []}
            pc_prefix_issued = [0]

            def pc_issue(n):
                for _ in range(n):
                    if pc_state["i"] >= len(pc_list):
                        return
                    d_, s_, g_ = pc_list[pc_state["i"]]; pc_state["i"] += 1
                    hist = pc_state["hist"]
                    if len(hist) >= 12:
                        psem, pval = hist[-12]
                        nc.gpsimd.wait_ge(psem, pval)
                    hist.append(S.dma("pool", d_, s_, writes=[s_pc[g_]], dslot=s_pc[g_]))

            def load_ln(idx):
                S.dma("sp", gB[:], lng[idx].partition_broadcast(128), writes=[s_gB], dslot=s_gB)
                S.dma("sp", bB[:], lng[idx + 1].partition_broadcast(128), writes=[s_bB], dslot=s_bB)

            def layer_norm(t, eps, xb, s_xb, use_pool=False, sc0=0, s_st=None):
                s_q = s_sm if s_st is None else s_st
                rt = R[:, t, :]
                c0, c1, c2, c3 = sc0, sc0 + 1, sc0 + 2, sc0 + 3
                for i in range(4):
                    dve(lambda v: v.bn_stats(out=st6[:, i, :], in_=rt[:, i * 512:(i + 1) * 512]), [s_R[t]], [s_st6])
                dve(lambda v: v.bn_aggr(out=sm[:, c0:c0 + 2], in_=st6[:].rearrange("p a b -> p (a b)")), [s_st6], [s_q])
                act(lambda a: a.activation(out=sm[:, c2:c2 + 1], in_=sm[:, c1:c1 + 1], func=AF.Sqrt, bias=float(eps), scale=1.0), [s_q], [s_q])
                dve(lambda v: v.reciprocal(out=sm[:, c2:c2 + 1], in_=sm[:, c2:c2 + 1]), [s_q], [s_q])
                if use_pool:
                    dve(lambda v: v.tensor_scalar(out=sm[:, c3:c3 + 1], in0=sm[:, c0:c0 + 1], scalar1=sm[:, c2:c2 + 1], scalar2=-1.0,
                                                  op0=ALU.mult, op1=ALU.mult), [s_q], [s_q])
                    act(lambda a: a.activation(out=rt, in_=rt, func=AF.Identity, bias=sm[:, c3:c3 + 1], scale=sm[:, c2:c2 + 1]),
                        [s_R[t], s_q], [s_R[t]])
                    pool(lambda g: g.tensor_tensor(out=rt, in0=rt, in1=gB[:], op=ALU.mult), [s_R[t], s_gB], [s_R[t]])
                    pool(lambda g: g.tensor_tensor(out=xb, in0=rt, in1=bB[:], op=ALU.add), [s_R[t], s_bB], [s_xb])
                    return
                dve(lambda v: v.scalar_tensor_tensor(out=rt, in0=rt, scalar=sm[:, c0:c0 + 1], in1=gB[:], op0=ALU.subtract, op1=ALU.mult),
                    [s_R[t], s_q, s_gB], [s_R[t]])
                dve(lambda v: v.scalar_tensor_tensor(out=rt, in0=rt, scalar=sm[:, c2:c2 + 1], in1=bB[:], op0=ALU.mult, op1=ALU.add),
                    [s_R[t], s_q, s_bB], [s_R[t]])
                act(lambda a: a.copy(out=xb, in_=rt), [s_R[t]], [s_xb])

            def transpose_to(xb, s_xb, dst_fn, s_dst, nchunks=NCH, pb=0):
                pbv = P[pb][:].bitcast(BF16)
                for g0 in range(0, nchunks, 8):
                    n = min(8, nchunks - g0)
                    for j in range(n):
                        c = g0 + j
                        pe(lambda t: t.transpose(out=pbv[:, j * 128:(j + 1) * 128], in_=xb[:, c * 128:(c + 1) * 128], identity=idb[:]),
                           [s_xb, s_idb], [PS[pb]], inc=(j == n - 1))
                    act(lambda a: a.copy(out=dst_fn(g0, n), in_=pbv[:, 0:n * 128].rearrange("p (c k) -> p c k", k=128)),
                        [PS[pb]], [s_dst])

            ring = {"slots": [], "aps": [], "i": 0}
            RING_E = 4096

            cur_xr = []

            def wload(src, shape_str, q="pool", xr=None):
                if xr is None:
                    xr = cur_xr
                i = ring["i"] % len(ring["slots"]); ring["i"] += 1
                sl = ring["slots"][i]
                n = 1
                for d in src.shape[1:]:
                    n *= d
                dst = ring["aps"][i][0:src.shape[0], 0:n]
                if len(src.shape) == 3:
                    dst = dst.rearrange("p (a b) -> p a b", b=src.shape[2])
                S.dma(q, dst, src, reads=list(xr), writes=[sl], dslot=sl)
                return dst, sl

            def wcols(wd, c0, n):
                return wd.rearrange("(c p) n -> p c n", p=128)[:, :, c0:c0 + n]

            def mlstm_gates(gate_ps, s_gate_ps, pg, vcol, co=0, s_gs=None):
                sq = s_sm if s_gs is None else s_gs
                k = lambda a, b: sm[:, co + a:co + b]
                if gate_ps is not None:
                    dve(lambda v: v.tensor_tensor(out=k(8, 16), in0=gate_ps, in1=bif[:], op=ALU.add), [s_gate_ps, s_bif], [sq])
                act(lambda a: a.activation(out=k(8, 16), in_=k(8, 16), func=AF.Tanh, scale=1.0 / 15.0), [sq], [sq])
                act(lambda a: a.activation(out=k(16, 20), in_=k(12, 16), func=AF.Exp, scale=-15.0), [sq], [sq])
                act(lambda a: a.activation(out=k(16, 20), in_=k(16, 20), func=AF.Ln, bias=1.0, scale=1.0), [sq], [sq])
                pe(lambda t: t.matmul(P[pg][:, 0:4], lhsT=tri, rhs=k(16, 20), start=True, stop=True), [s_c, sq], [PS[pg]], inc=False)
                pe(lambda t: t.matmul(P[pg][:, 4:8], lhsT=onesf, rhs=k(16, 20), start=True, stop=True), [s_c, sq], [PS[pg]])
                dve(lambda v: v.tensor_copy(out=k(20, 24), in_=P[pg][:, 0:4]), [PS[pg]], [sq])
                dve(lambda v: v.scalar_tensor_tensor(out=k(24, 28), in0=k(8, 12), scalar=15.0, in1=k(20, 24),
                                                     op0=ALU.mult, op1=ALU.add), [sq], [sq])
                dve(lambda v: v.tensor_tensor(out=k(28, 32), in0=k(24, 28), in1=P[pg][:, 4:8], op=ALU.subtract), [sq, PS[pg]], [sq])
                act(lambda a: a.activation(out=k(28, 32), in_=k(28, 32), func=AF.Exp), [sq], [sq])
                if vcol is not None:
                    dve(lambda v: v.tensor_scalar(out=k(28, 32), in0=k(28, 32), scalar1=vm[:, vcol:vcol + 1], scalar2=None,
                                                  op0=ALU.mult), [sq, s_vm], [sq])
                act(lambda a: a.activation(out=k(32, 36), in_=P[pg][:, 4:8], func=AF.Exp, scale=-1.0), [PS[pg]], [sq])

            def mlstm_update(ktok_ps, s_ktok, kw, s_kw, vaug, s_vaug, pu0, pu1, pn):
                for h in range(4):
                    act(lambda a: a.activation(out=kw[:, h, :], in_=ktok_ps[:, h, :], func=AF.Copy, scale=sm[:, 28 + h:29 + h]),
                        [s_ktok, s_sm], [s_kw])
                for h in range(4):
                    pb = pu0 if h < 2 else pu1
                    pe(lambda t: t.matmul(P[pb][:, (h % 2) * 256:(h % 2 + 1) * 256], lhsT=kw[:, h, :], rhs=vaug[:, h, 0:256],
                                          start=True, stop=True), [s_kw, s_vaug], [PS[pb]], inc=(h % 2 == 1))
                for h in range(4):
                    pe(lambda t: t.matmul(P[pn][:, 16 + h:17 + h], lhsT=kw[:, h, :], rhs=vaug[:, h, 256:257], start=True, stop=True),
                       [s_kw, s_vaug], [PS[pn]], inc=(h == 3))
                for h in range(4):
                    pb = pu0 if h < 2 else pu1
                    dve(lambda v: v.scalar_tensor_tensor(out=C32[:, h, :], in0=C32[:, h, :], scalar=sm[:, 32 + h:33 + h],
                                                         in1=P[pb][:, (h % 2) * 256:(h % 2 + 1) * 256], op0=ALU.mult, op1=ALU.add),
                        [s_C, s_sm, PS[pb]], [s_C])
                dve(lambda v: v.tensor_tensor(out=n32[:], in0=n32[:], in1=sm[:, 32:36], op=ALU.mult), [s_C, s_sm], [s_C])
                dve(lambda v: v.tensor_tensor(out=n32[:], in0=n32[:], in1=P[pn][:, 16:20], op=ALU.add), [s_C, PS[pn]], [s_C])
                act(lambda a: a.copy(out=Cb[:, :, 0:256], in_=C32[:]), [s_C], [s_Cb])
                act(lambda a: a.copy(out=Cb[:, :, 256:257], in_=n32[:].rearrange("p (h o) -> p h o", o=1)), [s_C], [s_Cb])

            PW = 512 + 1024 + 8 + 128 + 128
            o = 0
            wpre = ws(o, NCH * PW).rearrange("p (c n) -> p c n", n=PW); o += NCH * PW
            p_xb = []; p_hT = []; p_va = []; p_kt = []
            for b in range(2):
                p_xb.append(ws(o, D)); o += D
                p_hT.append(ws(o, NCH * 128).rearrange("p (c k) -> p c k", k=128)); o += NCH * 128
                p_va.append(ws(o, 4 * 258).rearrange("p (h k) -> p h k", k=258)); o += 4 * 258
                p_kt.append(ws(o, 512).rearrange("p (h k) -> p h k", k=128)); o += 512
            p_kw = ws(o, 512).rearrange("p (h k) -> p h k", k=128); o += 512
            HALO = WSN - 256
            assert o <= HALO
            kT_halo = ws(HALO, 128); V_halo = ws(HALO + 128, 128)
            s_wpre = dslot("wpre"); s_pkw = Slot(); s_halo = Slot()
            s_pxb = [Slot(), Slot()]; s_phT = [Slot(), Slot()]; s_pva = [Slot(), Slot()]; s_pkt = [Slot(), Slot()]
            s_g = [Slot(), Slot()]
            GCO = [0, 52]

            def pA(pt):
                b = pt % 2; t = pt % 4
                S.dma("sp", R[:, t, :], xin[pt * 128:(pt + 1) * 128, :], writes=[s_R[t]], dslot=s_xl[t])
                layer_norm(t, 1e-5, p_xb[b], s_pxb[b], use_pool=True, sc0=104 + 4 * b, s_st=s_g[b])

            def pT(pt):
                b = pt % 2
                transpose_to(p_xb[b], s_pxb[b], lambda c0, n: p_hT[b][:, c0:c0 + n, :], s_phT[b], pb=0)

            def pB(pt):
                b = pt % 2; co = GCO[b]
                for (pb, po, n) in [(1, 0, 512), (2, 512, 512), (3, 1024, 512)]:
                    for c in range(NCH):
                        pe(lambda tt: tt.matmul(P[pb][:, 0:n], lhsT=p_hT[b][:, c, :], rhs=wpre[:, c, po:po + n], start=(c == 0), stop=(c == NCH - 1)),
                           [s_phT[b], s_wpre], [PS[pb]], inc=(c == NCH - 1))
                for c in range(NCH):
                    pe(lambda tt: tt.matmul(P[6][:, 0:8], lhsT=p_hT[b][:, c, :], rhs=wpre[:, c, 1536:1544], start=(c == 0), stop=(c == NCH - 1)),
                       [s_phT[b], s_wpre], [PS[6]], inc=(c == NCH - 1))
                act(lambda a: a.activation(out=p_kt[b][:].rearrange("p h k -> p (h k)"), in_=P[1][:], func=AF.Copy, scale=128.0 ** -0.5), [PS[1]], [s_pkt[b]])
                act(lambda a: a.copy(out=p_va[b][:, 0:2, 0:256], in_=P[2][:].rearrange("p (h k) -> p h k", k=256)), [PS[2]], [s_pva[b]])
                act(lambda a: a.copy(out=p_va[b][:, 2:4, 0:256], in_=P[3][:].rearrange("p (h k) -> p h k", k=256)), [PS[3]], [s_pva[b]])
                dve(lambda v: v.tensor_tensor(out=sm[:, co + 8:co + 16], in0=P[6][:, 0:8], in1=bif[:], op=ALU.add), [PS[6], s_bif], [s_g[b]])
                if pt == NPRE - 1:
                    for c in range(NCH):
                        pe(lambda tt: tt.matmul(P[1][:, 0:128], lhsT=wpre[:, c, 1544:1672], rhs=p_hT[b][:, c, :], start=(c == 0), stop=(c == NCH - 1)),
                           [s_phT[b], s_wpre], [PS[1]], inc=(c == NCH - 1))
                    for c in range(NCH):
                        pe(lambda tt: tt.matmul(P[2][:, 0:128], lhsT=p_hT[b][:, c, :], rhs=wpre[:, c, 1672:1800], start=(c == 0), stop=(c == NCH - 1)),
                           [s_phT[b], s_wpre], [PS[2]], inc=(c == NCH - 1))
                    act(lambda a: a.copy(out=kT_halo, in_=P[1][:, 0:128]), [PS[1]], [s_halo])
                    act(lambda a: a.copy(out=V_halo, in_=P[2][:, 0:128]), [PS[2]], [s_halo])

            def pC1(pt):
                b = pt % 2
                mlstm_gates(None, None, 7, pt, co=GCO[b], s_gs=s_g[b])

            def pC2(pt):
                b = pt % 2; co = GCO[b]
                for h in range(4):
                    act(lambda a: a.activation(out=p_kw[:, h, :], in_=p_kt[b][:, h, :], func=AF.Copy, scale=sm[:, co + 28 + h:co + 29 + h]),
                        [s_pkt[b], s_g[b]], [s_pkw])
                for h in range(4):
                    pb = 4 if h < 2 else 5
                    pe(lambda tt: tt.matmul(P[pb][:, (h % 2) * 256:(h % 2 + 1) * 256], lhsT=p_kw[:, h, :], rhs=p_va[b][:, h, 0:256], start=True, stop=True),
                       [s_pkw, s_pva[b]], [PS[pb]], inc=(h % 2 == 1))
                for h in range(4):
                    pe(lambda tt: tt.matmul(P[7][:, 16 + h:17 + h], lhsT=p_kw[:, h, :], rhs=p_va[b][:, h, 256:257], start=True, stop=True),
                       [s_pkw, s_pva[b]], [PS[7]], inc=(h == 3))
                for h in range(4):
                    pb = 4 if h < 2 else 5
                    dve(lambda v: v.scalar_tensor_tensor(out=C32[:, h, :], in0=C32[:, h, :], scalar=sm[:, co + 32 + h:co + 33 + h],
                                                         in1=P[pb][:, (h % 2) * 256:(h % 2 + 1) * 256], op0=ALU.mult, op1=ALU.add),
                        [s_C, s_g[b], PS[pb]], [s_C])
                dve(lambda v: v.tensor_tensor(out=n32[:], in0=n32[:], in1=sm[:, co + 32:co + 36], op=ALU.mult), [s_C, s_g[b]], [s_C])
                dve(lambda v: v.tensor_tensor(out=n32[:], in0=n32[:], in1=P[7][:, 16:20], op=ALU.add), [s_C, PS[7]], [s_C])

            if NPRE > 0:
                segs = [(C_MK, 512), (C_MV, 1024), (C_MI, 8), (C_AK, 128), (C_AV, 128)]
                po = 0
                for (c0, n) in segs:
                    S.dma("pool", wpre[:, :, po:po + n], wcols(w_in, c0, n), writes=[s_wpre], dslot=s_wpre)
                    po += n
                for b in range(2):
                    dve(lambda v: v.memset(p_va[b][:], 1.0), [], [s_pva[b]])
                load_ln(0)
                pA(0); pT(0)
                for pt in range(NPRE):
                    if pt + 1 < NPRE:
                        pA(pt + 1)
                    pc_issue(1 if pt % 2 == 0 else 2)
                    pB(pt)
                    if pt >= 1:
                        pC2(pt - 1)
                    if pt + 1 < NPRE:
                        pT(pt + 1)
                    pC1(pt)
                pC2(NPRE - 1)
                pc_prefix_issued[0] = pc_state["i"]
                act(lambda a: a.copy(out=Cb[:, :, 0:256], in_=C32[:]), [s_C], [s_Cb])
                act(lambda a: a.copy(out=Cb[:, :, 256:257], in_=n32[:].rearrange("p (h o) -> p h o", o=1)), [s_C], [s_Cb])
            if NPRE == 0:
                dve(lambda v: v.memset(kT_halo, 0.0), [], [s_halo])
                dve(lambda v: v.memset(V_halo, 0.0), [], [s_halo])

            SB = 512
            RING_E = 4096
            NRM, NRE = 5, 12
            Sched.alias([s_sm] + s_g, [s_sm])
            ring_sl = [dslot("rg%d" % i) for i in range(NRE)]
            s_pf = dslot("pf")
            rot = {"i": 0}

            def nbank(banks=(1, 2, 6, 7)):
                rot["i"] += 1
                return banks[rot["i"] % len(banks)]

            def ring_setup(nslots):
                ring["slots"] = ring_sl[:nslots]
                ring["aps"] = [ws(i * RING_E, RING_E) for i in range(nslots)]
                ring["i"] = 0

            w3 = w_in.rearrange("(c p) n -> p c n", p=128)
            wba4 = w_ba.rearrange("(g j d) n -> g d j n", g=2, d=64)
            wbm3 = w_bm.rearrange("(c p) n -> p c n", p=128)
            moe_slots = None
            for blk in range(NBLK):
                S.new_epoch(engsems())
                if PRECAST and blk >= 1:
                    w_in, w_ba, w_bm, w_out, w_pg, w_pp = wb_in, wb_ba, wb_bm, wb_out, wb_pg, wb_pp
                    w3 = w_in.rearrange("(c p) n -> p c n", p=128)
                    wba4 = w_ba.rearrange("(g j d) n -> g d j n", g=2, d=64)
                    wbm3 = w_bm.rearrange("(c p) n -> p c n", p=128)
                    cur_xr[:] = [s_pc[0]]
                o = NRM * RING_E

                def take(n, dt=BF16):
                    nonlocal o
                    v = ws(o, n, dt); o += n * (2 if dt == F32 else 1); return v
                o_mg = o
                m_qT = take(8 * SB).rearrange("p (j k) -> p j k", k=SB)
                m_mqT = take(4 * SB).rearrange("p (h k) -> p h k", k=SB)
                m_mkT = take(4 * SB).rearrange("p (h k) -> p h k", k=SB)
                m_mgT = ws(o_mg, 16 * SB).rearrange("p (c k) -> p c k", k=SB)
                m_kT = take(5 * 128).rearrange("p (t k) -> p t k", k=128)
                m_V = take(5 * 128).rearrange("p (t k) -> p t k", k=128)
                m_va = take(4 * 4 * 258).rearrange("p (t h k) -> p t h k", h=4, k=258)
                m_gs = take(4 * 1024).rearrange("p (t k) -> p t k", k=1024)
                m_attnT = take(8 * SB).rearrange("p (h k) -> p h k", k=SB)
                m_mhT = take(8 * SB).rearrange("p (c k) -> p c k", k=SB)
                m_xb = take(D)
                m_sc = take(512, F32)
                m_PT = take(2 * 512).rearrange("p (t k) -> p t k", k=512)
                m_dn = take(512, F32)
                m_eb = take(512).rearrange("p (h k) -> p h k", k=128)
                m_Sq = take(512).rearrange("p (h k) -> p h k", k=128)
                m_qd = take(512).rearrange("p (h k) -> p h k", k=128)
                m_kw = take(512).rearrange("p (h k) -> p h k", k=128)
                m_mh = take(1024)
                m_Dt = m_dn.rearrange("p (h k) -> p h k", k=128)
                m_rE = m_sc.rearrange("p (h k) -> p h k", k=128)
                m_t1 = m_sc
                m_t2 = m_PT.rearrange("p t k -> p (t k)").bitcast(F32)
                assert o <= HALO, o
                names = "xb V va gs qT kT mqT mkT attnT mhT mgT sc PT eb Sq qd kw mh dn".split()
                sl = {n: Slot(n) for n in names}
                sl["Dt"] = sl["dn"]; sl["rE"] = sl["sc"]; sl["t1"] = sl["sc"]; sl["t2"] = sl["PT"]
                ring_setup(NRM)
                mix_slots = [v for k, v in sl.items() if k != "mgT"] + ring["slots"]
                if blk == 0:
                    Sched.alias([s_wpre, s_pkw] + s_pxb + s_phT + s_pva + s_pkt, mix_slots)
                else:
                    Sched.alias(moe_slots, mix_slots)
                dve(lambda v: v.memset(m_va[:], 1.0), [], [sl["va"]])
                tiles = [0, 1, 2, 3]
                gt0 = NPRE + blk * 4
                load_ln(0)
                for t in tiles:
                    S.dma("sp", R[:, t, :], xin[(gt0 + t) * 128:(gt0 + t + 1) * 128, :], writes=[s_R[t]], dslot=s_xl[t])
                for t in tiles:
                    layer_norm(t, 1e-5, m_xb, sl["xb"])
                    transpose_to(m_xb, sl["xb"], lambda c0, n: xT[:, c0:c0 + n, t * 128:(t + 1) * 128], s_xT, pb=0)
                act(lambda a: a.copy(out=m_kT[:, 0, :], in_=kT_halo), [s_halo], [sl["kT"]])
                act(lambda a: a.copy(out=m_V[:, 0, :], in_=V_halo), [s_halo], [sl["V"]])

                def fmm(wt2, wsl, pb):
                    for c in range(NCH):
                        pe(lambda tt: tt.matmul(P[pb][:], lhsT=wt2[:, c, :], rhs=xT[:, c, :], start=(c == 0), stop=(c == NCH - 1)),
                           [wsl, s_xT], [PS[pb]], inc=(c == NCH - 1))
                for jp in range(4):
                    i = ring["i"] % NRM; ring["i"] += 1
                    wsl = ring["slots"][i]
                    wt = ring["aps"][i].rearrange("p (m c g r) -> p m c g r", m=2, g=2, r=64)
                    for mm in range(2):
                        j = jp * 2 + mm
                        src = w3[:, :, j * 64:j * 64 + 1024].rearrange("p c (g r) -> p c g r", r=512)
                        for gg in range(2):
                            S.dma("pool", wt[:, mm, :, gg, :], src[:, :, gg, 0:64], reads=list(cur_xr), writes=[wsl], dslot=wsl)
                    wt2 = ring["aps"][i].rearrange("p (m c k) -> p m c k", m=2, k=128)
                    for mm in range(2):
                        j = jp * 2 + mm
                        pb = nbank()
                        fmm(wt2[:, mm], wsl, pb)
                        act(lambda a: a.copy(out=m_qT[:, j, :], in_=P[pb][:]), [PS[pb]], [sl["qT"]])
                wt, wsl = wload(wcols(w_in, C_AK, 128), "")
                pb = nbank(); fmm(wt, wsl, pb)
                act(lambda a: a.copy(out=m_kT[:, 1:5, :], in_=P[pb][:].rearrange("p (t k) -> p t k", k=128)), [PS[pb]], [sl["kT"]])
                for hp in range(2):
                    wt, wsl = wload(wcols(w_in, C_MQ + hp * 256, 256), "")
                    for mm in range(2):
                        h = hp * 2 + mm
                        pb = nbank(); fmm(wt[:, :, mm * 128:(mm + 1) * 128], wsl, pb)
                        act(lambda a: a.copy(out=m_mqT[:, h, :], in_=P[pb][:]), [PS[pb]], [sl["mqT"]])
                for hp in range(2):
                    wt, wsl = wload(wcols(w_in, C_MK + hp * 256, 256), "")
                    for mm in range(2):
                        h = hp * 2 + mm
                        pb = nbank(); fmm(wt[:, :, mm * 128:(mm + 1) * 128], wsl, pb)
                        act(lambda a: a.activation(out=m_mkT[:, h, :], in_=P[pb][:], func=AF.Copy, scale=128.0 ** -0.5), [PS[pb]], [sl["mkT"]])
                gate_sb = sm[:, 112:144].rearrange("p (t k) -> p t k", k=8)

                def tproj(c0, n, evac):
                    wt, wsl = wload(wcols(w_in, c0, n), "")
                    for t in tiles:
                        pb = nbank()
                        for c in range(NCH):
                            pe(lambda tt: tt.matmul(P[pb][:, 0:n], lhsT=xT[:, c, t * 128:(t + 1) * 128], rhs=wt[:, c, :], start=(c == 0), stop=(c == NCH - 1)),
                               [wsl, s_xT], [PS[pb]], inc=(c == NCH - 1))
                        evac(t, pb)
                tproj(C_AV, 128, lambda t, pb: act(lambda a: a.copy(out=m_V[:, 1 + t, :], in_=P[pb][:, 0:128]), [PS[pb]], [sl["V"]]))
                for h in range(4):
                    tproj(C_MV + h * 256, 256, lambda t, pb: act(lambda a: a.copy(out=m_va[:, t, h, 0:256], in_=P[pb][:, 0:256]), [PS[pb]], [sl["va"]]))
                for q in range(4):
                    def ev_mo(t, pb):
                        act(lambda a: a.activation(out=m_gs[:, t, q * 256:(q + 1) * 256], in_=P[pb][:, 0:256], func=AF.Sigmoid), [PS[pb]], [sl["gs"]])
                        dve(lambda v: v.tensor_tensor(out=m_gs[:, t, q * 256:(q + 1) * 256], in0=m_gs[:, t, q * 256:(q + 1) * 256],
                                                      in1=ngB[:, q * 256:(q + 1) * 256], op=ALU.mult), [sl["gs"], s_ng], [sl["gs"]])
                    tproj(C_MO + q * 256, 256, ev_mo)
                tproj(C_MI, 8, lambda t, pb: dve(lambda v: v.tensor_copy(out=gate_sb[:, t, :], in_=P[pb][:, 0:8]), [PS[pb]], [s_sm]))

                for t in tiles:
                    qc = slice(t * 128, (t + 1) * 128)
                    first_tile = (blk == 0 and t == 0)
                    for g in range(2):
                        pr = slice(g * 64, (g + 1) * 64)
                        for half in range(2):
                            hh = g * 8 + half * 4
                            for kt in range(2):
                                pb = nbank((1, 2))
                                pe(lambda tt: tt.matmul(P[pb][:], lhsT=m_kT[pr, t + kt, :], rhs=m_qT[pr, half * 4:half * 4 + 4, qc],
                                                        start=True, stop=True), [sl["kT"], sl["qT"]], [PS[pb]])
                                dve(lambda v: v.scalar_tensor_tensor(out=m_sc, in0=P[pb][:], scalar=0.125,
                                                                     in1=abias[:, kt, hh:hh + 4, :].rearrange("p h k -> p (h k)"),
                                                                     op0=ALU.mult, op1=ALU.add), [PS[pb], s_ab], [sl["sc"]])
                                if kt == 0 and first_tile:
                                    act(lambda a: a.activation(out=m_PT[:, kt, :], in_=m_sc, func=AF.Exp, bias=kvb[:, 0:1], scale=1.0),
                                        [sl["sc"], s_kvb], [sl["PT"]])
                                else:
                                    act(lambda a: a.activation(out=m_PT[:, kt, :], in_=m_sc, func=AF.Exp), [sl["sc"]], [sl["PT"]])
                            for kt in range(2):
                                pe(lambda tt: tt.matmul(P[4][:], lhsT=m_V[:, t + kt, :], rhs=m_PT[:, kt, :], start=(kt == 0), stop=(kt == 1)),
                                   [sl["V"], sl["PT"]], [PS[4]], inc=(kt == 1))
                            for kt in range(2):
                                pe(lambda tt: tt.matmul(P[5][:], lhsT=onesb[:], rhs=m_PT[:, kt, :], start=(kt == 0), stop=(kt == 1)),
                                   [s_idb, sl["PT"]], [PS[5]], inc=(kt == 1))
                            for hq in range(4):
                                dve(lambda v: v.tensor_scalar(out=m_dn[pr, hq * 128:(hq + 1) * 128], in0=P[5][pr, hq * 128:(hq + 1) * 128],
                                                              scalar1=esink[pr, hh + hq:hh + hq + 1], scalar2=None, op0=ALU.add),
                                    [PS[5], s_es], [sl["dn"]])
                            dve(lambda v: v.reciprocal(out=m_dn[pr, :], in_=m_dn[pr, :]), [sl["dn"]], [sl["dn"]])
                            dve(lambda v: v.tensor_tensor(out=m_attnT[pr, half * 4:half * 4 + 4, qc], in0=P[4][pr, :].rearrange("p (h k) -> p h k", k=128),
                                                          in1=m_dn[pr, :].rearrange("p (h k) -> p h k", k=128), op=ALU.mult),
                                [PS[4], sl["dn"]], [sl["attnT"]])

                for t in tiles:
                    qc = slice(t * 128, (t + 1) * 128)
                    mlstm_gates(gate_sb[:, t, :], s_sm, 7, None)
                    for h in range(4):
                        dve(lambda v: v.tensor_scalar(out=m_rE[:, h, :], in0=idf, scalar1=sm[:, 20 + h:21 + h], scalar2=None, op0=ALU.mult),
                            [s_c, s_sm], [sl["rE"]])
                    pe(lambda tt: tt.matmul(P[3][:], lhsT=onesf, rhs=m_rE[:].rearrange("p h k -> p (h k)"), start=True, stop=True),
                       [s_c, sl["rE"]], [PS[3]])
                    act(lambda a: a.activation(out=m_eb[:].rearrange("p h k -> p (h k)"), in_=P[3][:], func=AF.Exp, scale=-1.0), [PS[3]], [sl["eb"]])
                    for h in range(4):
                        act(lambda a: a.activation(out=m_Dt[:, h, :], in_=P[3][:, h * 128:(h + 1) * 128], func=AF.Exp,
                                                   bias=sm[:, 24 + h:25 + h], scale=-1.0), [PS[3], s_sm], [sl["Dt"]])
                    for h in range(4):
                        dve(lambda v: v.tensor_tensor(out=m_Dt[:, h, :], in0=m_Dt[:, h, :], in1=tri, op=ALU.mult), [sl["Dt"], s_c], [sl["Dt"]])
                    for h in range(4):
                        pe(lambda tt: tt.matmul(P[1][:, h * 128:(h + 1) * 128], lhsT=m_mkT[:, h, qc], rhs=m_mqT[:, h, qc], start=True, stop=True),
                           [sl["mkT"], sl["mqT"]], [PS[1]], inc=(h == 3))
                    dve(lambda v: v.tensor_tensor(out=m_Sq[:].rearrange("p h k -> p (h k)"), in0=P[1][:],
                                                  in1=m_Dt[:].rearrange("p h k -> p (h k)"), op=ALU.mult), [PS[1], sl["Dt"]], [sl["Sq"]])
                    dve(lambda v: v.tensor_tensor(out=m_qd[:], in0=m_mqT[:, :, qc], in1=m_eb[:], op=ALU.mult), [sl["mqT"], sl["eb"]], [sl["qd"]])
                    for h in range(4):
                        pb = 4 if h < 2 else 5
                        osl = P[pb][:, (h % 2) * 256:(h % 2 + 1) * 256]
                        pe(lambda tt: tt.matmul(osl, lhsT=m_Sq[:, h, :], rhs=m_va[:, t, h, 0:256], start=True, stop=False),
                           [sl["Sq"], sl["va"]], [PS[pb]], inc=False)
                        pe(lambda tt: tt.matmul(osl, lhsT=m_qd[:, h, :], rhs=Cb[:, h, 0:256], start=False, stop=True),
                           [sl["qd"], s_Cb], [PS[pb]], inc=(h % 2 == 1))
                    for h in range(4):
                        pe(lambda tt: tt.matmul(P[7][:, 32 + h:33 + h], lhsT=m_Sq[:, h, :], rhs=m_va[:, t, h, 256:257], start=True, stop=False),
                           [sl["Sq"], sl["va"]], [PS[7]], inc=False)
                        pe(lambda tt: tt.matmul(P[7][:, 32 + h:33 + h], lhsT=m_qd[:, h, :], rhs=Cb[:, h, 256:257], start=False, stop=True),
                           [sl["qd"], s_Cb], [PS[7]], inc=(h == 3))
                    act(lambda a: a.activation(out=sm[:, 36:40], in_=P[7][:, 32:36], func=AF.Abs), [PS[7]], [s_sm])
                    dve(lambda v: v.tensor_scalar(out=sm[:, 36:40], in0=sm[:, 36:40], scalar1=1.0, scalar2=None, op0=ALU.max), [s_sm], [s_sm])
                    dve(lambda v: v.reciprocal(out=sm[:, 36:40], in_=sm[:, 36:40]), [s_sm], [s_sm])
                    for h in range(4):
                        pb = 4 if h < 2 else 5
                        act(lambda a: a.activation(out=junk[:], in_=P[pb][:, (h % 2) * 256:(h % 2 + 1) * 256], func=AF.Square,
                                                   accum_out=sm[:, 40 + h:41 + h]), [PS[pb]], [s_junk, s_sm])
                    dve(lambda v: v.tensor_tensor(out=sm[:, 48:52], in0=sm[:, 40:44], in1=sm[:, 36:40], op=ALU.mult), [s_sm], [s_sm])
                    dve(lambda v: v.tensor_tensor(out=sm[:, 48:52], in0=sm[:, 48:52], in1=sm[:, 36:40], op=ALU.mult), [s_sm], [s_sm])
                    act(lambda a: a.activation(out=sm[:, 48:52], in_=sm[:, 48:52], func=AF.Sqrt, bias=1e-6, scale=1.0 / 256.0), [s_sm], [s_sm])
                    dve(lambda v: v.reciprocal(out=sm[:, 48:52], in_=sm[:, 48:52]), [s_sm], [s_sm])
                    dve(lambda v: v.tensor_tensor(out=sm[:, 44:48], in0=sm[:, 48:52], in1=sm[:, 36:40], op=ALU.mult), [s_sm], [s_sm])
                    for h in range(4):
                        pb = 4 if h < 2 else 5
                        dve(lambda v: v.scalar_tensor_tensor(out=m_mh[:, h * 256:(h + 1) * 256], in0=P[pb][:, (h % 2) * 256:(h % 2 + 1) * 256],
                                                             scalar=sm[:, 44 + h:45 + h], in1=m_gs[:, t, h * 256:(h + 1) * 256],
                                                             op0=ALU.mult, op1=ALU.mult), [PS[pb], s_sm, sl["gs"]], [sl["mh"]])
                    transpose_to(m_mh, sl["mh"], lambda c0, n: m_mhT[:, c0:c0 + n, qc], sl["mhT"], nchunks=8, pb=0)
                    pbv = P[0][:].bitcast(BF16)
                    for h in range(4):
                        pe(lambda tt: tt.transpose(out=pbv[:, h * 128:(h + 1) * 128], in_=m_mkT[:, h, qc], identity=idb[:]),
                           [sl["mkT"], s_idb], [PS[0]], inc=(h == 3))
                    mlstm_update(pbv[:, 0:512].rearrange("p (h k) -> p h k", k=128), PS[0], m_kw, sl["kw"], m_va[:, t], sl["va"], 4, 5, 7)
                act(lambda a: a.copy(out=kT_halo, in_=m_kT[:, 4, :]), [sl["kT"]], [s_halo])
                act(lambda a: a.copy(out=V_halo, in_=m_V[:, 4, :]), [sl["V"]], [s_halo])

                Sched.alias([sl["qT"], sl["mqT"], sl["mkT"]], [sl["mgT"]])
                for mp in range(8):
                    wga, s1 = wload(wcols(w_in, C_GA + mp * 256, 256), "")
                    wgb, s2 = wload(wcols(w_in, C_GB + mp * 256, 256), "")
                    i = ring["i"] % NRM; ring["i"] += 1
                    s3 = ring["slots"][i]
                    wa = ring["aps"][i][:, 0:2048].rearrange("p (j n) -> p j n", n=256)
                    for gg in range(2):
                        S.dma("pool", wa[gg * 64:(gg + 1) * 64], wba4[gg][:, :, mp * 256:(mp + 1) * 256], reads=list(cur_xr), writes=[s3], dslot=s3)
                    wb, s4 = wload(wbm3[:, :, mp * 256:(mp + 1) * 256], "")
                    for mm in range(2):
                        m = mp * 2 + mm
                        ms = slice(mm * 128, (mm + 1) * 128)
                        b1, b2, b3, b4 = (1, 2, 3, 6) if m % 2 == 0 else (4, 5, 7, 0)
                        for c in range(NCH):
                            pe(lambda tt: tt.matmul(P[b1][:], lhsT=wga[:, c, ms], rhs=xT[:, c, :], start=(c == 0), stop=(c == NCH - 1)),
                               [s1, s_xT], [PS[b1]], inc=(c == NCH - 1))
                        for c in range(NCH):
                            pe(lambda tt: tt.matmul(P[b2][:], lhsT=wgb[:, c, ms], rhs=xT[:, c, :], start=(c == 0), stop=(c == NCH - 1)),
                               [s2, s_xT], [PS[b2]], inc=(c == NCH - 1))
                        for c in range(8):
                            pe(lambda tt: tt.matmul(P[b3][:], lhsT=wa[:, c, ms], rhs=m_attnT[:, c, :], start=(c == 0), stop=(c == 7)),
                               [s3, sl["attnT"]], [PS[b3]], inc=(c == 7))
                        for c in range(8):
                            pe(lambda tt: tt.matmul(P[b4][:], lhsT=wb[:, c, ms], rhs=m_mhT[:, c, :], start=(c == 0), stop=(c == 7)),
                               [s4, sl["mhT"]], [PS[b4]], inc=(c == 7))
                        act(lambda a: a.activation(out=m_t1, in_=P[b1][:], func=AF.Sigmoid), [PS[b1]], [sl["t1"]])
                        act(lambda a: a.activation(out=m_t2, in_=P[b2][:], func=AF.Sigmoid), [PS[b2]], [sl["t2"]])
                        dve(lambda v: v.tensor_tensor(out=m_t1, in0=m_t1, in1=P[b3][:], op=ALU.mult), [sl["t1"], PS[b3]], [sl["t1"]])
                        dve(lambda v: v.tensor_tensor(out=m_t2, in0=m_t2, in1=P[b4][:], op=ALU.mult), [sl["t2"], PS[b4]], [sl["t2"]])
                        dve(lambda v: v.tensor_tensor(out=m_mgT[:, m, :], in0=m_t1, in1=m_t2, op=ALU.add), [sl["t1"], sl["t2"]], [sl["mgT"]])
                for nb in range(8):
                    wt, wsl = wload(wcols(w_out, nb * 256, 256), "")
                    for t in tiles:
                        pb = nbank()
                        for c in range(NCH):
                            pe(lambda tt: tt.matmul(P[pb][:, 0:256], lhsT=m_mgT[:, c, t * 128:(t + 1) * 128], rhs=wt[:, c, :], start=(c == 0), stop=(c == NCH - 1)),
                               [wsl, sl["mgT"]], [PS[pb]], inc=(c == NCH - 1))
                        dve(lambda v: v.scalar_tensor_tensor(out=R[:, t, nb * 256:(nb + 1) * 256], in0=P[pb][:, 0:256], scalar=IALPHA,
                                                             in1=R[:, t, nb * 256:(nb + 1) * 256], op0=ALU.mult, op1=ALU.add),
                            [PS[pb], s_R[t]], [s_R[t]])
                load_ln(2)
                if SPARSE:
                    X1tok = ws(38176, 4 * D).rearrange("p (t k) -> p t k", k=D)
                    s_x1t = Slot("x1t")
                    Sched.alias([sl["attnT"], sl["mhT"]], [s_x1t])
                for t in tiles:
                    if SPARSE:
                        layer_norm(t, EPS_A, X1tok[:, t, :], s_x1t)
                        transpose_to(X1tok[:, t, :], s_x1t, lambda c0, n: xT[:, c0:c0 + n, t * 128:(t + 1) * 128], s_xT, pb=0)
                    else:
                        layer_norm(t, EPS_A, m_xb, sl["xb"])
                        transpose_to(m_xb, sl["xb"], lambda c0, n: xT[:, c0:c0 + n, t * 128:(t + 1) * 128], s_xT, pb=0)

                if SPARSE:
                    o = 46368
                    x_mskf = take(256, F32).rearrange("p (t k) -> p t k", k=64)
                    x_rank = take(256, F32).rearrange("p (t k) -> p t k", k=64)
                    x_mskb = take(256).rearrange("p (t k) -> p t k", k=64)
                    s_msk = Slot("msk")
                    Sched.alias([sl["xb"]], [s_msk])
                for t in range(4):
                    pb = nbank()
                    for c in range(NCH):
                        pe(lambda tt: tt.matmul(P[pb][:, 0:NE], lhsT=xT[:, c, t * 128:(t + 1) * 128], rhs=wrt[:, c, :], start=(c == 0), stop=(c == NCH - 1)),
                           [s_xT, s_wrt], [PS[pb]], inc=(c == NCH - 1))
                    sc_ = m_sc[:, 0:64]; sel_ = m_sc[:, 64:128]; top_ = m_sc[:, 128:136]; msk_ = m_sc[:, 192:256]
                    act(lambda a: a.activation(out=sc_, in_=P[pb][:, 0:NE], func=AF.Sigmoid), [PS[pb]], [sl["sc"]])
                    dve(lambda v: v.tensor_tensor(out=sel_, in0=sc_, in1=brB[:], op=ALU.add), [sl["sc"], s_br], [sl["sc"]])
                    dve(lambda v: v.max(out=top_, in_=sel_), [sl["sc"]], [sl["sc"]])
                    dve(lambda v: v.tensor_scalar(out=msk_, in0=sel_, scalar1=top_[:, 7:8], scalar2=None, op0=ALU.is_ge), [sl["sc"]], [sl["sc"]])
                    if SPARSE:
                        dve(lambda v: v.tensor_copy(out=x_mskf[:, t, :], in_=msk_), [sl["sc"]], [s_msk])
                        dve(lambda v: v.tensor_copy(out=x_mskb[:, t, :], in_=msk_), [sl["sc"]], [s_msk])
                    dve(lambda v: v.tensor_tensor(out=msk_, in0=msk_, in1=sc_, op=ALU.mult), [sl["sc"]], [sl["sc"]])
                    dve(lambda v: v.reduce_sum(out=sm[:, 150:151], in_=msk_, axis=AX.X), [sl["sc"]], [s_sm])
                    dve(lambda v: v.reciprocal(out=sm[:, 150:151], in_=sm[:, 150:151]), [s_sm], [s_sm])
                    dve(lambda v: v.tensor_scalar(out=gw[:, t, 0:NE], in0=msk_, scalar1=sm[:, 150:151], scalar2=2.5 * IALPHA,
                                                  op0=ALU.mult, op1=ALU.mult), [sl["sc"], s_sm], [s_gw])
                if SPARSE:
                    for t in range(4):
                        pb = nbank()
                        seq = [(stri[:], x_mskb[:, t, :])] + [(onesb[:], x_mskb[:, tp, :]) for tp in range(t)]
                        for i, (lt, rh) in enumerate(seq):
                            pe(lambda tt: tt.matmul(P[pb][:, 0:NE], lhsT=lt, rhs=rh, start=(i == 0), stop=(i == len(seq) - 1)),
                               [s_idb, s_msk], [PS[pb]], inc=(i == len(seq) - 1))
                        dve(lambda v: v.tensor_copy(out=x_rank[:, t, :], in_=P[pb][:, 0:NE]), [PS[pb]], [s_msk])
                    pb = nbank()
                    for t in range(4):
                        pe(lambda tt: tt.matmul(P[pb][:, 0:NE], lhsT=onesb[:], rhs=x_mskb[:, t, :], start=(t == 0), stop=(t == 3)),
                           [s_idb, s_msk], [PS[pb]], inc=(t == 3))
                    dve(lambda v: v.tensor_reduce(out=sm[:, 152:153], in_=P[pb][:, 0:NE], axis=AX.X, op=ALU.max), [PS[pb]], [s_sm])
                    dve(lambda v: v.tensor_tensor(out=flagmax[:], in0=flagmax[:], in1=sm[:, 152:153], op=ALU.max), [s_sm, s_flag], [s_flag])

                wq = "pool"

                def eload(e):
                    if PRECAST and (blk >= 1 or (pc_group[e] + 1) * 24 <= pc_prefix_issued[0]):
                        wg3 = wbg[e].rearrange("(c p) n -> p c n", p=128)
                        wu3 = wbu[e].rearrange("(c p) n -> p c n", p=128)
                        wd3 = wbd[e].rearrange("(c p) n -> p c n", p=128)
                        xr = [s_pc[pc_group[e]]]
                    else:
                        wg3 = w_eg[e].rearrange("(c p) n -> p c n", p=128)
                        wu3 = w_eu[e].rearrange("(c p) n -> p c n", p=128)
                        wd3 = w_ed[e].rearrange("(c p) n -> p c n", p=128)
                        xr = []
                    wgs = [wload(wg3[:, :, hp * 256:(hp + 1) * 256], "", q=wq, xr=xr) for hp in range(2)]
                    wus = [wload(wu3[:, :, hp * 256:(hp + 1) * 256], "", q=wq, xr=xr) for hp in range(2)]
                    wds = [wload(wd3[:, hp * 2:hp * 2 + 2, :], "", q=wq, xr=xr) for hp in range(2)]
                    return wgs, wus, wds

                def dense_expert(e, e_h1, e_sg, s_h1, s_sg):
                    wgs, wus, wds = eload(e)
                    for m in range(4):
                        pg_, pu_ = (0, 1) if m % 2 == 0 else (2, 3)
                        wg_, sg_w = wgs[m // 2]; wu_, su_w = wus[m // 2]
                        ms = slice((m % 2) * 128, (m % 2 + 1) * 128)
                        for c in range(NCH):
                            pe(lambda tt: tt.matmul(P[pg_][:], lhsT=wg_[:, c, ms], rhs=xT[:, c, :], start=(c == 0), stop=(c == NCH - 1)),
                               [sg_w, s_xT], [PS[pg_]], inc=(c == NCH - 1))
                        for c in range(NCH):
                            pe(lambda tt: tt.matmul(P[pu_][:], lhsT=wu_[:, c, ms], rhs=xT[:, c, :], start=(c == 0), stop=(c == NCH - 1)),
                               [su_w, s_xT], [PS[pu_]], inc=(c == NCH - 1))
                        act(lambda a: a.activation(out=e_sg, in_=P[pg_][:], func=AF.Silu), [PS[pg_]], [s_sg])
                        dve(lambda v: v.tensor_tensor(out=e_h1[:, m, :], in0=e_sg, in1=P[pu_][:], op=ALU.mult), [s_sg, PS[pu_]], [s_h1])
                    for t in range(4):
                        for nb in range(4):
                            for m in range(4):
                                wd_, sd_w = wds[m // 2]
                                pe(lambda tt: tt.matmul(P[4 + nb][:], lhsT=e_h1[:, m, t * 128:(t + 1) * 128], rhs=wd_[:, m % 2, nb * 512:(nb + 1) * 512],
                                                        start=(m == 0), stop=(m == 3)), [s_h1, sd_w], [PS[4 + nb]], inc=(m == 3))
                            dve(lambda v: v.scalar_tensor_tensor(out=R[:, t, nb * 512:(nb + 1) * 512], in0=P[4 + nb][:], scalar=gw[:, t, e:e + 1],
                                                                 in1=R[:, t, nb * 512:(nb + 1) * 512], op0=ALU.mult, op1=ALU.add),
                                [PS[4 + nb], s_gw, s_R[t]], [s_R[t]])

                if not SPARSE:
                    o = NRE * RING_E
                    e_h1 = take(4 * 512).rearrange("p (m k) -> p m k", k=512)
                    e_sg = take(512, F32)
                    assert o <= HALO
                    s_h1 = Slot("h1"); s_sg = Slot("sg")
                    ring_setup(NRE)
                    moe_slots = [s_h1, s_sg] + ring["slots"]
                    Sched.alias(list(sl.values()) + ring_sl[:NRM], moe_slots)
                    for e in range(NE + 1):
                        dense_expert(e, e_h1, e_sg, s_h1, s_sg)
                    ple_olds = [s_h1, s_sg]
                    PLE_O = NRE * RING_E
                else:
                    NRS = 8
                    ring_setup(NRS)
                    o = NRS * RING_E
                    x_Sel = [take(512).rearrange("p (t k) -> p t k", k=128) for _ in range(2)]
                    x_SelT = take(512).rearrange("p (t k) -> p t k", k=128)
                    x_h1 = take(512); x_h1T = take(512).rearrange("p (m k) -> p m k", k=128)
                    x_yb = take(2048)
                    assert o <= 38176, o
                    o = 49440
                    oB = o
                    x_XeT = [take(2048).rearrange("p (c k) -> p c k", k=128) for _ in range(2)]
                    x_sg = take(512, F32)
                    assert o <= HALO, o
                    e_h1 = ws(oB, 2048).rearrange("p (m k) -> p m k", k=512)
                    e_sg = ws(oB + 2048, 512, F32)
                    s_h1 = Slot("h1"); s_sg = Slot("sg")
                    s_Sel = [Slot(), Slot()]; s_SelT = Slot(); s_xh1 = Slot(); s_xh1T = Slot(); s_yb = Slot()
                    s_XeT = [Slot(), Slot()]; s_xsg = Slot()
                    first = [s_h1, s_sg] + s_Sel + [s_SelT, s_xh1, s_xh1T, s_yb] + ring["slots"]
                    Sched.alias([v for k, v in sl.items() if k not in ("attnT", "mhT", "xb")] + ring_sl[:NRM], first)
                    dense_expert(NE, e_h1, e_sg, s_h1, s_sg)
                    Sched.alias([s_h1, s_sg], s_XeT + [s_xsg])
                    moe_slots = first + s_XeT + [s_xsg, s_x1t, s_msk]

                    def xSel(e):
                        b = e % 2
                        for t in range(4):
                            dve(lambda v: v.tensor_scalar(out=x_Sel[b][:, t, :], in0=iotaf, scalar1=x_rank[:, t, e:e + 1], scalar2=x_mskf[:, t, e:e + 1],
                                                          op0=ALU.is_equal, op1=ALU.mult), [s_c, s_msk], [s_Sel[b]])

                    def xGather(e):
                        b = e % 2
                        for g4 in range(4):
                            pb = g4 % 2
                            for cc in range(4):
                                c = g4 * 4 + cc
                                for t in range(4):
                                    pe(lambda tt: tt.matmul(P[pb][:, cc * 128:(cc + 1) * 128], lhsT=X1tok[:, t, c * 128:(c + 1) * 128], rhs=x_Sel[b][:, t, :],
                                                            start=(t == 0), stop=(t == 3)), [s_x1t, s_Sel[b]], [PS[pb]], inc=(cc == 3 and t == 3))
                            act(lambda a: a.copy(out=x_XeT[b][:, g4 * 4:(g4 + 1) * 4, :], in_=P[pb][:].rearrange("p (c k) -> p c k", k=128)),
                                [PS[pb]], [s_XeT[b]])

                    def xFFN(e, wgs, wus, between=None):
                        b = e % 2
                        q = 0
                        for (bank, ws_) in ((2, wgs), (3, wus)):
                            for hp in range(2):
                                w_, wsl_ = ws_[hp]
                                for c in range(NCH):
                                    pe(lambda tt: tt.matmul(P[bank][:, hp * 256:(hp + 1) * 256], lhsT=x_XeT[b][:, c, :], rhs=w_[:, c, :],
                                                            start=(c == 0), stop=(c == NCH - 1)), [s_XeT[b], wsl_], [PS[bank]], inc=(c == NCH - 1))
                                if between is not None:
                                    between(q)
                                q += 1
                        act(lambda a: a.activation(out=x_sg, in_=P[2][:], func=AF.Silu), [PS[2]], [s_xsg])
                        dve(lambda v: v.tensor_tensor(out=x_h1, in0=x_sg, in1=P[3][:], op=ALU.mult), [s_xsg, PS[3]], [s_xh1])

                    def xT1(e):
                        transpose_to(x_h1, s_xh1, lambda c0, n: x_h1T[:, c0:c0 + n, :], s_xh1T, nchunks=4, pb=4)

                    def xY(e, wds):
                        for nb in range(4):
                            pb = 5 + nb % 2
                            for m in range(4):
                                wd_, sd_w = wds[m // 2]
                                pe(lambda tt: tt.matmul(P[pb][:], lhsT=x_h1T[:, m, :], rhs=wd_[:, m % 2, nb * 512:(nb + 1) * 512], start=(m == 0), stop=(m == 3)),
                                   [s_xh1T, sd_w], [PS[pb]], inc=(m == 3))
                            act(lambda a: a.copy(out=x_yb[:, nb * 512:(nb + 1) * 512], in_=P[pb][:]), [PS[pb]], [s_yb])

                    def xT2(e):
                        b = e % 2
                        transpose_to(x_Sel[b][:].rearrange("p t k -> p (t k)"), s_Sel[b], lambda c0, n: x_SelT[:, c0:c0 + n, :], s_SelT, nchunks=4, pb=4)

                    sc_rot = {"i": 0}

                    def xScatterT(e, t):
                        for nb in range(4):
                            pb = (7, 5, 6, 4)[sc_rot["i"] % 4]; sc_rot["i"] += 1
                            pe(lambda tt: tt.matmul(P[pb][:], lhsT=x_SelT[:, t, :], rhs=x_yb[:, nb * 512:(nb + 1) * 512], start=True, stop=True),
                               [s_SelT, s_yb], [PS[pb]])
                            dve(lambda v: v.scalar_tensor_tensor(out=R[:, t, nb * 512:(nb + 1) * 512], in0=P[pb][:], scalar=gw[:, t, e:e + 1],
                                                                 in1=R[:, t, nb * 512:(nb + 1) * 512], op0=ALU.mult, op1=ALU.add),
                                [PS[pb], s_gw, s_R[t]], [s_R[t]])

                    xSel(0); xGather(0)
                    wcur = eload(0)
                    for e in range(NE):
                        if e + 1 < NE:
                            xSel(e + 1)
                        xFFN(e, wcur[0], wcur[1], between=(lambda q, ep=e - 1: xScatterT(ep, q)) if e >= 1 else None)
                        wds = wcur[2]
                        if e + 1 < NE:
                            xGather(e + 1)
                        xT1(e)
                        xY(e, wds)
                        if e + 1 < NE:
                            wcur = eload(e + 1)
                        if blk == 0:
                            pc_issue(1)
                        xT2(e)
                    for t in range(4):
                        xScatterT(NE - 1, t)
                    if blk == 0:
                        pc_issue(len(pc_list))
                    ple_olds = [s_Sel[0], s_Sel[1], s_SelT, s_xh1, s_xh1T, s_yb]
                    PLE_O = NRS * RING_E
                o = PLE_O
                p_xb2 = take(D); p_pb = take(256); p_pT = take(2 * 512).rearrange("p (c k) -> p c k", k=512)
                p_pf = take(256, F32); p_sg = take(256, F32)
                assert o <= (38176 if SPARSE else HALO), o
                s_xb2 = Slot(); s_pb = Slot(); s_pT = Slot(); s_psg = Slot()
                Sched.alias(ple_olds, [s_xb2, s_pb, s_pT, s_pf, s_psg])
                moe_slots = moe_slots + [s_xb2, s_pb, s_pT, s_pf, s_psg]
                load_ln(4)
                for t in range(4):
                    layer_norm(t, EPS_A, p_xb2, s_xb2)
                    transpose_to(p_xb2, s_xb2, lambda c0, n: xT[:, c0:c0 + n, t * 128:(t + 1) * 128], s_xT, pb=0)
                    S.dma("sp", p_pf, pin[(blk * 4 + t) * 128:(blk * 4 + t + 1) * 128, :], writes=[s_pf], dslot=s_pf)
                    act(lambda a: a.copy(out=p_pb, in_=p_pf), [s_pf], [s_pb])
                    transpose_to(p_pb, s_pb, lambda c0, n: p_pT[:, c0:c0 + n, t * 128:(t + 1) * 128], s_pT, nchunks=2, pb=0)
                wpp3 = w_pp.rearrange("(c p) n -> p c n", p=128)
                for nb in range(8):
                    wt, wsl = wload(wcols(w_pg, nb * 256, 256), "")
                    wp, wpl = wload(wpp3[:, :, nb * 256:(nb + 1) * 256], "")
                    for t in range(4):
                        b1, b2 = ((1, 2), (3, 6), (4, 5), (7, 0))[t]
                        for c in range(NCH):
                            pe(lambda tt: tt.matmul(P[b1][:, 0:256], lhsT=xT[:, c, t * 128:(t + 1) * 128], rhs=wt[:, c, :], start=(c == 0), stop=(c == NCH - 1)),
                               [wsl, s_xT], [PS[b1]], inc=(c == NCH - 1))
                        for c in range(2):
                            pe(lambda tt: tt.matmul(P[b2][:, 0:256], lhsT=p_pT[:, c, t * 128:(t + 1) * 128], rhs=wp[:, c, :], start=(c == 0), stop=(c == 1)),
                               [wpl, s_pT], [PS[b2]], inc=(c == 1))
                        act(lambda a: a.activation(out=p_sg, in_=P[b1][:, 0:256], func=AF.Sigmoid), [PS[b1]], [s_psg])
                        dve(lambda v: v.scalar_tensor_tensor(out=p_sg, in0=P[b2][:, 0:256], scalar=IALPHA, in1=p_sg, op0=ALU.mult, op1=ALU.mult),
                            [PS[b2], s_psg], [s_psg])
                        dve(lambda v: v.tensor_tensor(out=R[:, t, nb * 256:(nb + 1) * 256], in0=R[:, t, nb * 256:(nb + 1) * 256], in1=p_sg, op=ALU.add),
                            [s_psg, s_R[t]], [s_R[t]])
                load_ln(6)
                for t in range(4):
                    layer_norm(t, EPS_A, p_xb2, s_xb2)
                    S.dma("sp", out[(blk * 4 + t) * 128:(blk * 4 + t + 1) * 128, :], R[:, t, :], reads=[s_R[t]], dslot=s_xl[t])
            S.dma("sp", flag_o, flagmax[:], reads=[s_flag], dslot=s_flag)
            S.wait_all("sp", s_R + [s_flag])
        print("built: ops", S.nops, "waits", S.nwaits, flush=True)
    return nc


def _consts():
    cst = np.zeros((128, 4, 128), np.float32)
    cst[:, 3, :] = np.arange(128, dtype=np.float32)[None, :]
    cst[:, 0, :] = np.eye(128, dtype=np.float32)
    cst[:, 1, :] = np.triu(np.ones((128, 128), np.float32))
    cst[:, 2, :] = 1.0
    slopes = np.exp2(-8.0 / 16 * np.arange(1, 17, dtype=np.float32)).astype(np.float32)
    j = np.arange(128)[:, None]; i = np.arange(128)[None, :]
    ab = np.zeros((128, 2, 16, 128), np.float32)
    for kt in range(2):
        dist = (i - j + (128 if kt == 0 else 0)).astype(np.float32)
        ok = (dist >= 0) & (dist < 128)
        for h in range(16):
            ab[:, kt, h, :] = np.where(ok, -slopes[h] * dist, NEGBIG)
    return cst, ab


_CACHE = {}


def kernel(x, p, ln_in_g, ln_in_b, w_in, attn_sinks, mlstm_b_i, mlstm_b_f, mlstm_norm_g,
           w_branch_attn, w_branch_mlstm, w_out, ln_mix_g, ln_mix_b, w_router, b_router,
           w_exp_gate, w_exp_up, w_exp_down, w_sh_gate, w_sh_up, w_sh_down, ln_ffn_g, ln_ffn_b,
           w_ple_proj, w_ple_gate, ln_ple_g, ln_ple_b):
    f = lambda a: np.ascontiguousarray(np.asarray(a, dtype=np.float32))
    x = f(x); p = f(p)
    B, SEQ, _ = x.shape
    NQ = 8 // B
    TOK = SEQ // NQ
    NPRE = (SEQ - TOK) // 128
    def get(sparse):
        key = (TOK, NPRE, sparse)
        if key not in _CACHE:
            _CACHE[key] = build(TOK, NPRE, sparse)
        return _CACHE[key]
    nc = get(True)
    cst, ab = _consts()
    shared = {
        "cst": cst, "abias": ab,
        "lng": np.stack([f(ln_in_g), f(ln_in_b), f(ln_mix_g)[0], f(ln_mix_b)[0], f(ln_ffn_g)[0], f(ln_ffn_b)[0],
                         f(ln_ple_g)[0], f(ln_ple_b)[0]]),
        "w_in": f(w_in)[0], "sinks": f(attn_sinks).reshape(1, 16),
        "bif": np.concatenate([f(mlstm_b_i).reshape(-1), f(mlstm_b_f).reshape(-1)]).reshape(1, 8),
        "ng": f(mlstm_norm_g).reshape(1, 1024),
        "w_ba": f(w_branch_attn)[0], "w_bm": f(w_branch_mlstm)[0], "w_out": f(w_out)[0],
        "w_rt": f(w_router)[0], "b_rt": f(b_router).reshape(1, NE),
        "w_eg": np.concatenate([f(w_exp_gate)[0], f(w_sh_gate)], axis=0),
        "w_eu": np.concatenate([f(w_exp_up)[0], f(w_sh_up)], axis=0),
        "w_ed": np.concatenate([f(w_exp_down)[0], f(w_sh_down)], axis=0),
        "w_pp": f(w_ple_proj)[0], "w_pg": f(w_ple_gate)[0],
    }
    in_maps = []
    for c in range(8):
        b, j = c // NQ, c % NQ
        end = (j + 1) * TOK
        xin = np.zeros((SEQ, D), np.float32)
        xin[SEQ - end:] = x[b, :end]
        vm = np.zeros((128, NPRE + 1), np.float32)
        nvalid = (j * TOK) // 128
        if nvalid > 0:
            vm[:, NPRE - nvalid:NPRE] = 1.0
        vm[:, NPRE] = 1.0 if j > 0 else 0.0
        m = dict(shared)
        m["xin"] = xin; m["vmk"] = vm
        m["pin"] = np.ascontiguousarray(p[0, b, j * TOK:(j + 1) * TOK])
        in_maps.append(m)
    res = run_bass_kernel_spmd(nc, in_maps, core_ids=list(range(8)))
    if max(float(np.asarray(r["flag"]).max()) for r in res.results) > 128.5:
        res = run_bass_kernel_spmd(get(False), in_maps, core_ids=list(range(8)))
    outp = np.zeros((B, SEQ, D), np.float32)
    for c in range(8):
        b, j = c // NQ, c % NQ
        outp[b, j * TOK:(j + 1) * TOK] = np.asarray(res.results[c]["out"], dtype=np.float32)
    return outp
```
